# Optimizing a Trainium2 kernel written in Bass

```python
import jax, jax.numpy as jnp
from jax import lax
import numpy as np

D_MODEL = 1024
BATCH = 2
SEQ = 8192
DEPTH = 1

HEAD_DIM = 64
MOBA_HEADS = 8
FOX_HEADS = 8
A_WIDTH = MOBA_HEADS * HEAD_DIM
B_WIDTH = FOX_HEADS * HEAD_DIM
ROPE_DIM = HEAD_DIM // 4
ROPE_THETA = 500000.0
MOBA_BLOCK = 256
MOBA_TOPK = 3
MOBA_Q_CHUNK = 64
Q_BLOCK = 128
FORGET_BIAS_CENTER = 3.0
PEER_HEADS = 8
PEER_NKEYS = 128
PEER_NEXPERTS = PEER_NKEYS * PEER_NKEYS
PEER_KEY_DIM = 256
PEER_HALF = PEER_KEY_DIM // 2
PEER_TOPK = 16
PEER_TOKEN_CHUNK = 128
IN_COLS = 3 * A_WIDTH + 3 * B_WIDTH + FOX_HEADS + 2 * D_MODEL
RMS_EPS = 1e-6

kernel_name = "moba_fox_gated_peer_hybrid"


def rms_norm(x, g):
    xf = x.astype(jnp.float32)
    y = xf * lax.rsqrt(jnp.mean(xf * xf, axis=-1, keepdims=True) + RMS_EPS)
    return (y * g.astype(jnp.float32)).astype(x.dtype)


def partial_rope(x, pos):
    half = ROPE_DIM // 2
    inv_freq = jnp.power(ROPE_THETA, -jnp.arange(half, dtype=jnp.float32) / half)
    ang = pos.astype(jnp.float32)[:, None] * inv_freq[None, :]
    cos = jnp.cos(ang).astype(x.dtype)
    sin = jnp.sin(ang).astype(x.dtype)
    x1 = x[..., :half]
    x2 = x[..., half:ROPE_DIM]
    return jnp.concatenate([x1 * cos - x2 * sin, x1 * sin + x2 * cos, x[..., ROPE_DIM:]], axis=-1)


def moba_attention(q, k, v):
    B, H, S, dh = q.shape
    L = MOBA_BLOCK
    nb = -(-S // L)
    pad = nb * L - S
    k_blk = jnp.pad(k, ((0, 0), (0, 0), (0, pad), (0, 0))).reshape(B, H, nb, L, dh)
    v_blk = jnp.pad(v, ((0, 0), (0, 0), (0, pad), (0, 0))).reshape(B, H, nb, L, dh)
    k_mean = jnp.mean(k_blk.astype(jnp.float32), axis=3).astype(k.dtype)
    scale = dh ** -0.5
    qpos = jnp.arange(S)
    gate = jnp.einsum('bhsd,bhnd->bhsn', q, k_mean).astype(jnp.float32)
    past = jnp.arange(nb)[None, :] < (qpos // L)[:, None]
    gate = jnp.where(past, gate, -jnp.inf)
    kk = min(MOBA_TOPK, nb)
    top_val, top_idx = lax.top_k(gate, kk)
    sel_valid = jnp.isfinite(top_val)
    C = MOBA_Q_CHUNK
    nq = S // C
    q_c = q.reshape(B, H, nq, C, dh).transpose(2, 0, 1, 3, 4)
    idx_c = top_idx.reshape(B, H, nq, C, kk).transpose(2, 0, 1, 3, 4)
    val_c = sel_valid.reshape(B, H, nq, C, kk).transpose(2, 0, 1, 3, 4)
    take_blocks = jax.vmap(jax.vmap(lambda tab, ix: tab[ix]))

    def step(args):
        ci, qi, ii, vi = args
        q0 = ci * C
        own = q0 // L
        ks = take_blocks(k_blk, ii)
        vs = take_blocks(v_blk, ii)
        s_sel = jnp.einsum('bhcd,bhcjld->bhcjl', qi, ks).astype(jnp.float32) * scale
        s_sel = jnp.where(vi[..., None], s_sel, -jnp.inf).reshape(B, H, C, kk * L)
        k_own = lax.dynamic_index_in_dim(k_blk, own, axis=2, keepdims=False)
        v_own = lax.dynamic_index_in_dim(v_blk, own, axis=2, keepdims=False)
        s_own = jnp.einsum('bhcd,bhld->bhcl', qi, k_own).astype(jnp.float32) * scale
        causal = (own * L + jnp.arange(L))[None, :] <= (q0 + jnp.arange(C))[:, None]
        s_own = jnp.where(causal, s_own, -jnp.inf)
        p = jax.nn.softmax(jnp.concatenate([s_sel, s_own], axis=-1), axis=-1)
        p_sel = p[..., :kk * L].reshape(B, H, C, kk, L).astype(v.dtype)
        p_own = p[..., kk * L:].astype(v.dtype)
        return (jnp.einsum('bhcjl,bhcjld->bhcd', p_sel, vs)
                + jnp.einsum('bhcl,bhld->bhcd', p_own, v_own))

    out = lax.map(step, (jnp.arange(nq), q_c, idx_c, val_c))
    return out.transpose(1, 2, 0, 3, 4).reshape(B, H, S, dh)


def forgetting_attention(q, k, v, log_f):
    B, H, S, dh = q.shape
    scale = dh ** -0.5
    c = jnp.cumsum(log_f, axis=-1)
    nq = S // Q_BLOCK
    q_c = q.reshape(B, H, nq, Q_BLOCK, dh).transpose(2, 0, 1, 3, 4)
    c_c = c.reshape(B, H, nq, Q_BLOCK).transpose(2, 0, 1, 3)
    kpos = jnp.arange(S)

    def step(args):
        ci, qi, cq = args
        s = (jnp.einsum('bhqd,bhkd->bhqk', qi, k).astype(jnp.float32) * scale
             + cq[..., None] - c[:, :, None, :])
        qpos = ci * Q_BLOCK + jnp.arange(Q_BLOCK)
        s = jnp.where(kpos[None, :] <= qpos[:, None], s, -jnp.inf)
        p = jax.nn.softmax(s, axis=-1).astype(v.dtype)
        return jnp.einsum('bhqk,bhkd->bhqd', p, v)

    out = lax.map(step, (jnp.arange(nq), q_c, c_c))
    return out.transpose(1, 2, 0, 3, 4).reshape(B, H, S, dh)


def hybrid_mixer(xn, w_in, b_forget, w_branch_a, w_branch_b, w_out):
    B, S, _ = xn.shape
    proj = xn @ w_in
    cuts = [A_WIDTH, 2 * A_WIDTH, 3 * A_WIDTH,
            3 * A_WIDTH + B_WIDTH, 3 * A_WIDTH + 2 * B_WIDTH, 3 * A_WIDTH + 3 * B_WIDTH,
            3 * A_WIDTH + 3 * B_WIDTH + FOX_HEADS, 3 * A_WIDTH + 3 * B_WIDTH + FOX_HEADS + D_MODEL]
    qa, ka, va, qb, kb, vb, f_logit, g_a, g_b = jnp.split(proj, cuts, axis=-1)

    def heads(t, n):
        return t.reshape(B, S, n, HEAD_DIM).transpose(0, 2, 1, 3)

    pos = jnp.arange(S)
    ya = moba_attention(partial_rope(heads(qa, MOBA_HEADS), pos),
                        partial_rope(heads(ka, MOBA_HEADS), pos),
                        heads(va, MOBA_HEADS))
    ya = ya.transpose(0, 2, 1, 3).reshape(B, S, A_WIDTH) @ w_branch_a
    log_f = jax.nn.log_sigmoid((f_logit + b_forget).astype(jnp.float32)).transpose(0, 2, 1)
    yb = forgetting_attention(heads(qb, FOX_HEADS), heads(kb, FOX_HEADS), heads(vb, FOX_HEADS), log_f)
    yb = yb.transpose(0, 2, 1, 3).reshape(B, S, B_WIDTH) @ w_branch_b
    merged = jax.nn.sigmoid(g_a) * ya + jax.nn.sigmoid(g_b) * yb
    return merged @ w_out


def peer_ffn(xn, w_peer_q, sub_keys, expert_u, expert_v):
    B, S, D = xn.shape
    T = B * S
    xt = xn.reshape(T, D)
    qh = (xt @ w_peer_q).reshape(T, PEER_HEADS, 2, PEER_HALF)
    s = jnp.einsum('thpd,hpnd->thpn', qh, sub_keys).astype(jnp.float32)
    sv, si = lax.top_k(s, PEER_TOPK)
    cand = (sv[:, :, 0, :, None] + sv[:, :, 1, None, :]).reshape(T, PEER_HEADS, PEER_TOPK * PEER_TOPK)
    cand_idx = (si[:, :, 0, :, None] * PEER_NKEYS + si[:, :, 1, None, :]).reshape(T, PEER_HEADS, PEER_TOPK * PEER_TOPK)
    best, pick = lax.top_k(cand, PEER_TOPK)
    expert_idx = jnp.take_along_axis(cand_idx, pick, axis=-1)
    gates = jax.nn.softmax(best, axis=-1)
    C = PEER_TOKEN_CHUNK
    n_chunks = T // C

    def step(args):
        xc, ic, gc = args
        u = expert_u[ic]
        hval = jnp.einsum('cd,chkd->chk', xc, u).astype(jnp.float32)
        a = (gc * jax.nn.gelu(hval, approximate=False)).astype(xc.dtype)
        return jnp.einsum('chk,chkd->cd', a, expert_v[ic])

    out = lax.map(step, (xt.reshape(n_chunks, C, D),
                         expert_idx.reshape(n_chunks, C, PEER_HEADS, PEER_TOPK),
                         gates.reshape(n_chunks, C, PEER_HEADS, PEER_TOPK)))
    return out.reshape(B, S, D)


def setup_inputs(seed: int = 0) -> dict:
    key = jax.random.key(seed)
    ks = jax.random.split(key, 14)
    f32 = jnp.float32

    def nrm(k, shape, scale):
        return jax.random.normal(k, shape, f32) * scale

    return {
        "x": nrm(ks[0], (BATCH, SEQ, D_MODEL), 1.0),
        "norm_mix_g": 1.0 + nrm(ks[1], (DEPTH, D_MODEL), 0.02),
        "w_in": nrm(ks[2], (DEPTH, D_MODEL, IN_COLS), D_MODEL ** -0.5),
        "b_forget": FORGET_BIAS_CENTER + nrm(ks[3], (DEPTH, FOX_HEADS), 0.5),
        "w_branch_a": nrm(ks[4], (DEPTH, A_WIDTH, D_MODEL), A_WIDTH ** -0.5),
        "w_branch_b": nrm(ks[5], (DEPTH, B_WIDTH, D_MODEL), B_WIDTH ** -0.5),
        "w_out": nrm(ks[6], (DEPTH, D_MODEL, D_MODEL), D_MODEL ** -0.5),
        "norm_ffn_g": 1.0 + nrm(ks[7], (DEPTH, D_MODEL), 0.02),
        "w_peer_q": nrm(ks[8], (DEPTH, D_MODEL, PEER_HEADS * PEER_KEY_DIM), D_MODEL ** -0.5),
        "peer_sub_keys": nrm(ks[9], (DEPTH, PEER_HEADS, 2, PEER_NKEYS, PEER_HALF), PEER_HALF ** -0.5),
        "peer_expert_u": nrm(ks[10], (DEPTH, PEER_NEXPERTS, D_MODEL), D_MODEL ** -0.5),
        "peer_expert_v": nrm(ks[11], (DEPTH, PEER_NEXPERTS, D_MODEL), PEER_HEADS ** -0.5),
        "norm_final_g": 1.0 + nrm(ks[12], (D_MODEL,), 0.02),
    }


def reference(x, norm_mix_g, w_in, b_forget, w_branch_a, w_branch_b, w_out, norm_ffn_g,
              w_peer_q, peer_sub_keys, peer_expert_u, peer_expert_v, norm_final_g):
    h = x
    for l in range(DEPTH):
        h = h + hybrid_mixer(rms_norm(h, norm_mix_g[l]), w_in[l], b_forget[l],
                             w_branch_a[l], w_branch_b[l], w_out[l])
        h = h + peer_ffn(rms_norm(h, norm_ffn_g[l]), w_peer_q[l], peer_sub_keys[l],
                         peer_expert_u[l], peer_expert_v[l])
    return rms_norm(h, norm_final_g)
```

```python
import numpy as np
from contextlib import ExitStack
import concourse.bass as bass
import concourse.mybir as mybir
from concourse.bass_utils import run_bass_kernel_spmd

F32 = mybir.dt.float32
BF16 = mybir.dt.bfloat16
U32 = mybir.dt.uint32
I32 = mybir.dt.int32
AF = mybir.ActivationFunctionType
ALU = mybir.AluOpType
AX = mybir.AxisListType

NDS = 48
SAME_ENG_WAIT = True
NEG = -30000.0
FILLER = False
DEBUG = False
STOP_AFTER = 99


class FW:
    def __init__(self, nc):
        self.nc = nc
        self.E = {'pe': nc.tensor, 'act': nc.scalar, 'dve': nc.vector,
                  'pool': nc.gpsimd, 'sp': nc.sync}
        self.sems = {}
        for e in self.E:
            self.sems['c' + e] = nc.alloc_semaphore(name='c_' + e)
        for i in range(NDS):
            self.sems['d%d' % i] = nc.alloc_semaphore(name='d_%d' % i)
        self.val = {k: 0 for k in self.sems}
        self.seen = {e: {} for e in self.E}
        self.res = {}
        self.dnext = {}
        self.ninst = {e: 0 for e in self.E}

    def _wait(self, eng, toks):
        need = {}
        for t in toks:
            if t is None:
                continue
            s, v = t
            if s == 'c' + eng and (eng == 'pe' or not SAME_ENG_WAIT):
                continue
            if v > need.get(s, 0):
                need[s] = v
        for s, v in need.items():
            if v > self.seen[eng].get(s, 0):
                self.E[eng].wait_ge(self.sems[s], v)
                self.seen[eng][s] = v

    def _deps(self, reads, writes):
        toks = []
        for r in reads:
            st = self.res.get(r)
            if st:
                toks.append(st['w'])
        for w in writes:
            st = self.res.get(w)
            if st:
                toks.append(st['w'])
                toks.extend(st['r'].values())
        return toks

    def _commit(self, tok, reads, writes):
        for r in reads:
            st = self.res.setdefault(r, {'w': None, 'r': {}})
            st['r'][tok[0]] = tok
        for w in writes:
            self.res[w] = {'w': tok, 'r': {}}

    def prewait(self, eng, reads=(), writes=()):
        self._wait(eng, self._deps(reads, writes))

    def op(self, eng, fn, reads=(), writes=()):
        self._wait(eng, self._deps(reads, writes))
        inst = fn(self.E[eng])
        s = 'c' + eng
        self.val[s] += 1
        inst.then_inc(self.sems[s], 1)
        self.ninst[eng] += 1
        self._commit((s, self.val[s]), reads, writes)
        return inst

    def dma(self, eng, fn, reads=(), writes=()):
        half = NDS // 2
        k = self.dnext.get(eng, 0)
        self.dnext[eng] = (k + 1) % half
        i = k + (half if eng == 'pool' else 0)
        s = 'd%d' % i
        toks = self._deps(reads, writes)
        toks.append((s, self.val[s]) if self.val[s] else None)
        self._wait(eng, toks)
        inst = fn(self.E[eng])
        self.val[s] += 16
        inst.then_inc(self.sems[s], 16)
        self.ninst[eng] += 1
        self._commit((s, self.val[s]), reads, writes)
        return inst

    def barrier(self):
        toks = [(s, v) for s, v in self.val.items() if v]
        for e in self.E:
            for s, v in toks:
                if s == 'c' + e:
                    continue
                if v > self.seen[e].get(s, 0):
                    self.E[e].wait_ge(self.sems[s], v)
                    self.seen[e][s] = v
        self.res = {}


NT = 64
NO = 16
D = 1024


def core_units(c):
    j = c % 4
    return [(0, k) for k in range(8) if k < j] + [(1, k) for k in range(8) if k < 7 - j and k != j]


def chunk_order(c):
    j = c % 4
    return [j, 7 - j] + [k for (_, k) in core_units(c)]


def build():
    nc = bass.Bass("TRN2", target_bir_lowering=False)
    fw = FW(nc)

    def din(name, shape, dt=F32):
        return nc.dram_tensor(name, list(shape), dt, kind="ExternalInput").ap()

    def dscr(name, shape, dt):
        return nc.dram_tensor(name, list(shape), dt,
                              kind="ExternalOutput" if DEBUG else "Internal").ap()

    xT = din("xT", [NT, 128, 8, 128])
    xown = din("xown", [NO, 128, D])
    w_kv = din("w_kv", [128, 8, 2056])
    w_q = din("w_q", [128, 8, 1024])
    w_g = din("w_g", [128, 8, 2048])
    g_mix = din("g_mix", [128, 8])
    bfg = din("bfg", [128, 8])
    ropec = din("ropec", [128, NT, 8])
    ropes = din("ropes", [128, NT, 8])
    wba = din("wba", [64, 8, D])
    wbb = din("wbb", [64, 8, D])
    wout = din("wout", [128, 8, D])
    g_ffn = din("g_ffn", [128, D])
    wpq = din("wpq", [128, 8, 2048])
    skT = din("skT", [128, 16, 128])
    Ur = din("Ur", [128, 128, 8, 128])
    Vr = din("Vr", [128, 128, D])
    iota128d = din("iota128", [128, 128])
    g_fin = din("g_fin", [128, D])
    trim = din("trim", [128, 4, 512])
    tric = din("tric", [128, 128])
    onehot = din("onehot", [32, 8192])
    pastb = din("pastb", [128, NO, 32])
    ownm = din("ownm", [128, NO, 32])
    visA = din("visA", [128, NT])
    visB = din("visB", [128, NT])
    Mord = din("Mord", [64, 64])
    iota16 = din("iota16", [128, 16])
    uwd = din("uw", [128, 6, 2])
    y = nc.dram_tensor("y", [NO, 128, D], F32, kind="ExternalOutput").ap()

    KTA = dscr("KTA", [8, 64, 8192], BF16)
    KTB = dscr("KTB", [8, 64, 8192], BF16)
    VA = dscr("VA", [8, 128, NT, 65], BF16)
    VB = dscr("VB", [8, 128, NT, 65], BF16)
    QTA = dscr("QTA", [8, 96, 2048], BF16)
    QTB = dscr("QTB", [8, 65, 2048], BF16)
    ATT = dscr("ATT", [2, 8, 64, 2048], BF16)
    HS = dscr("HS", [NO, 128, D], F32)
    Gscr = nc.dram_tensor("Gscr", [128, 128, 2048], BF16, kind="Internal").ap()

    psall = nc.alloc_psum_tensor("psall", [128, 4096], F32)
    ps = [psall[:, i * 512:(i + 1) * 512] for i in range(8)]
    P = ['ps%d' % i for i in range(8)]

    glob = ExitStack()

    def sb(stack, name, shape, dt):
        return stack.enter_context(nc.sbuf_tensor(name, list(shape), dt))

    identf = sb(glob, "identf", [128, 128], F32)
    identb = sb(glob, "identb", [128, 128], BF16)
    ones_f = sb(glob, "ones_f", [128, 128], F32)
    rstd_all = sb(glob, "rstd_all", [128, NT], F32)
    gmix_sb = sb(glob, "gmix_sb", [128, 8], F32)
    negcA = sb(glob, "negcA", [128, NT, 8], F32)
    negcB = sb(glob, "negcB", [128, NT, 8], F32)
    eps_sb = sb(glob, "eps_sb", [128, 1], F32)

    fw.op('pool', lambda e: e.memset(identf[:], 0.0), writes=['identf'])
    fw.op('pool', lambda e: e.affine_select(out=identf[:], in_=identf[:], pattern=[[-1, 128]],
                                            compare_op=ALU.not_equal, fill=1.0, base=0,
                                            channel_multiplier=1), reads=['identf'], writes=['identf'])
    fw.op('dve', lambda e: e.tensor_copy(out=identb[:], in_=identf[:]), reads=['identf'], writes=['identb'])
    fw.op('dve', lambda e: e.memset(ones_f[:], 1.0), writes=['ones_f'])
    fw.op('dve', lambda e: e.memset(eps_sb[:], 1e-6), writes=['eps_sb'])
    fw.dma('sp', lambda e: e.dma_start(out=gmix_sb[:], in_=g_mix), writes=['gmix'])

    def load_xg(i, xtf, xg, slot):
        fw.dma('sp', lambda e: e.dma_start(out=xtf[slot][:], in_=xT[i]), writes=[('xtf', slot)])
        fw.op('dve', lambda e: e.tensor_tensor(
            out=xg[slot][:], in0=xtf[slot][:],
            in1=gmix_sb[:].unsqueeze(2).to_broadcast([128, 8, 128]), op=ALU.mult),
            reads=[('xtf', slot), 'gmix'], writes=[('xg', slot)])

    def rope(src3, i, tmp, key):
        x1 = src3[:, :, 0:8]
        x2 = src3[:, :, 8:16]
        cs = ropec_sb[:, i, :].unsqueeze(1).to_broadcast([128, 8, 8])
        sn = ropes_sb[:, i, :].unsqueeze(1).to_broadcast([128, 8, 8])
        rk = [key, 'rope_tab']
        fw.op('pool', lambda e: e.tensor_tensor(out=tmp[:, 0], in0=x1, in1=cs, op=ALU.mult), reads=rk, writes=['rt0'])
        fw.op('pool', lambda e: e.tensor_tensor(out=tmp[:, 1], in0=x2, in1=sn, op=ALU.mult), reads=rk, writes=['rt1'])
        fw.op('pool', lambda e: e.tensor_tensor(out=tmp[:, 2], in0=x1, in1=sn, op=ALU.mult), reads=rk, writes=['rt2'])
        fw.op('pool', lambda e: e.tensor_tensor(out=tmp[:, 3], in0=x2, in1=cs, op=ALU.mult), reads=rk, writes=['rt3'])
        fw.op('pool', lambda e: e.tensor_tensor(out=x1, in0=tmp[:, 0], in1=tmp[:, 1], op=ALU.subtract),
              reads=['rt0', 'rt1'], writes=[key])
        fw.op('pool', lambda e: e.tensor_tensor(out=x2, in0=tmp[:, 2], in1=tmp[:, 3], op=ALU.add),
              reads=['rt2', 'rt3'], writes=[key])

    p1 = ExitStack()
    p1a = ExitStack()
    ropec_sb = sb(p1, "ropec_sb", [128, NT, 8], F32)
    ropes_sb = sb(p1, "ropes_sb", [128, NT, 8], F32)
    xtf = [sb(p1, "xtf%d" % k, [128, 8, 128], F32) for k in range(2)]
    xg = [sb(p1, "xg%d" % k, [128, 8, 128], BF16) for k in range(2)]
    rtmp = sb(p1, "rtmp", [128, 4, 8, 8], F32)
    kmT = sb(p1, "kmT", [64, 8, 32], BF16)
    negc = sb(p1, "negc", [128, NT, 8], F32)
    wkv_sb = sb(p1a, "wkv_sb", [128, 8, 2056], BF16)
    sq = sb(p1a, "sq", [128, 8, 128], BF16)
    ones_b = sb(p1a, "ones_b", [128, 1], BF16)
    ssb = sb(p1a, "ssb", [128, 1], F32)
    kf = sb(p1a, "kf", [128, 8, 64], F32)
    kab = [sb(p1a, "kab%d" % k, [128, 8, 64], BF16) for k in range(2)]
    kbb = [sb(p1a, "kbb%d" % k, [128, 8, 64], BF16) for k in range(2)]
    KV_BACK = [None]
    stKA = [sb(p1a, "stKA%d" % k, [64, 8, 512], BF16) for k in range(2)]
    stKB = [sb(p1a, "stKB%d" % k, [64, 8, 512], BF16) for k in range(2)]
    stVA = [sb(p1a, "stVA%d" % k, [128, 8, 8, 65], BF16) for k in range(2)]
    stVB = [sb(p1a, "stVB%d" % k, [128, 8, 8, 65], BF16) for k in range(2)]
    fl_all = sb(p1a, "fl_all", [128, NT, 8], F32)
    ksum = sb(p1a, "ksum", [64, 8, NT], F32)

    for k in range(8):
        fw.dma('pool', lambda e, k=k: e.dma_start(out=wkv_sb[:, k, :], in_=w_kv[:, k, :]), writes=['wkv'])
    fw.dma('sp', lambda e: e.dma_start(out=ropec_sb[:], in_=ropec), writes=['rope_tab'])
    fw.dma('sp', lambda e: e.dma_start(out=ropes_sb[:], in_=ropes), writes=['rope_tab'])
    fw.op('pool', lambda e: e.memset(ones_b[:], 1.0), writes=['ones_b'])
    for k in range(2):
        fw.op('pool', lambda e, k=k: e.memset(stVA[k][:], 1.0), writes=[('stVA', k)])
        fw.op('pool', lambda e, k=k: e.memset(stVB[k][:], 1.0), writes=[('stVB', k)])

    def kv_back(i):
        g4 = (i // 4) % 2
        kb2 = i % 2
        ptA = ps[6][0:64, :].bitcast(BF16).rearrange("p (h t) -> p h t", h=8)
        ptB = ps[7][0:64, :].bitcast(BF16).rearrange("p (h t) -> p h t", h=8)
        for h in range(8):
            fw.op('pe', lambda e, h=h: e.transpose(out=ptA[:, h, :], in_=kab[kb2][:, h, :], identity=identb[:]),
                  reads=[('kab', kb2), 'identb'], writes=[P[6]])
        fw.op('dve', lambda e: e.tensor_copy(out=stKA[g4][:, :, (i % 4) * 128:(i % 4 + 1) * 128], in_=ptA),
              reads=[P[6]], writes=[('stKA', g4)])
        fw.op('dve', lambda e: e.tensor_reduce(out=ksum[:, :, i], in_=ptA, axis=AX.X, op=ALU.add),
              reads=[P[6]], writes=['ksum'])
        for h in range(8):
            fw.op('pe', lambda e, h=h: e.transpose(out=ptB[:, h, :], in_=kbb[kb2][:, h, :], identity=identb[:]),
                  reads=[('kbb', kb2), 'identb'], writes=[P[7]])
        fw.op('dve', lambda e: e.tensor_copy(out=stKB[g4][:, :, (i % 4) * 128:(i % 4 + 1) * 128], in_=ptB),
              reads=[P[7]], writes=[('stKB', g4)])
        if i % 4 == 3:
            g = i // 4
            fw.dma('sp', lambda e, g=g: e.dma_start(
                out=KTA[:, :, g * 512:(g + 1) * 512].rearrange("h d s -> d h s"), in_=stKA[g4][:]),
                reads=[('stKA', g4)], writes=['KTA'])
            fw.dma('sp', lambda e, g=g: e.dma_start(
                out=KTB[:, :, g * 512:(g + 1) * 512].rearrange("h d s -> d h s"), in_=stKB[g4][:]),
                reads=[('stKB', g4)], writes=['KTB'])

    KV_BACK[0] = kv_back
    for i in range(NT):
        s = i % 2
        load_xg(i, xtf, xg, s)
        if i >= 1:
            KV_BACK[0](i - 1)
        fw.op('act', lambda e: e.activation(out=sq[:], in_=xtf[s][:], func=AF.Square),
              reads=[('xtf', s)], writes=['sq'])
        for c in range(8):
            fw.op('pe', lambda e, c=c: e.matmul(ps[5][:, 0:1], lhsT=sq[:, c, :], rhs=ones_b[:, 0:1],
                                                 start=(c == 0), stop=(c == 7)),
                  reads=['sq', 'ones_b'], writes=[P[5]])
        fw.op('act', lambda e: e.activation(out=ssb[:], in_=ps[5][:, 0:1], func=AF.Sqrt,
                                            bias=eps_sb[:], scale=1.0 / D),
              reads=[P[5], 'eps_sb'], writes=['ssb'])
        fw.op('dve', lambda e: e.reciprocal(out=rstd_all[:, i:i + 1], in_=ssb[:]),
              reads=['ssb'], writes=[('rstd', i)])
        rs = rstd_all[:, i:i + 1]
        for bnk, (c0, c1) in enumerate([(0, 512), (512, 1024), (1024, 1536), (1536, 2048), (2048, 2056)]):
            for c in range(8):
                fw.op('pe', lambda e, c=c, bnk=bnk, c0=c0, c1=c1: e.matmul(
                    ps[bnk][:, 0:c1 - c0], lhsT=xg[s][:, c, :], rhs=wkv_sb[:, c, c0:c1],
                    start=(c == 0), stop=(c == 7)),
                    reads=[('xg', s), 'wkv'], writes=[P[bnk]])
        g8 = (i // 8) % 2
        kb2 = i % 2
        fw.op('act', lambda e: e.activation(out=kf[:].rearrange("p h d -> p (h d)"), in_=ps[0][:],
                                            func=AF.Copy, scale=rs),
              reads=[P[0], ('rstd', i)], writes=['kf'])
        rope(kf[:], i, rtmp, 'kf')
        fw.op('pool', lambda e: e.tensor_copy(out=kab[kb2][:], in_=kf[:]), reads=['kf'], writes=[('kab', kb2)])
        fw.op('dve', lambda e: e.tensor_scalar(
            out=stVA[g8][:, :, i % 8, 0:64], in0=ps[1][:].rearrange("p (h d) -> p h d", h=8),
            scalar1=rs, scalar2=None, op0=ALU.mult),
            reads=[P[1], ('rstd', i)], writes=[('stVA', g8)])
        fw.op('act', lambda e: e.activation(out=kbb[kb2][:].rearrange("p h d -> p (h d)"), in_=ps[2][:],
                                            func=AF.Copy, scale=rs),
              reads=[P[2], ('rstd', i)], writes=[('kbb', kb2)])
        fw.op('act', lambda e: e.activation(
            out=stVB[g8][:, :, i % 8, 0:64], in_=ps[3][:].rearrange("p (h d) -> p h d", h=8),
            func=AF.Copy, scale=rs),
            reads=[P[3], ('rstd', i)], writes=[('stVB', g8)])
        fw.op('dve', lambda e: e.tensor_scalar(out=fl_all[:, i, :], in0=ps[4][:, 0:8], scalar1=rs,
                                               scalar2=None, op0=ALU.mult),
              reads=[P[4], ('rstd', i)], writes=['fl_all'])
        if i % 8 == 7:
            g = i // 8
            fw.dma('sp', lambda e, g=g: e.dma_start(
                out=VA[:, :, g * 8:(g + 1) * 8, :].rearrange("h t i e -> t h i e"), in_=stVA[g8][:]),
                reads=[('stVA', g8)], writes=['VA'])
            fw.dma('sp', lambda e, g=g: e.dma_start(
                out=VB[:, :, g * 8:(g + 1) * 8, :].rearrange("h t i e -> t h i e"), in_=stVB[g8][:]),
                reads=[('stVB', g8)], writes=['VB'])

    kv_back(NT - 1)

    kv = ksum[:].rearrange("d h (n two) -> d h n two", two=2)
    ksum2 = sb(p1a, "ksum2", [64, 8, 32], F32)
    fw.op('dve', lambda e: e.tensor_tensor(out=ksum2[:], in0=kv[:, :, :, 0], in1=kv[:, :, :, 1], op=ALU.add),
          reads=['ksum'], writes=['ksum2'])
    fw.op('dve', lambda e: e.tensor_scalar(out=kmT[:], in0=ksum2[:], scalar1=1.0 / 256, scalar2=None, op0=ALU.mult),
          reads=['ksum2'], writes=['kmT'])

    bf_sb = sb(p1a, "bf_sb", [128, 8], F32)
    tri_sb = sb(p1a, "tri_sb", [128, 128], F32)
    M_sb = sb(p1a, "M_sb", [64, 64], F32)
    visA_sb = sb(p1a, "visA_sb", [128, NT], F32)
    visB_sb = sb(p1a, "visB_sb", [128, NT], F32)
    Lt = sb(p1a, "Lt", [128, NT, 8], F32)
    T2 = sb(p1a, "T2", [64, 8], F32)
    R = sb(p1a, "R", [64, NT, 8], F32)
    fw.dma('sp', lambda e: e.dma_start(out=bf_sb[:], in_=bfg), writes=['bf_sb'])
    fw.dma('sp', lambda e: e.dma_start(out=tri_sb[:], in_=tric), writes=['tri_sb'])
    fw.dma('sp', lambda e: e.dma_start(out=M_sb[:], in_=Mord), writes=['M_sb'])
    fw.dma('sp', lambda e: e.dma_start(out=visA_sb[:], in_=visA), writes=['visA_sb'])
    fw.dma('sp', lambda e: e.dma_start(out=visB_sb[:], in_=visB), writes=['visB_sb'])
    fw.op('dve', lambda e: e.tensor_tensor(out=Lt[:], in0=fl_all[:],
                                           in1=bf_sb[:].unsqueeze(1).to_broadcast([128, NT, 8]), op=ALU.add),
          reads=['fl_all', 'bf_sb'], writes=['Lt'])
    Lt2 = Lt[:].rearrange("p j h -> p (j h)")
    fw.op('act', lambda e: e.activation(out=Lt2, in_=Lt2, func=AF.Exp, scale=-1.0), reads=['Lt'], writes=['Lt'])
    fw.op('act', lambda e: e.activation(out=Lt2, in_=Lt2, func=AF.Ln, bias=ones_f[:, 0:1], scale=1.0),
          reads=['Lt', 'ones_f'], writes=['Lt'])
    for h in range(8):
        fw.op('pe', lambda e, h=h: e.matmul(ps[1][0:64, h:h + 1], lhsT=Lt[:, :, h], rhs=ones_f[:, 0:1],
                                             start=True, stop=True),
              reads=['Lt', 'ones_f'], writes=[P[1]])
    fw.op('dve', lambda e: e.tensor_copy(out=T2[:], in_=ps[1][0:64, 0:8]), reads=[P[1]], writes=['T2'])
    fw.op('dve', lambda e: e.tensor_tensor(out=R[:], in0=M_sb[:].unsqueeze(2).to_broadcast([64, NT, 8]),
                                           in1=T2[:].unsqueeze(1).to_broadcast([64, NT, 8]), op=ALU.mult),
          reads=['M_sb', 'T2'], writes=['R'])
    fw.op('pe', lambda e: e.matmul(ps[0][:], lhsT=tri_sb[:], rhs=Lt2, start=True, stop=False),
          reads=['tri_sb', 'Lt'], writes=[P[0]])
    fw.op('pe', lambda e: e.matmul(ps[0][:], lhsT=ones_f[0:64, :], rhs=R[:].rearrange("p j h -> p (j h)"),
                                   start=False, stop=True),
          reads=['ones_f', 'R'], writes=[P[0]])
    fw.op('dve', lambda e: e.tensor_copy(out=negc[:].rearrange("p j h -> p (j h)"), in_=ps[0][:]),
          reads=[P[0]], writes=['negc'])
    fw.op('dve', lambda e: e.tensor_tensor(out=negcA[:], in0=negc[:],
                                           in1=visA_sb[:].unsqueeze(2).to_broadcast([128, NT, 8]), op=ALU.add),
          reads=['negc', 'visA_sb'], writes=['negcA'])
    fw.op('dve', lambda e: e.tensor_tensor(out=negcB[:], in0=negc[:],
                                           in1=visB_sb[:].unsqueeze(2).to_broadcast([128, NT, 8]), op=ALU.add),
          reads=['negc', 'visB_sb'], writes=['negcB'])

    fw.barrier()
    p1a.close()
    wq_sb = sb(p1, "wq_sb", [128, 8, 1024], BF16)
    for k in range(8):
        fw.dma('pool', lambda e, k=k: e.dma_start(out=wq_sb[:, k, :], in_=w_q[:, k, :]), writes=['wq'])
    pastb_sb = sb(p1, "pastb_sb", [128, NO, 32], F32)
    ownm_sb = sb(p1, "ownm_sb", [128, NO, 32], F32)
    fw.dma('sp', lambda e: e.dma_start(out=pastb_sb[:], in_=pastb), writes=['pastb_sb'])
    fw.dma('sp', lambda e: e.dma_start(out=ownm_sb[:], in_=ownm), writes=['ownm_sb'])
    qf = sb(p1, "qf", [128, 8, 64], F32)
    qaA = [sb(p1, "qaA%d" % k, [128, 8, 96], BF16) for k in range(2)]
    qaB = [sb(p1, "qaB%d" % k, [128, 8, 65], BF16) for k in range(2)]
    qT = sb(p1, "qT", [64, 8, 128], BF16)
    gm = sb(p1, "gm", [128, 8, 32], F32)
    m8 = sb(p1, "m8", [128, 8, 8], F32)
    thr = sb(p1, "thr", [128, 8], F32)
    sel = sb(p1, "sel", [128, 8, 32], F32)
    stQA = [sb(p1, "stQA%d" % k, [96, 8, 512], BF16) for k in range(2)]
    stQB = [sb(p1, "stQB%d" % k, [65, 8, 512], BF16) for k in range(2)]

    def q_front(i):
        s = i % 2
        load_xg(i, xtf, xg, s)
        rs = rstd_all[:, i:i + 1]
        for bnk, (c0, c1) in enumerate([(0, 512), (512, 1024)]):
            for c in range(8):
                fw.op('pe', lambda e, c=c, bnk=bnk, c0=c0, c1=c1: e.matmul(
                    ps[bnk][:], lhsT=xg[s][:, c, :], rhs=wq_sb[:, c, c0:c1],
                    start=(c == 0), stop=(c == 7)),
                    reads=[('xg', s), 'wq'], writes=[P[bnk]])
        fw.op('act', lambda e: e.activation(out=qf[:].rearrange("p h d -> p (h d)"), in_=ps[0][:],
                                            func=AF.Copy, scale=rs),
              reads=[P[0], ('rstd', i)], writes=['qf'])
        rope(qf[:], i, rtmp, 'qf')
        fw.op('pool', lambda e: e.tensor_copy(out=qaA[s][:, :, 0:64], in_=qf[:]), reads=['qf'], writes=[('qaA', s)])
        fw.op('act', lambda e: e.activation(out=qaB[s][:, :, 0:64], in_=ps[1][:].rearrange("p (h d) -> p h d", h=8),
                                            func=AF.Copy, scale=rs),
              reads=[P[1], ('rstd', i)], writes=[('qaB', s)])
        fw.op('pool', lambda e: e.tensor_scalar(out=qaB[s][:, :, 64], in0=negc[:, i, :], scalar1=-8.0, scalar2=None,
                                                op0=ALU.mult),
              reads=['negc', ('qaB', s)], writes=[('qaB', s)])

    def q_back(i):
        s = i % 2
        g4 = (i // 4) % 2
        pt64 = ps[6][0:64, :].bitcast(BF16).rearrange("p (h t) -> p h t", h=8)
        for h in range(8):
            fw.op('pe', lambda e, h=h: e.transpose(out=pt64[:, h, :], in_=qaA[s][:, h, 0:64], identity=identb[:]),
                  reads=[('qaA', s), 'identb'], writes=[P[6]])
        fw.op('dve', lambda e: e.tensor_copy(out=qT[:], in_=pt64), reads=[P[6]], writes=['qT'])
        psg = ps[2][:, 0:256].rearrange("p (h n) -> p h n", h=8)
        for h in range(8):
            fw.op('pe', lambda e, h=h: e.matmul(psg[:, h, :], lhsT=qT[:, h, :], rhs=kmT[:, h, :],
                                                 start=True, stop=True),
                  reads=['qT', 'kmT'], writes=[P[2]])
        pt65 = ps[3][0:65, :].bitcast(BF16).rearrange("p (h t) -> p h t", h=8)
        for h in range(8):
            fw.op('pe', lambda e, h=h: e.transpose(out=pt65[:, h, :], in_=qaB[s][:, h, :], identity=identb[:]),
                  reads=[('qaB', s), 'identb'], writes=[P[3]])
        fw.op('dve', lambda e: e.tensor_tensor(out=gm[:], in0=psg,
                                               in1=pastb_sb[:, i, :].unsqueeze(1).to_broadcast([128, 8, 32]),
                                               op=ALU.add),
              reads=[P[2], 'pastb_sb'], writes=['gm'])
        for h in range(8):
            fw.op('dve', lambda e, h=h: e.max(out=m8[:, h, :], in_=gm[:, h, :]), reads=['gm'], writes=[('m8', h)])
        fw.op('dve', lambda e: e.tensor_scalar(out=thr[:], in0=m8[:, :, 2], scalar1=-1e29, scalar2=None,
                                               op0=ALU.max),
              reads=[('m8', h_) for h_ in range(8)], writes=['thr'])
        fw.op('dve', lambda e: e.tensor_tensor(out=sel[:], in0=gm[:],
                                               in1=thr[:].unsqueeze(2).to_broadcast([128, 8, 32]), op=ALU.is_ge),
              reads=['gm', 'thr'], writes=['sel'])
        fw.op('dve', lambda e: e.tensor_tensor(out=sel[:], in0=sel[:],
                                               in1=ownm_sb[:, i, :].unsqueeze(1).to_broadcast([128, 8, 32]),
                                               op=ALU.add),
              reads=['sel', 'ownm_sb'], writes=['sel'])
        fw.op('dve', lambda e: e.tensor_scalar(out=qaA[s][:, :, 64:96], in0=sel[:], scalar1=-1.0, scalar2=-NEG,
                                               op0=ALU.add, op1=ALU.mult),
              reads=['sel', ('qaA', s)], writes=[('qaA', s)])
        fw.op('act', lambda e: e.activation(out=stQB[g4][:, :, (i % 4) * 128:(i % 4 + 1) * 128], in_=pt65, func=AF.Copy),
              reads=[P[3]], writes=[('stQB', g4)])
        pt96 = ps[7][0:96, :].bitcast(BF16).rearrange("p (h t) -> p h t", h=8)
        for h in range(8):
            fw.op('pe', lambda e, h=h: e.transpose(out=pt96[:, h, :], in_=qaA[s][:, h, :], identity=identb[:]),
                  reads=[('qaA', s), 'identb'], writes=[P[7]])
        fw.op('act', lambda e: e.activation(out=stQA[g4][:, :, (i % 4) * 128:(i % 4 + 1) * 128], in_=pt96, func=AF.Copy),
              reads=[P[7]], writes=[('stQA', g4)])
        if i % 4 == 3:
            g = i // 4
            fw.dma('sp', lambda e, g=g: e.dma_start(
                out=QTA[:, :, g * 512:(g + 1) * 512].rearrange("h d s -> d h s"), in_=stQA[g4][:]),
                reads=[('stQA', g4)], writes=['QTA'])
            fw.dma('sp', lambda e, g=g: e.dma_start(
                out=QTB[:, :, g * 512:(g + 1) * 512].rearrange("h d s -> d h s"), in_=stQB[g4][:]),
                reads=[('stQB', g4)], writes=['QTB'])

    q_front(0)
    for i in range(NO):
        if i + 1 < NO:
            q_front(i + 1)
        q_back(i)
    fw.barrier()
    p1.close()
    if STOP_AFTER <= 1:
        return finish(nc, fw, y, glob)

    p2 = ExitStack()
    KT = [sb(p2, "KT%d" % k, [128, 8192], BF16) for k in range(2)]
    VV = [sb(p2, "VV%d" % k, [128, NT, 128], BF16) for k in range(2)]
    QT = [sb(p2, "QT%d" % k, [128, 2048], BF16) for k in range(2)]
    for k in range(2):
        fw.op('pool', lambda e, k=k: e.memset(VV[k][:], 0.0), writes=[('VV', k)])
    NPT = 3
    gslot = [0]
    PT = [sb(p2, "PT%d" % k, [128, 1024], BF16) for k in range(NPT)]
    trim_sb = sb(p2, "trim_sb", [128, 4, 512], BF16)
    acc = [[sb(p2, "acc%d_%d" % (k, t), [65, 1024], F32) for t in range(2)] for k in range(2)]
    QU = [sb(p2, "QU%d" % k, [128, 1024], BF16) for k in range(2)]
    uw_sb = sb(p2, "uw_sb", [128, 6, 2], F32)
    fw.dma('sp', lambda e: e.dma_start(out=uw_sb[:], in_=uwd), writes=['uw'])
    rrow = [sb(p2, "rrow%d" % k, [65, 512], BF16) for k in range(4)]
    njob = 0
    ones_bb = sb(p2, "ones_bb", [65, 64], BF16)
    fw.op('pool', lambda e: e.memset(ones_bb[:], 1.0), writes=['ones_bb'])
    pending = []
    attst = [sb(p2, "attst%d" % k, [64, 512], BF16) for k in range(4)]
    fw.dma('pool', lambda e: e.dma_start(out=trim_sb[:], in_=trim), writes=['trim_sb'])
    trimb_sb = sb(p2, "trimb_sb", [128, 4, 512], F32)
    stmp = [sb(p2, "stmp%d" % k, [128, 512], F32) for k in range(2)]
    fw.dma('sp', lambda e: e.dma_start(out=trimb_sb[:], in_=trim), writes=['trimb_sb'])
    fw.op('dve', lambda e: e.tensor_scalar(out=trimb_sb[:], in0=trimb_sb[:], scalar1=-1.0, scalar2=1e6,
                                           op0=ALU.add, op1=ALU.mult), reads=['trimb_sb'], writes=['trimb_sb'])
    ndiag = 0

    scale = 0.125
    npair = 0
    nq = 0
    for br in range(2):
        for h in range(8):
            hs = (br * 8 + h) % 2
            Kd, Vd, Qd = (KTA, VA, QTA) if br == 0 else (KTB, VB, QTB)
            Rq = 96 if br == 0 else 65
            Rr = 128
            fw.op('pool', lambda e: e.memset(KT[hs][64:128, :], 0.0), writes=[('KT', hs)])
            fw.op('pool', lambda e: e.memset(QT[hs][64:128, :], 0.0), writes=[('QT', hs)])
            fw.dma('sp', lambda e, h=h, Kd=Kd: e.dma_start(out=KT[hs][0:64, :], in_=Kd[h]),
                   reads=['KTA', 'KTB'], writes=[('KT', hs)])
            if br == 0:
                fw.dma('pool', lambda e: e.dma_start(out=KT[hs][64:96, :], in_=onehot),
                       writes=[('KT', hs)])
            else:
                fw.op('pool', lambda e: e.memset(KT[hs][64:65, :], 1.0), writes=[('KT', hs)])
            fw.dma('sp', lambda e, h=h, Vd=Vd: e.dma_start(out=VV[hs][:, :, 0:65], in_=Vd[h]),
                   reads=['VA', 'VB'], writes=[('VV', hs)])
            fw.dma('sp', lambda e, h=h, Qd=Qd: e.dma_start(out=QT[hs][0:Rq, :], in_=Qd[h]),
                   reads=['QTA', 'QTB'], writes=[('QT', hs)])
            hp = (br * 8 + h) % 2
            jobs = []
            for qt in range(4):
                u = qt % 2
                if qt < 2:
                    tiles = [(t, None) for t in range(0, 4 * u)] + [(4 * u + o, o) for o in range(4)]
                else:
                    tiles = [(t, None) for t in range(0, 8)] + [(8 + t, None) for t in range(0, 4 * u)] \
                        + [(8 + 4 * u + o, o) for o in range(4)]
                jobs.append(('own', QT[hs][:, qt * 512:(qt + 1) * 512], ('QT', hs), tiles, qt // 2, qt % 2, None))
            for un in range(6):
                tiles = [(16 + 8 * un + t, None) for t in range(8)]
                jobs.append(('unit', QU[un % 2], ('QU', un % 2), tiles, None, None, un))
            items = []
            for jn, job in enumerate(jobs):
                for n, (kt, o) in enumerate(job[3]):
                    items.append((jn, kt, o, n == 0, n == len(job[3]) - 1))

            def blend(nxt):
                ub = nxt % 2
                fw.op('dve', lambda e: e.tensor_scalar(out=QU[ub][:], in0=QT[hs][:, 0:1024], scalar1=uw_sb[:, nxt, 0:1],
                                                       scalar2=None, op0=ALU.mult),
                      reads=[('QT', hs), 'uw'], writes=[('QU', ub)])
                fw.op('dve', lambda e: e.scalar_tensor_tensor(out=QU[ub][:], in0=QT[hs][:, 1024:2048],
                                                              scalar=uw_sb[:, nxt, 1:2], in1=QU[ub][:],
                                                              op0=ALU.mult, op1=ALU.add),
                      reads=[('QT', hs), 'uw', ('QU', ub)], writes=[('QU', ub)])

            def emit_S(it):
                jn, kt, o, first, last = it
                kind, qap, qkey, _, tgt, half, un = jobs[jn]
                b0 = 2 * (gslot[0] % 2)
                pk = gslot[0] % NPT
                gslot[0] += 1
                if first and kind == 'own' and jn == 3:
                    blend(0)
                elif first and kind == 'unit' and un + 1 < 6:
                    blend(un + 1)
                W = 512 if kind == 'own' else 1024
                for hh_ in range(W // 512):
                    rq = qap if kind == 'own' else qap[:, hh_ * 512:(hh_ + 1) * 512]
                    fw.op('pe', lambda e, rq=rq, hh_=hh_: e.matmul(
                        ps[b0 + hh_][:], lhsT=KT[hs][:, kt * 128:(kt + 1) * 128], rhs=rq, start=True, stop=True),
                        reads=[('KT', hs), qkey], writes=[P[b0 + hh_]])
                src = psall[:, b0 * 512:b0 * 512 + W]
                rk = [P[b0]] if W == 512 else [P[b0], P[b0 + 1]]
                dst = PT[pk][:, 0:W]
                if br == 0:
                    fw.op('act', lambda e: e.activation(out=dst, in_=src, func=AF.Exp, scale=scale),
                          reads=rk, writes=[('PT', pk)])
                    if o is not None:
                        fw.op('dve', lambda e: e.tensor_tensor(
                            out=dst, in0=dst, in1=trim_sb[:, o, :], op=ALU.mult),
                            reads=[('PT', pk), 'trim_sb'], writes=[('PT', pk)])
                elif o is not None:
                    dk = gslot[0] % 2
                    fw.op('dve', lambda e: e.tensor_tensor(
                        out=stmp[dk][:], in0=src, in1=trimb_sb[:, o, :], op=ALU.add),
                        reads=rk + ['trimb_sb'], writes=[('stmp', dk)])
                    fw.op('act', lambda e: e.activation(
                        out=dst, in_=stmp[dk][:], func=AF.Exp, bias=negcA[:, kt, h:h + 1], scale=scale),
                        reads=[('stmp', dk), 'negcA'], writes=[('PT', pk)])
                else:
                    fw.op('act', lambda e: e.activation(
                        out=dst, in_=src, func=AF.Exp, bias=negcA[:, kt, h:h + 1], scale=scale),
                        reads=rk + ['negcA'], writes=[('PT', pk)])
                return pk

            def emit_PV(it, pk):
                jn, kt, o, first, last = it
                kind, qap, qkey, _, tgt, half, un = jobs[jn]
                if kind == 'own':
                    ob = 4 + jn
                    hsl = slice(half * 512, (half + 1) * 512)
                    fw.op('pe', lambda e: e.matmul(
                        ps[ob][:], lhsT=VV[hs][:, kt, :], rhs=PT[pk][:, 0:512], start=first, stop=last),
                        reads=[('VV', hs), ('PT', pk)], writes=[P[ob]])
                    if last:
                        fw.op('dve', lambda e: e.tensor_copy(out=acc[hp][tgt][:, hsl], in_=ps[ob][0:65, :]),
                              reads=[P[ob]], writes=[('acc', hp, tgt, half)])
                else:
                    obp = 4 + 2 * (un % 2)
                    for hh_ in range(2):
                        fw.op('pe', lambda e, hh_=hh_: e.matmul(
                            ps[obp + hh_][:], lhsT=VV[hs][:, kt, :], rhs=PT[pk][:, hh_ * 512:(hh_ + 1) * 512],
                            start=first, stop=last),
                            reads=[('VV', hs), ('PT', pk)], writes=[P[obp + hh_]])
                    if last:
                        for hh_ in range(2):
                            hsl = slice(hh_ * 512, (hh_ + 1) * 512)
                            for tg2 in range(2):
                                fw.op('dve', lambda e, tg2=tg2, hh_=hh_, hsl=hsl: e.scalar_tensor_tensor(
                                    out=acc[hp][tg2][:, hsl], in0=ps[obp + hh_][0:65, :], scalar=uw_sb[0:65, un, tg2:tg2 + 1],
                                    in1=acc[hp][tg2][:, hsl], op0=ALU.mult, op1=ALU.add),
                                    reads=[P[obp + hh_], 'uw', ('acc', hp, tg2, hh_)], writes=[('acc', hp, tg2, hh_)])

            nit = len(items)
            pks = {}
            for idx in range(nit + 1):
                if idx < nit:
                    pks[idx] = emit_S(items[idx])
                if idx - 1 >= 0:
                    emit_PV(items[idx - 1], pks.pop(idx - 1))
                for pd in list(pending):
                    pd[0] -= 1
                    if pd[0] <= 0:
                        pending.remove(pd)
                        pd[1]()
            for qt in range(4):
                tgt, half = qt // 2, qt % 2
                hsl = slice(half * 512, (half + 1) * 512)
                with nc.allow_low_precision("softmax normaliser 1/l is broadcast through a bf16 K=1 matmul"):
                    fw.op('dve', lambda e, qt=qt, tgt=tgt, hsl=hsl: e.reciprocal(out=rrow[qt][64:65, :], in_=acc[hp][tgt][64:65, hsl]),
                          reads=[('acc', hp, tgt, half)], writes=[('rrow', qt)])

                def fin(qt=qt, tgt=tgt, half=half, hsl=hsl, hp=hp, br=br, h=h):
                    bb = 2 * (gslot[0] % 2)
                    gslot[0] += 1
                    fw.op('pe', lambda e: e.matmul(ps[bb][0:64, :], lhsT=ones_bb[64:65, 0:64], rhs=rrow[qt][64:65, :],
                                                   start=True, stop=True),
                          reads=[('rrow', qt), 'ones_bb'], writes=[P[bb]])
                    fw.op('dve', lambda e: e.tensor_tensor(out=attst[qt][:], in0=acc[hp][tgt][0:64, hsl], in1=ps[bb][0:64, :],
                                                           op=ALU.mult),
                          reads=[('acc', hp, tgt, half), P[bb]], writes=[('attst', qt)])
                    fw.dma('sp', lambda e: e.dma_start(
                        out=ATT[br, h, :, qt * 512:(qt + 1) * 512], in_=attst[qt][:]),
                        reads=[('attst', qt)], writes=['ATT'])
                pending.append([6 + 3 * qt, fin])
    for pd in pending:
        pd[1]()
    fw.barrier()
    p2.close()
    if STOP_AFTER <= 2:
        return finish(nc, fw, y, glob)

    p3 = ExitStack()
    wg_sb = sb(p3, "wg_sb", [128, 8, 2048], BF16)
    wba_sb = sb(p3, "wba_sb", [64, 8, D], BF16)
    wbb_sb = sb(p3, "wbb_sb", [64, 8, D], BF16)
    wout_sb = sb(p3, "wout_sb", [128, 8, D], BF16)
    for k in range(8):
        fw.dma('pool', lambda e, k=k: e.dma_start(out=wg_sb[:, k, :], in_=w_g[:, k, :]), writes=['wg'])
        fw.dma('pool', lambda e, k=k: e.dma_start(out=wout_sb[:, k, :], in_=wout[:, k, :]), writes=['wout'])
        fw.dma('pool', lambda e, k=k: e.dma_start(out=wba_sb[:, k, :], in_=wba[:, k, :]), writes=['wba'])
        fw.dma('pool', lambda e, k=k: e.dma_start(out=wbb_sb[:, k, :], in_=wbb[:, k, :]), writes=['wbb'])
    xtf3 = [sb(p3, "xtf3_%d" % k, [128, 8, 128], F32) for k in range(2)]
    xg3 = [sb(p3, "xg3_%d" % k, [128, 8, 128], BF16) for k in range(2)]
    att = [sb(p3, "att%d" % k, [64, 16, 128], BF16) for k in range(2)]
    xo = [sb(p3, "xo%d" % k, [128, D], F32) for k in range(2)]
    sgA = sb(p3, "sgA", [128, D], F32)
    sgB = sb(p3, "sgB", [128, D], F32)
    mg = sb(p3, "mg", [128, D], F32)
    mgb = sb(p3, "mgb", [128, D], BF16)
    mT = sb(p3, "mT", [128, 8, 128], BF16)
    hh3 = [sb(p3, "hh3_%d" % k, [128, D], F32) for k in range(2)]
    for i in range(NO):
        s = i % 2
        fw.dma('sp', lambda e: e.dma_start(out=xtf3[s][:], in_=xT[i]), writes=[('xtf3', s)])
        fw.dma('sp', lambda e: e.dma_start(
            out=att[s][:], in_=ATT[:, :, :, i * 128:(i + 1) * 128].rearrange("b h d t -> d (b h) t")),
            reads=['ATT'], writes=[('att', s)])
        fw.dma('sp', lambda e: e.dma_start(out=xo[s][:], in_=xown[i]), writes=[('xo', s)])
        fw.op('dve', lambda e: e.tensor_tensor(
            out=xg3[s][:], in0=xtf3[s][:],
            in1=gmix_sb[:].unsqueeze(2).to_broadcast([128, 8, 128]), op=ALU.mult),
            reads=[('xtf3', s), 'gmix'], writes=[('xg3', s)])
        rs = rstd_all[:, i:i + 1]
        for bnk in range(4):
            for c in range(8):
                fw.op('pe', lambda e, c=c, bnk=bnk: e.matmul(
                    ps[bnk][:], lhsT=xg3[s][:, c, :], rhs=wg_sb[:, c, bnk * 512:(bnk + 1) * 512],
                    start=(c == 0), stop=(c == 7)),
                    reads=[('xg3', s), 'wg'], writes=[P[bnk]])
        for bnk in range(4):
            dst = (sgA if bnk < 2 else sgB)[:, (bnk % 2) * 512:(bnk % 2 + 1) * 512]
            fw.op('act', lambda e, bnk=bnk, dst=dst: e.activation(out=dst, in_=ps[bnk][:], func=AF.Sigmoid, scale=rs),
                  reads=[P[bnk]], writes=[('sg', bnk)])
        for br in range(2):
            wsb = wba_sb if br == 0 else wbb_sb
            for half in range(2):
                bnk = 4 + br * 2 + half
                for h in range(8):
                    fw.op('pe', lambda e, h=h, bnk=bnk, br=br, half=half, wsb=wsb: e.matmul(
                        ps[bnk][:], lhsT=att[s][:, br * 8 + h, :], rhs=wsb[:, h, half * 512:(half + 1) * 512],
                        start=(h == 0), stop=(h == 7)),
                        reads=[('att', s), 'wba', 'wbb'], writes=[P[bnk]])
        for half in range(2):
            sl = slice(half * 512, (half + 1) * 512)
            fw.op('dve', lambda e, half=half, sl=sl: e.tensor_tensor(out=mg[:, sl], in0=sgA[:, sl], in1=ps[4 + half][:], op=ALU.mult),
                  reads=[('sg', half), P[4 + half]], writes=[('mg', half)])
            fw.op('dve', lambda e, half=half, sl=sl: e.tensor_tensor(out=sgB[:, sl], in0=sgB[:, sl], in1=ps[6 + half][:], op=ALU.mult),
                  reads=[('sg', 2 + half), P[6 + half]], writes=[('sg', 2 + half)])
            fw.op('dve', lambda e, half=half, sl=sl: e.tensor_tensor(out=mgb[:, sl], in0=mg[:, sl], in1=sgB[:, sl], op=ALU.add),
                  reads=[('mg', half), ('sg', 2 + half)], writes=[('mgb', half)])
        ptm = ps[0][:].bitcast(BF16).rearrange("p (c t) -> p c t", c=8)
        for c in range(8):
            fw.op('pe', lambda e, c=c: e.transpose(out=ptm[:, c, :], in_=mgb[:, c * 128:(c + 1) * 128], identity=identb[:]),
                  reads=[('mgb', c // 4), 'identb'], writes=[P[0]])
        fw.op('act', lambda e: e.activation(out=mT[:], in_=ptm, func=AF.Copy), reads=[P[0]], writes=['mT'])
        for half in range(2):
            for c in range(8):
                fw.op('pe', lambda e, c=c, half=half: e.matmul(
                    ps[1 + half][:], lhsT=mT[:, c, :], rhs=wout_sb[:, c, half * 512:(half + 1) * 512],
                    start=(c == 0), stop=(c == 7)),
                    reads=['mT', 'wout'], writes=[P[1 + half]])
        for half in range(2):
            sl = slice(half * 512, (half + 1) * 512)
            fw.op('dve', lambda e, half=half, sl=sl: e.tensor_tensor(out=hh3[s][:, sl], in0=xo[s][:, sl], in1=ps[1 + half][:], op=ALU.add),
                  reads=[('xo', s), P[1 + half]], writes=[('hh3', s)])
        fw.dma('sp', lambda e, i=i: e.dma_start(out=HS[i], in_=hh3[s][:]), reads=[('hh3', s)], writes=['HS'])
    fw.barrier()
    p3.close()
    if STOP_AFTER <= 3:
        return finish(nc, fw, y, glob)

    p4 = ExitStack()
    xn2T_all = sb(p4, "xn2T_all", [128, 8, 2048], BF16)
    eps2 = sb(p4, "eps2", [128, 1], F32)
    p4b = ExitStack()
    selT_all = sb(p4b, "selT_all", [128, NO, 3, 128], BF16)
    p41 = ExitStack()
    wpq_sb = sb(p41, "wpq_sb", [128, 8, 2048], BF16)
    skT_sb = sb(p41, "skT_sb", [128, 16, 128], BF16)
    gffn_sb = sb(p41, "gffn_sb", [128, D], F32)
    iota_sb = sb(p41, "iota_sb", [128, 16], F32)
    lo_sb = sb(p41, "lo_sb", [128, 16], F32)
    hi_sb = sb(p41, "hi_sb", [128, 16], F32)
    for k in range(8):
        fw.dma('pool', lambda e, k=k: e.dma_start(out=wpq_sb[:, k, :], in_=wpq[:, k, :]), writes=['wpq'])
    fw.dma('pool', lambda e: e.dma_start(out=skT_sb[:], in_=skT), writes=['skT'])
    fw.dma('sp', lambda e: e.dma_start(out=gffn_sb[:], in_=g_ffn), writes=['gffn'])
    fw.dma('sp', lambda e: e.dma_start(out=iota_sb[:], in_=iota16), writes=['iota'])
    fw.op('dve', lambda e: e.tensor_scalar(out=lo_sb[:], in0=iota_sb[:], scalar1=16.0, scalar2=None, op0=ALU.mult),
          reads=['iota'], writes=['lo'])
    fw.op('dve', lambda e: e.tensor_scalar(out=hi_sb[:], in0=iota_sb[:], scalar1=16.0, scalar2=16.0, op0=ALU.mult, op1=ALU.add),
          reads=['iota'], writes=['hi'])
    hh = [sb(p41, "hh%d" % k, [128, D], F32) for k in range(2)]
    sel3 = sb(p41, "sel3", [128, 3, 128], F32)
    junk = sb(p41, "junk", [128, D], F32)
    ss2 = sb(p41, "ss2", [128, 1], F32)
    rs2 = sb(p41, "rs2", [128, 1], F32)
    xn2b = sb(p41, "xn2b", [128, D], BF16)
    qpb = sb(p41, "qpb", [128, 2048], BF16)
    qpT = sb(p41, "qpT", [128, 16, 128], BF16)
    scw = sb(p41, "scw", [128, 16, 128], F32)
    sv = sb(p41, "sv", [128, 16, 16], F32)
    si = sb(p41, "si", [128, 16, 16], U32)
    sif = sb(p41, "sif", [128, 16, 16], F32)
    cand = sb(p41, "cand", [128, 8, 256], F32)
    candw = sb(p41, "candw", [128, 8, 256], F32)
    best = sb(p41, "best", [128, 8, 16], F32)
    pick = sb(p41, "pick", [128, 8, 16], U32)
    pf = sb(p41, "pf", [128, 8, 16], F32)
    af = sb(p41, "af", [128, 8, 16], F32)
    bfl = sb(p41, "bfl", [128, 8, 16], F32)
    eq = sb(p41, "eq", [128, 8, 16, 16], F32)
    eq2 = sb(p41, "eq2", [128, 8, 16, 16], F32)
    gex = sb(p41, "gex", [128, 8, 16], F32)
    gsum = sb(p41, "gsum", [128, 8], F32)
    B4 = [128, 8, 16, 16]

    def stage_F(i):
        s = i % 2
        B0 = 4 * (i % 2)
        fw.dma('sp', lambda e, i=i: e.dma_start(out=hh[s][:], in_=HS[i]), reads=['HS'], writes=[('hh', s)])
        fw.op('act', lambda e: e.activation(out=junk[:], in_=hh[s][:], func=AF.Square, accum_out=ss2[:]),
              reads=[('hh', s)], writes=['junk', 'ss2'])
        fw.op('act', lambda e: e.activation(out=rs2[:], in_=ss2[:], func=AF.Sqrt, bias=eps_sb[:], scale=1.0 / D),
              reads=['ss2', 'eps_sb'], writes=['rs2'])
        fw.op('dve', lambda e: e.reciprocal(out=rs2[:], in_=rs2[:]), reads=['rs2'], writes=['rs2'])
        fw.op('dve', lambda e: e.scalar_tensor_tensor(out=xn2b[:], in0=hh[s][:], scalar=rs2[:, 0:1], in1=gffn_sb[:],
                                                      op0=ALU.mult, op1=ALU.mult),
              reads=[('hh', s), 'rs2', 'gffn'], writes=['xn2b'])
        ptx = ps[B0][:].bitcast(BF16).rearrange("p (c t) -> p c t", c=8)
        for c in range(8):
            fw.op('pe', lambda e, c=c: e.transpose(out=ptx[:, c, :], in_=xn2b[:, c * 128:(c + 1) * 128], identity=identb[:]),
                  reads=['xn2b', 'identb'], writes=[P[B0]])
        xT_i = xn2T_all[:, :, i * 128:(i + 1) * 128]
        fw.op('act', lambda e: e.activation(out=xT_i, in_=ptx, func=AF.Copy), reads=[P[B0]], writes=[('xn2T', i)])
        QB = [B0 + 1, B0 + 2, B0 + 3, B0]
        for bnk in range(4):
            for c in range(8):
                fw.op('pe', lambda e, c=c, bnk=bnk: e.matmul(
                    ps[QB[bnk]][:], lhsT=xn2T_all[:, c, i * 128:(i + 1) * 128], rhs=wpq_sb[:, c, bnk * 512:(bnk + 1) * 512],
                    start=(c == 0), stop=(c == 7)),
                    reads=[('xn2T', i), 'wpq'], writes=[P[QB[bnk]]])
            fw.op('act', lambda e, bnk=bnk: e.activation(out=qpb[:, bnk * 512:(bnk + 1) * 512], in_=ps[QB[bnk]][:], func=AF.Copy),
                  reads=[P[QB[bnk]]], writes=[('qpb', bnk)])
        for half in range(2):
            ptq = ps[B0 + 1 + half][:].bitcast(BF16).rearrange("p (c t) -> p c t", c=8)
            for c in range(8):
                gidx = half * 8 + c
                fw.op('pe', lambda e, c=c, gidx=gidx, ptq=ptq: e.transpose(
                    out=ptq[:, c, :], in_=qpb[:, gidx * 128:(gidx + 1) * 128], identity=identb[:]),
                    reads=[('qpb', gidx // 4), 'identb'], writes=[P[B0 + 1 + half]])
            fw.op('act', lambda e, ptq=ptq, half=half: e.activation(out=qpT[:, half * 8:(half + 1) * 8, :], in_=ptq, func=AF.Copy),
                  reads=[P[B0 + 1 + half]], writes=[('qpT', half)])
        for gidx in range(16):
            bnk = B0 + gidx // 4
            fw.op('pe', lambda e, gidx=gidx, bnk=bnk: e.matmul(
                ps[bnk][:, (gidx % 4) * 128:(gidx % 4 + 1) * 128], lhsT=qpT[:, gidx, :], rhs=skT_sb[:, gidx, :],
                start=True, stop=True),
                reads=[('qpT', gidx // 8), 'skT'], writes=[P[bnk]])
    def stage_T(i):
        s = i % 2
        B0 = 4 * (i % 2)
        def srcg(gidx):
            return ps[B0 + gidx // 4][:, (gidx % 4) * 128:(gidx % 4 + 1) * 128]
        for gidx in range(16):
            fw.op('dve', lambda e, gidx=gidx: e.max(out=sv[:, gidx, 0:8], in_=srcg(gidx)),
                  reads=[P[B0 + gidx // 4]], writes=[('sv', gidx, 0)])
        for gidx in range(16):
            fw.op('dve', lambda e, gidx=gidx: e.max_index(out=si[:, gidx, 0:8], in_max=sv[:, gidx, 0:8], in_values=srcg(gidx)),
                  reads=[P[B0 + gidx // 4], ('sv', gidx, 0)], writes=[('si', gidx, 0)])
        for gidx in range(16):
            fw.op('dve', lambda e, gidx=gidx: e.match_replace(out=scw[:, gidx, :], in_to_replace=sv[:, gidx, 0:8],
                                                              in_values=srcg(gidx), imm_value=-1e30),
                  reads=[P[B0 + gidx // 4], ('sv', gidx, 0)], writes=[('scw', gidx)])
        for gidx in range(16):
            fw.op('dve', lambda e, gidx=gidx: e.max(out=sv[:, gidx, 8:16], in_=scw[:, gidx, :]),
                  reads=[('scw', gidx)], writes=[('sv', gidx, 1)])
        for gidx in range(16):
            fw.op('dve', lambda e, gidx=gidx: e.max_index(out=si[:, gidx, 8:16], in_max=sv[:, gidx, 8:16], in_values=scw[:, gidx, :]),
                  reads=[('scw', gidx), ('sv', gidx, 1)], writes=[('si', gidx, 1)])
        fw.op('dve', lambda e: e.tensor_copy(out=sif[:], in_=si[:]),
              reads=[('si', g_, k_) for g_ in range(16) for k_ in range(2)], writes=['sif'])
        sv4 = sv[:].rearrange("p (h two) k -> p h two k", two=2)
        sif4 = sif[:].rearrange("p (h two) k -> p h two k", two=2)
        cand4 = cand[:].rearrange("p h (a b) -> p h a b", a=16)
        fw.op('dve', lambda e: e.tensor_tensor(out=cand4, in0=sv4[:, :, 0, :].unsqueeze(3).to_broadcast(B4),
                                               in1=sv4[:, :, 1, :].unsqueeze(2).to_broadcast(B4), op=ALU.add),
              reads=[('sv', g_, k_) for g_ in range(16) for k_ in range(2)], writes=['cand'])
        for h in []:
            fw.op('dve', lambda e, h=h: e.max(out=best[:, h, 0:8], in_=cand[:, h, :]), reads=['cand'], writes=['best'])
            fw.op('dve', lambda e, h=h: e.max_index(out=pick[:, h, 0:8], in_max=best[:, h, 0:8], in_values=cand[:, h, :]),
                  reads=['cand', 'best'], writes=['pick'])
            fw.op('dve', lambda e, h=h: e.match_replace(out=candw[:, h, :], in_to_replace=best[:, h, 0:8],
                                                        in_values=cand[:, h, :], imm_value=-1e30),
                  reads=['cand', 'best'], writes=['candw'])
            fw.op('dve', lambda e, h=h: e.max(out=best[:, h, 8:16], in_=candw[:, h, :]), reads=['candw'], writes=['best'])
            fw.op('dve', lambda e, h=h: e.max_index(out=pick[:, h, 8:16], in_max=best[:, h, 8:16], in_values=candw[:, h, :]),
                  reads=['candw', 'best'], writes=['pick'])
        for h in range(8):
            fw.op('dve', lambda e, h=h: e.max(out=best[:, h, 0:8], in_=cand[:, h, :]), reads=['cand'], writes=[('best', h, 0)])
        for h in range(8):
            fw.op('dve', lambda e, h=h: e.max_index(out=pick[:, h, 0:8], in_max=best[:, h, 0:8], in_values=cand[:, h, :]),
                  reads=['cand', ('best', h, 0)], writes=[('pick', h, 0)])
        for h in range(8):
            fw.op('dve', lambda e, h=h: e.match_replace(out=candw[:, h, :], in_to_replace=best[:, h, 0:8],
                                                        in_values=cand[:, h, :], imm_value=-1e30),
                  reads=['cand', ('best', h, 0)], writes=[('candw', h)])
        for h in range(8):
            fw.op('dve', lambda e, h=h: e.max(out=best[:, h, 8:16], in_=candw[:, h, :]), reads=[('candw', h)], writes=[('best', h, 1)])
        for h in range(8):
            fw.op('dve', lambda e, h=h: e.max_index(out=pick[:, h, 8:16], in_max=best[:, h, 8:16], in_values=candw[:, h, :]),
                  reads=[('candw', h), ('best', h, 1)], writes=[('pick', h, 1)])
        BESTK = [('best', h_, k_) for h_ in range(8) for k_ in range(2)]
        PICKK = [('pick', h_, k_) for h_ in range(8) for k_ in range(2)]
        fw.op('dve', lambda e: e.tensor_copy(out=pf[:], in_=pick[:]), reads=PICKK, writes=['pf'])
        pf4 = pf[:].unsqueeze(3).to_broadcast(B4)
        lo4 = lo_sb[:].unsqueeze(1).unsqueeze(1).to_broadcast(B4)
        hi4 = hi_sb[:].unsqueeze(1).unsqueeze(1).to_broadcast(B4)
        io4 = iota_sb[:].unsqueeze(1).unsqueeze(1).to_broadcast(B4)
        e1v = sel3[:, 0, :].rearrange("p (h k) -> p h k", h=8)
        e2v = sel3[:, 1, :].rearrange("p (h k) -> p h k", h=8)
        gtv = sel3[:, 2, :].rearrange("p (h k) -> p h k", h=8)
        fw.op('dve', lambda e: e.tensor_tensor(out=eq[:], in0=pf4, in1=lo4, op=ALU.is_ge), reads=['pf', 'lo'], writes=['eq'])
        fw.op('dve', lambda e: e.tensor_tensor(out=eq2[:], in0=pf4, in1=hi4, op=ALU.is_lt), reads=['pf', 'hi'], writes=['eq2'])
        fw.op('dve', lambda e: e.tensor_tensor(out=eq[:], in0=eq[:], in1=eq2[:], op=ALU.mult), reads=['eq', 'eq2'], writes=['eq'])
        fw.op('dve', lambda e: e.tensor_tensor(out=eq2[:], in0=eq[:], in1=io4, op=ALU.mult), reads=['eq', 'iota'], writes=['eq2'])
        fw.op('dve', lambda e: e.tensor_reduce(out=af[:], in_=eq2[:], axis=AX.X, op=ALU.add), reads=['eq2'], writes=['af'])
        fw.op('dve', lambda e: e.tensor_tensor(out=eq2[:], in0=eq[:], in1=sif4[:, :, 0, :].unsqueeze(2).to_broadcast(B4), op=ALU.mult),
              reads=['eq', 'sif'], writes=['eq2'])
        fw.op('dve', lambda e: e.tensor_reduce(out=e1v, in_=eq2[:], axis=AX.X, op=ALU.add), reads=['eq2'], writes=['sel3'])
        fw.op('dve', lambda e: e.scalar_tensor_tensor(out=bfl[:], in0=af[:], scalar=-16.0, in1=pf[:], op0=ALU.mult, op1=ALU.add),
              reads=['af', 'pf'], writes=['bfl'])
        fw.op('dve', lambda e: e.tensor_tensor(out=eq[:], in0=bfl[:].unsqueeze(3).to_broadcast(B4), in1=io4, op=ALU.is_equal),
              reads=['bfl', 'iota'], writes=['eq'])
        fw.op('dve', lambda e: e.tensor_tensor(out=eq2[:], in0=eq[:], in1=sif4[:, :, 1, :].unsqueeze(2).to_broadcast(B4), op=ALU.mult),
              reads=['eq', 'sif'], writes=['eq2'])
        fw.op('dve', lambda e: e.tensor_reduce(out=e2v, in_=eq2[:], axis=AX.X, op=ALU.add), reads=['eq2'], writes=['sel3'])
        fw.op('dve', lambda e: e.tensor_tensor(out=gex[:], in0=best[:], in1=best[:, :, 0:1].to_broadcast([128, 8, 16]),
                                               op=ALU.subtract),
              reads=BESTK, writes=['gex'])
        fw.op('act', lambda e: e.activation(out=gex[:], in_=gex[:], func=AF.Exp), reads=['gex'], writes=['gex'])
        fw.op('dve', lambda e: e.tensor_reduce(out=gsum[:], in_=gex[:], axis=AX.X, op=ALU.add), reads=['gex'], writes=['gsum'])
        fw.op('dve', lambda e: e.reciprocal(out=gsum[:], in_=gsum[:]), reads=['gsum'], writes=['gsum'])
        fw.op('dve', lambda e: e.tensor_tensor(out=gtv, in0=gex[:],
                                               in1=gsum[:].unsqueeze(2).to_broadcast([128, 8, 16]), op=ALU.mult),
              reads=['gex', 'gsum'], writes=['sel3'])
        pst = ps[B0][:, 0:384].rearrange("p (a t) -> p a t", a=3)
        for a in range(3):
            fw.op('pe', lambda e, a=a: e.transpose(out=pst[:, a, :], in_=sel3[:, a, :], identity=identf[:]),
                  reads=['sel3', 'identf'], writes=[P[B0]])
        fw.op('act', lambda e, i=i: e.activation(out=selT_all[:, i, :, :], in_=pst, func=AF.Copy),
              reads=[P[B0]], writes=[('selT', i)])
    stage_F(0)
    for i in range(NO):
        if i + 1 < NO:
            stage_F(i + 1)
        stage_T(i)
    fw.barrier()
    p41.close()
    if STOP_AFTER <= 4:
        p4b.close()
        p4.close()
        return finish(nc, fw, y, glob)

    p42 = ExitStack()
    iota128 = sb(p42, "iota128_sb", [128, 128], BF16)
    TB = 32
    Aoh = [sb(p42, "Aoh%d" % k, [128, TB, 128], BF16) for k in range(2)]
    Boh = [sb(p42, "Boh%d" % k, [128, TB, 128], BF16) for k in range(2)]
    Gs = sb(p42, "Gs", [128, 128, 256], BF16)
    fw.dma('pool', lambda e: e.dma_start(out=iota128[:], in_=iota128d), writes=['iota128'])
    nblk = 0
    nev = 0
    for i in range(NO):
        ch = i // 2
        for blk in range(128 // TB):
            ab = nblk % 2
            nblk += 1
            t0 = blk * TB
            io3 = iota128[:].unsqueeze(1).to_broadcast([128, TB, 128])
            for t in range(TB):
                fw.op('dve', lambda e, t=t: e.tensor_scalar(
                    out=Aoh[ab][:, t, :], in0=iota128[:], scalar1=selT_all[:, i, 0, t0 + t:t0 + t + 1],
                    scalar2=selT_all[:, i, 2, t0 + t:t0 + t + 1], op0=ALU.is_equal, op1=ALU.mult),
                    reads=['iota128'], writes=[('Aoh', ab, t)])
            fw.op('dve', lambda e: e.tensor_tensor(
                out=Boh[ab][:], in0=io3, in1=selT_all[:, i, 1, t0:t0 + TB].unsqueeze(2).to_broadcast([128, TB, 128]),
                op=ALU.is_equal), reads=['iota128'], writes=[('Boh', ab)])
            for q16 in range(TB // 16):
                grp = nev % 2
                nev += 1
                for tt in range(16):
                    t = q16 * 16 + tt
                    fw.op('pe', lambda e, t=t, tt=tt, grp=grp: e.matmul(
                        psall[:, grp * 2048 + tt * 128:grp * 2048 + (tt + 1) * 128],
                        lhsT=Aoh[ab][:, t, :], rhs=Boh[ab][:, t, :], start=True, stop=True),
                        reads=[('Aoh', ab, t), ('Boh', ab)], writes=[('psg', grp)])
                tg = (i % 2) * 128 + t0 + q16 * 16
                dst = Gs[:, :, tg:tg + 16]
                srcp = psall[:, grp * 2048:(grp + 1) * 2048].rearrange("p (t j) -> p j t", t=16)
                fw.op('act', lambda e, dst=dst, srcp=srcp: e.activation(out=dst, in_=srcp, func=AF.Copy),
                      reads=[('psg', grp)], writes=['Gs'])
        if i % 2 == 1:
            for jb in range(8):
                fw.dma('sp', lambda e, jb=jb, ch=ch: e.dma_start(
                    out=Gscr[jb * 16:(jb + 1) * 16, :, ch * 256:(ch + 1) * 256].rearrange("j i t -> i j t"),
                    in_=Gs[:, jb * 16:(jb + 1) * 16, :]),
                    reads=['Gs'], writes=['Gscr'])
    fw.barrier()
    p42.close()
    p4b.close()
    if STOP_AFTER <= 5:
        p4.close()
        return finish(nc, fw, y, glob)

    p43 = ExitStack()
    JG = 4
    NJG = 128 // JG
    acc = sb(p43, "acc", [128, NO, D], F32)
    gfin_sb = sb(p43, "gfin_sb", [128, D], F32)
    Uj = [sb(p43, "Uj%d" % k, [128, 8, 128], BF16) for k in range(2 * JG)]
    Vj = [sb(p43, "Vj%d" % k, [128, D], BF16) for k in range(2 * JG)]
    Gj = [sb(p43, "Gj%d" % k, [128, 2048], BF16) for k in range(2)]
    gl = [sb(p43, "gl%d" % k, [128, 512], BF16) for k in range(2)]
    AT = [sb(p43, "AT%d" % k, [128, 2048], BF16) for k in range(2 * JG)]
    junk3 = sb(p43, "junk3", [128, D], F32)
    ss3 = sb(p43, "ss3", [128, 1], F32)
    rs3 = sb(p43, "rs3", [128, 1], F32)
    yo = [sb(p43, "yo%d" % k, [128, D], F32) for k in range(2)]
    fw.dma('sp', lambda e: e.dma_start(out=gfin_sb[:], in_=g_fin), writes=['gfin'])
    for i in range(NO):
        fw.dma('sp', lambda e, i=i: e.dma_start(out=acc[:, i, :], in_=HS[i]), reads=['HS'], writes=[('acc', i)])

    cnt = {'h': 0, 'g': 0, 'o': 0}

    def H_units(jg):
        units = []
        for jj in range(JG):
            j = jg * JG + jj
            slot = (jg % 2) * JG + jj

            def load(j=j, slot=slot):
                fw.dma('pool', lambda e: e.dma_start(out=Uj[slot][:], in_=Ur[j]), writes=[('Uj', slot)])
                fw.dma('pool', lambda e: e.dma_start(out=Vj[slot][:], in_=Vr[j]), writes=[('Vj', slot)])
            for tg in range(4):
                def unit(j=j, slot=slot, tg=tg, load=load):
                    if tg == 0:
                        load()
                        gsl = cnt['g'] % 2
                        cnt['g'] += 1
                        fw.dma('sp', lambda e: e.dma_start(out=Gj[gsl][:], in_=Gscr[j]), reads=['Gscr'], writes=[('Gj', gsl)])
                        unit_state['gsl'] = gsl
                    gsl = unit_state['gsl']
                    hb = cnt['h'] % 2
                    cnt['h'] += 1
                    for c in range(8):
                        fw.op('pe', lambda e, c=c: e.matmul(
                            ps[hb][:], lhsT=Uj[slot][:, c, :], rhs=xn2T_all[:, c, tg * 512:(tg + 1) * 512],
                            start=(c == 0), stop=(c == 7)),
                            reads=[('Uj', slot), 'xn2T_all'], writes=[P[hb]])
                    fw.op('act', lambda e: e.activation(out=gl[hb][:], in_=ps[hb][:], func=AF.Gelu),
                          reads=[P[hb]], writes=[('gl', hb)])
                    fw.op('dve', lambda e: e.tensor_tensor(
                        out=AT[slot][:, tg * 512:(tg + 1) * 512], in0=gl[hb][:], in1=Gj[gsl][:, tg * 512:(tg + 1) * 512],
                        op=ALU.mult),
                        reads=[('gl', hb), ('Gj', gsl)], writes=[('AT', slot, tg)])
                units.append(unit)
        return units

    unit_state = {}

    def V_units(jg):
        units = []
        for tile in range(NO):
            for half in range(2):
                def unit(tile=tile, half=half):
                    ob = 2 + (cnt['o'] % 6)
                    cnt['o'] += 1
                    for jj in range(JG):
                        slot = (jg % 2) * JG + jj
                        fw.op('pe', lambda e, jj=jj, slot=slot: e.matmul(
                            ps[ob][:], lhsT=AT[slot][:, tile * 128:(tile + 1) * 128],
                            rhs=Vj[slot][:, half * 512:(half + 1) * 512], start=(jj == 0), stop=(jj == JG - 1)),
                            reads=[('AT', slot, tile // 4), ('Vj', slot)], writes=[P[ob]])
                    dst = acc[:, tile, half * 512:(half + 1) * 512]
                    fw.op('dve', lambda e, dst=dst, ob=ob: e.tensor_tensor(out=dst, in0=dst, in1=ps[ob][:], op=ALU.add),
                          reads=[P[ob], ('acc', tile)], writes=[('acc', tile)])
                units.append(unit)
        return units

    for u in H_units(0):
        u()
    for jg in range(NJG):
        hu = H_units(jg + 1) if jg + 1 < NJG else []
        vu = V_units(jg)
        hi_ = 0
        for k, v in enumerate(vu):
            if k % 2 == 0 and hi_ < len(hu):
                hu[hi_]()
                hi_ += 1
            v()
        while hi_ < len(hu):
            hu[hi_]()
            hi_ += 1
    for i in range(NO):
        s = i % 2
        fw.op('act', lambda e, i=i: e.activation(out=junk3[:], in_=acc[:, i, :], func=AF.Square, accum_out=ss3[:]),
              reads=[('acc', i)], writes=['junk3', 'ss3'])
        fw.op('act', lambda e: e.activation(out=rs3[:], in_=ss3[:], func=AF.Sqrt, bias=eps_sb[:], scale=1.0 / D),
              reads=['ss3', 'eps_sb'], writes=['rs3'])
        fw.op('dve', lambda e: e.reciprocal(out=rs3[:], in_=rs3[:]), reads=['rs3'], writes=['rs3'])
        fw.op('dve', lambda e, i=i: e.scalar_tensor_tensor(out=yo[s][:], in0=acc[:, i, :], scalar=rs3[:, 0:1], in1=gfin_sb[:],
                                                           op0=ALU.mult, op1=ALU.mult),
              reads=[('acc', i), 'rs3', 'gfin'], writes=[('yo', s)])
        fw.dma('sp', lambda e, i=i: e.dma_start(out=y[i], in_=yo[s][:]), reads=[('yo', s)], writes=['y'])
    fw.barrier()
    p43.close()
    p4.close()
    return finish(nc, fw, y, glob)


def finish(nc, fw, y, glob):
    fw.barrier()
    glob.close()
    return nc


def prep(inputs):
    x = np.asarray(inputs["x"], np.float32)
    w_in = np.asarray(inputs["w_in"], np.float32)[0]

    def pc(w):
        return np.ascontiguousarray(w.reshape(8, 128, -1).transpose(1, 0, 2))

    cols_kv = np.r_[512:1024, 1024:1536, 2048:2560, 2560:3072, 3072:3080]
    cols_q = np.r_[0:512, 1536:2048]
    cols_g = np.r_[3080:5128]
    shared = {
        "w_kv": pc(w_in[:, cols_kv]),
        "w_q": pc(w_in[:, cols_q]),
        "w_g": pc(w_in[:, cols_g]),
        "g_mix": np.ascontiguousarray(np.asarray(inputs["norm_mix_g"], np.float32)[0].reshape(8, 128).T),
        "bfg": np.ascontiguousarray(np.broadcast_to(np.asarray(inputs["b_forget"], np.float32)[0][None, :], (128, 8))),
        "wba": np.ascontiguousarray(np.asarray(inputs["w_branch_a"], np.float32)[0].reshape(8, 64, D).transpose(1, 0, 2)),
        "wbb": np.ascontiguousarray(np.asarray(inputs["w_branch_b"], np.float32)[0].reshape(8, 64, D).transpose(1, 0, 2)),
        "wout": pc(np.asarray(inputs["w_out"], np.float32)[0]),
        "g_ffn": np.ascontiguousarray(np.broadcast_to(np.asarray(inputs["norm_ffn_g"], np.float32)[0][None, :], (128, D))),
        "wpq": pc(np.asarray(inputs["w_peer_q"], np.float32)[0]),
        "skT": np.ascontiguousarray(np.asarray(inputs["peer_sub_keys"], np.float32)[0].reshape(16, 128, 128).transpose(2, 0, 1)),
        "Ur": np.ascontiguousarray(np.asarray(inputs["peer_expert_u"], np.float32)[0].reshape(128, 128, 8, 128).transpose(1, 3, 2, 0)),
        "Vr": np.ascontiguousarray(np.asarray(inputs["peer_expert_v"], np.float32)[0].reshape(128, 128, D).transpose(1, 0, 2)),
        "iota128": np.ascontiguousarray(np.broadcast_to(np.arange(128, dtype=np.float32)[None, :], (128, 128))),
        "g_fin": np.ascontiguousarray(np.broadcast_to(np.asarray(inputs["norm_final_g"], np.float32)[None, :], (128, D))),
    }
    s_ = np.arange(128)[:, None]
    t_ = np.arange(512)[None, :]
    trim = np.stack([(o * 128 + s_ <= t_) for o in range(4)], axis=1).astype(np.float32)
    shared["trim"] = np.ascontiguousarray(trim)
    shared["tric"] = (np.arange(128)[:, None] <= np.arange(128)[None, :]).astype(np.float32)
    shared["onehot"] = (np.arange(32)[:, None] == (np.arange(8192)[None, :] // 256)).astype(np.float32)
    shared["iota16"] = np.ascontiguousarray(np.broadcast_to(np.arange(16, dtype=np.float32)[None, :], (128, 16)))
    half = 8
    inv_freq = np.power(np.float32(500000.0), -np.arange(half, dtype=np.float32) / np.float32(half)).astype(np.float32)
    maps = []
    for c in range(8):
        b = c // 4
        order = chunk_order(c)
        tok = np.concatenate([np.arange(k * 1024, (k + 1) * 1024) for k in order])
        xb = x[b][tok]
        m = dict(shared)
        m["xT"] = np.ascontiguousarray(xb.reshape(NT, 128, 8, 128).transpose(0, 3, 2, 1))
        m["xown"] = np.ascontiguousarray(xb[:2048].reshape(NO, 128, D))
        ang = tok.astype(np.float32)[:, None] * inv_freq[None, :]
        m["ropec"] = np.ascontiguousarray(np.cos(ang).astype(np.float32).reshape(NT, 128, 8).transpose(1, 0, 2))
        m["ropes"] = np.ascontiguousarray(np.sin(ang).astype(np.float32).reshape(NT, 128, 8).transpose(1, 0, 2))
        true_blk = np.array([order[n // 4] * 4 + n % 4 for n in range(32)])
        true_tile = np.array([order[n // 8] * 8 + n % 8 for n in range(NT)])
        pastb = np.zeros((NO, 32), np.float32)
        ownm = np.zeros((NO, 32), np.float32)
        units = core_units(c)
        slot_sel = np.array([-1, -1] + [q for (q, _) in units])
        for i in range(NO):
            qb = true_blk[i // 2]
            qsel = i // 8
            slot_of_blk = np.arange(32) // 4
            allowed = (slot_of_blk == 0) | ((slot_of_blk == 1) & (qsel == 1)) | (slot_sel[slot_of_blk] == qsel)
            pastb[i] = np.where((true_blk < qb) & allowed, 0.0, -1e30)
            ownm[i, i // 2] = 1.0
        uw = np.zeros((6, 2), np.float32)
        for u, (q, _) in enumerate(units):
            uw[u, q] = 1.0
        m["uw"] = np.ascontiguousarray(np.broadcast_to(uw[None], (128, 6, 2)))
        m["pastb"] = np.ascontiguousarray(np.broadcast_to(pastb[None], (128, NO, 32)))
        m["ownm"] = np.ascontiguousarray(np.broadcast_to(ownm[None], (128, NO, 32)))
        chunk_of_tile = np.array([order[n // 8] for n in range(NT)])
        visA = np.zeros(NT, np.float32)
        visB = np.zeros(NT, np.float32)
        m["visA"] = np.ascontiguousarray(np.broadcast_to(visA[None], (128, NT)))
        m["visB"] = np.ascontiguousarray(np.broadcast_to(visB[None], (128, NT)))
        first_occ = np.array([n == int(np.argmax(true_tile == true_tile[n])) for n in range(NT)])
        m["Mord"] = ((true_tile[:, None] < true_tile[None, :]) & first_occ[:, None]).astype(np.float32)
        maps.append(m)
    return maps


_NC = None


def kernel(**inputs):
    global _NC
    maps = prep(inputs)
    if _NC is None:
        _NC = build()
    res = run_bass_kernel_spmd(_NC, maps, core_ids=list(range(8)))
    out = np.zeros((2, 8192, D), np.float32)
    for c in range(8):
        b = c // 4
        order = chunk_order(c)
        yc = np.asarray(res.results[c]["y"]).reshape(2048, D)
        out[b, order[0] * 1024:(order[0] + 1) * 1024] = yc[:1024]
        out[b, order[1] * 1024:(order[1] + 1) * 1024] = yc[1024:]
    return out
```

```python
import numpy as np
from contextlib import ExitStack
import concourse.bass as bass
import concourse.mybir as mybir
from concourse.bass_utils import run_bass_kernel_spmd

F32 = mybir.dt.float32
BF16 = mybir.dt.bfloat16
U32 = mybir.dt.uint32
I32 = mybir.dt.int32
AF = mybir.ActivationFunctionType
ALU = mybir.AluOpType
AX = mybir.AxisListType

NDS = 48
SAME_ENG_WAIT = True
NEG = -30000.0
FILLER = False
DEBUG = False
STOP_AFTER = 99


class FW:
    def __init__(self, nc):
        self.nc = nc
        self.E = {'pe': nc.tensor, 'act': nc.scalar, 'dve': nc.vector,
                  'pool': nc.gpsimd, 'sp': nc.sync}
        self.sems = {}
        for e in self.E:
            self.sems['c' + e] = nc.alloc_semaphore(name='c_' + e)
        for i in range(NDS):
            self.sems['d%d' % i] = nc.alloc_semaphore(name='d_%d' % i)
        self.val = {k: 0 for k in self.sems}
        self.seen = {e: {} for e in self.E}
        self.res = {}
        self.dnext = {}
        self.ninst = {e: 0 for e in self.E}

    def _wait(self, eng, toks):
        need = {}
        for t in toks:
            if t is None:
                continue
            s, v = t
            if s == 'c' + eng and (eng == 'pe' or not SAME_ENG_WAIT):
                continue
            if v > need.get(s, 0):
                need[s] = v
        for s, v in need.items():
            if v > self.seen[eng].get(s, 0):
                self.E[eng].wait_ge(self.sems[s], v)
                self.seen[eng][s] = v

    def _deps(self, reads, writes):
        toks = []
        for r in reads:
            st = self.res.get(r)
            if st:
                toks.append(st['w'])
        for w in writes:
            st = self.res.get(w)
            if st:
                toks.append(st['w'])
                toks.extend(st['r'].values())
        return toks

    def _commit(self, tok, reads, writes):
        for r in reads:
            st = self.res.setdefault(r, {'w': None, 'r': {}})
            st['r'][tok[0]] = tok
        for w in writes:
            self.res[w] = {'w': tok, 'r': {}}

    def prewait(self, eng, reads=(), writes=()):
        self._wait(eng, self._deps(reads, writes))

    def op(self, eng, fn, reads=(), writes=()):
        self._wait(eng, self._deps(reads, writes))
        inst = fn(self.E[eng])
        s = 'c' + eng
        self.val[s] += 1
        inst.then_inc(self.sems[s], 1)
        self.ninst[eng] += 1
        self._commit((s, self.val[s]), reads, writes)
        return inst

    def dma(self, eng, fn, reads=(), writes=()):
        half = NDS // 2
        k = self.dnext.get(eng, 0)
        self.dnext[eng] = (k + 1) % half
        i = k + (half if eng == 'pool' else 0)
        s = 'd%d' % i
        toks = self._deps(reads, writes)
        toks.append((s, self.val[s]) if self.val[s] else None)
        self._wait(eng, toks)
        inst = fn(self.E[eng])
        self.val[s] += 16
        inst.then_inc(self.sems[s], 16)
        self.ninst[eng] += 1
        self._commit((s, self.val[s]), reads, writes)
        return inst

    def barrier(self):
        toks = [(s, v) for s, v in self.val.items() if v]
        for e in self.E:
            for s, v in toks:
                if s == 'c' + e:
                    continue
                if v > self.seen[e].get(s, 0):
                    self.E[e].wait_ge(self.sems[s], v)
                    self.seen[e][s] = v
        self.res = {}


NT = 64
NO = 16
D = 1024


def core_units(c):
    j = c % 4
    return [(0, k) for k in range(8) if k < j] + [(1, k) for k in range(8) if k < 7 - j and k != j]


def chunk_order(c):
    j = c % 4
    return [j, 7 - j] + [k for (_, k) in core_units(c)]


def build():
    nc = bass.Bass("TRN2", target_bir_lowering=False)
    fw = FW(nc)

    def din(name, shape, dt=F32):
        return nc.dram_tensor(name, list(shape), dt, kind="ExternalInput").ap()

    def dscr(name, shape, dt):
        return nc.dram_tensor(name, list(shape), dt,
                              kind="ExternalOutput" if DEBUG else "Internal").ap()

    xT = din("xT", [NT, 128, 8, 128])
    xown = din("xown", [NO, 128, D])
    w_kv = din("w_kv", [128, 8, 2056])
    w_q = din("w_q", [128, 8, 1024])
    w_g = din("w_g", [128, 8, 2048])
    g_mix = din("g_mix", [128, 8])
    bfg = din("bfg", [128, 8])
    ropec = din("ropec", [128, NT, 8])
    ropes = din("ropes", [128, NT, 8])
    wba = din("wba", [64, 8, D])
    wbb = din("wbb", [64, 8, D])
    wout = din("wout", [128, 8, D])
    g_ffn = din("g_ffn", [128, D])
    wpq = din("wpq", [128, 8, 2048])
    skT = din("skT", [128, 16, 128])
    Ur = din("Ur", [128, 128, 8, 128])
    Vr = din("Vr", [128, 128, D])
    iota128d = din("iota128", [128, 128])
    g_fin = din("g_fin", [128, D])
    trim = din("trim", [128, 4, 512])
    tric = din("tric", [128, 128])
    onehot = din("onehot", [32, 8192])
    pastb = din("pastb", [128, NO, 32])
    ownm = din("ownm", [128, NO, 32])
    visA = din("visA", [128, NT])
    visB = din("visB", [128, NT])
    Mord = din("Mord", [64, 64])
    iota16 = din("iota16", [128, 16])
    uwd = din("uw", [128, 6, 2])
    y = nc.dram_tensor("y", [NO, 128, D], F32, kind="ExternalOutput").ap()

    KTA = dscr("KTA", [8, 64, 8192], BF16)
    KTB = dscr("KTB", [8, 64, 8192], BF16)
    VA = dscr("VA", [8, 128, NT, 65], BF16)
    VB = dscr("VB", [8, 128, NT, 65], BF16)
    QTA = dscr("QTA", [8, 96, 2048], BF16)
    QTB = dscr("QTB", [8, 65, 2048], BF16)
    ATT = dscr("ATT", [2, 8, 64, 2048], BF16)
    HS = dscr("HS", [NO, 128, D], F32)
    Gscr = nc.dram_tensor("Gscr", [128, 128, 2048], BF16, kind="Internal").ap()

    psall = nc.alloc_psum_tensor("psall", [128, 4096], F32)
    ps = [psall[:, i * 512:(i + 1) * 512] for i in range(8)]
    P = ['ps%d' % i for i in range(8)]

    glob = ExitStack()

    def sb(stack, name, shape, dt):
        return stack.enter_context(nc.sbuf_tensor(name, list(shape), dt))

    identf = sb(glob, "identf", [128, 128], F32)
    identb = sb(glob, "identb", [128, 128], BF16)
    ones_f = sb(glob, "ones_f", [128, 128], F32)
    rstd_all = sb(glob, "rstd_all", [128, NT], F32)
    gmix_sb = sb(glob, "gmix_sb", [128, 8], F32)
    negcA = sb(glob, "negcA", [128, NT, 8], F32)
    negcB = sb(glob, "negcB", [128, NT, 8], F32)
    eps_sb = sb(glob, "eps_sb", [128, 1], F32)

    fw.op('pool', lambda e: e.memset(identf[:], 0.0), writes=['identf'])
    fw.op('pool', lambda e: e.affine_select(out=identf[:], in_=identf[:], pattern=[[-1, 128]],
                                            compare_op=ALU.not_equal, fill=1.0, base=0,
                                            channel_multiplier=1), reads=['identf'], writes=['identf'])
    fw.op('dve', lambda e: e.tensor_copy(out=identb[:], in_=identf[:]), reads=['identf'], writes=['identb'])
    fw.op('dve', lambda e: e.memset(ones_f[:], 1.0), writes=['ones_f'])
    fw.op('dve', lambda e: e.memset(eps_sb[:], 1e-6), writes=['eps_sb'])
    fw.dma('sp', lambda e: e.dma_start(out=gmix_sb[:], in_=g_mix), writes=['gmix'])

    def load_xg(i, xtf, xg, slot):
        fw.dma('sp', lambda e: e.dma_start(out=xtf[slot][:], in_=xT[i]), writes=[('xtf', slot)])
        fw.op('dve', lambda e: e.tensor_tensor(
            out=xg[slot][:], in0=xtf[slot][:],
            in1=gmix_sb[:].unsqueeze(2).to_broadcast([128, 8, 128]), op=ALU.mult),
            reads=[('xtf', slot), 'gmix'], writes=[('xg', slot)])

    def rope(src3, i, tmp, key):
        x1 = src3[:, :, 0:8]
        x2 = src3[:, :, 8:16]
        cs = ropec_sb[:, i, :].unsqueeze(1).to_broadcast([128, 8, 8])
        sn = ropes_sb[:, i, :].unsqueeze(1).to_broadcast([128, 8, 8])
        rk = [key, 'rope_tab']
        fw.op('pool', lambda e: e.tensor_tensor(out=tmp[:, 0], in0=x1, in1=cs, op=ALU.mult), reads=rk, writes=['rt0'])
        fw.op('pool', lambda e: e.tensor_tensor(out=tmp[:, 1], in0=x2, in1=sn, op=ALU.mult), reads=rk, writes=['rt1'])
        fw.op('pool', lambda e: e.tensor_tensor(out=tmp[:, 2], in0=x1, in1=sn, op=ALU.mult), reads=rk, writes=['rt2'])
        fw.op('pool', lambda e: e.tensor_tensor(out=tmp[:, 3], in0=x2, in1=cs, op=ALU.mult), reads=rk, writes=['rt3'])
        fw.op('pool', lambda e: e.tensor_tensor(out=x1, in0=tmp[:, 0], in1=tmp[:, 1], op=ALU.subtract),
              reads=['rt0', 'rt1'], writes=[key])
        fw.op('pool', lambda e: e.tensor_tensor(out=x2, in0=tmp[:, 2], in1=tmp[:, 3], op=ALU.add),
              reads=['rt2', 'rt3'], writes=[key])

    p1 = ExitStack()
    p1a = ExitStack()
    ropec_sb = sb(p1, "ropec_sb", [128, NT, 8], F32)
    ropes_sb = sb(p1, "ropes_sb", [128, NT, 8], F32)
    xtf = [sb(p1, "xtf%d" % k, [128, 8, 128], F32) for k in range(2)]
    xg = [sb(p1, "xg%d" % k, [128, 8, 128], BF16) for k in range(2)]
    rtmp = sb(p1, "rtmp", [128, 4, 8, 8], F32)
    kmT = sb(p1, "kmT", [64, 8, 32], BF16)
    negc = sb(p1, "negc", [128, NT, 8], F32)
    wkv_sb = sb(p1a, "wkv_sb", [128, 8, 2056], BF16)
    sq = sb(p1a, "sq", [128, 8, 128], BF16)
    ones_b = sb(p1a, "ones_b", [128, 1], BF16)
    ssb = sb(p1a, "ssb", [128, 1], F32)
    kf = sb(p1a, "kf", [128, 8, 64], F32)
    kab = [sb(p1a, "kab%d" % k, [128, 8, 64], BF16) for k in range(2)]
    kbb = [sb(p1a, "kbb%d" % k, [128, 8, 64], BF16) for k in range(2)]
    KV_BACK = [None]
    stKA = [sb(p1a, "stKA%d" % k, [64, 8, 512], BF16) for k in range(2)]
    stKB = [sb(p1a, "stKB%d" % k, [64, 8, 512], BF16) for k in range(2)]
    stVA = [sb(p1a, "stVA%d" % k, [128, 8, 8, 65], BF16) for k in range(2)]
    stVB = [sb(p1a, "stVB%d" % k, [128, 8, 8, 65], BF16) for k in range(2)]
    fl_all = sb(p1a, "fl_all", [128, NT, 8], F32)
    ksum = sb(p1a, "ksum", [64, 8, NT], F32)

    for k in range(8):
        fw.dma('pool', lambda e, k=k: e.dma_start(out=wkv_sb[:, k, :], in_=w_kv[:, k, :]), writes=['wkv'])
    fw.dma('sp', lambda e: e.dma_start(out=ropec_sb[:], in_=ropec), writes=['rope_tab'])
    fw.dma('sp', lambda e: e.dma_start(out=ropes_sb[:], in_=ropes), writes=['rope_tab'])
    fw.op('pool', lambda e: e.memset(ones_b[:], 1.0), writes=['ones_b'])
    for k in range(2):
        fw.op('pool', lambda e, k=k: e.memset(stVA[k][:], 1.0), writes=[('stVA', k)])
        fw.op('pool', lambda e, k=k: e.memset(stVB[k][:], 1.0), writes=[('stVB', k)])

    def kv_back(i):
        g4 = (i // 4) % 2
        kb2 = i % 2
        ptA = ps[6][0:64, :].bitcast(BF16).rearrange("p (h t) -> p h t", h=8)
        ptB = ps[7][0:64, :].bitcast(BF16).rearrange("p (h t) -> p h t", h=8)
        for h in range(8):
            fw.op('pe', lambda e, h=h: e.transpose(out=ptA[:, h, :], in_=kab[kb2][:, h, :], identity=identb[:]),
                  reads=[('kab', kb2), 'identb'], writes=[P[6]])
        fw.op('dve', lambda e: e.tensor_copy(out=stKA[g4][:, :, (i % 4) * 128:(i % 4 + 1) * 128], in_=ptA),
              reads=[P[6]], writes=[('stKA', g4)])
        fw.op('dve', lambda e: e.tensor_reduce(out=ksum[:, :, i], in_=ptA, axis=AX.X, op=ALU.add),
              reads=[P[6]], writes=['ksum'])
        for h in range(8):
            fw.op('pe', lambda e, h=h: e.transpose(out=ptB[:, h, :], in_=kbb[kb2][:, h, :], identity=identb[:]),
                  reads=[('kbb', kb2), 'identb'], writes=[P[7]])
        fw.op('dve', lambda e: e.tensor_copy(out=stKB[g4][:, :, (i % 4) * 128:(i % 4 + 1) * 128], in_=ptB),
              reads=[P[7]], writes=[('stKB', g4)])
        if i % 4 == 3:
            g = i // 4
            fw.dma('sp', lambda e, g=g: e.dma_start(
                out=KTA[:, :, g * 512:(g + 1) * 512].rearrange("h d s -> d h s"), in_=stKA[g4][:]),
                reads=[('stKA', g4)], writes=['KTA'])
            fw.dma('sp', lambda e, g=g: e.dma_start(
                out=KTB[:, :, g * 512:(g + 1) * 512].rearrange("h d s -> d h s"), in_=stKB[g4][:]),
                reads=[('stKB', g4)], writes=['KTB'])

    KV_BACK[0] = kv_back
    for i in range(NT):
        s = i % 2
        load_xg(i, xtf, xg, s)
        if i >= 1:
            KV_BACK[0](i - 1)
        fw.op('act', lambda e: e.activation(out=sq[:], in_=xtf[s][:], func=AF.Square),
              reads=[('xtf', s)], writes=['sq'])
        for c in range(8):
            fw.op('pe', lambda e, c=c: e.matmul(ps[5][:, 0:1], lhsT=sq[:, c, :], rhs=ones_b[:, 0:1],
                                                 start=(c == 0), stop=(c == 7)),
                  reads=['sq', 'ones_b'], writes=[P[5]])
        fw.op('act', lambda e: e.activation(out=ssb[:], in_=ps[5][:, 0:1], func=AF.Sqrt,
                                            bias=eps_sb[:], scale=1.0 / D),
              reads=[P[5], 'eps_sb'], writes=['ssb'])
        fw.op('dve', lambda e: e.reciprocal(out=rstd_all[:, i:i + 1], in_=ssb[:]),
              reads=['ssb'], writes=[('rstd', i)])
        rs = rstd_all[:, i:i + 1]
        for bnk, (c0, c1) in enumerate([(0, 512), (512, 1024), (1024, 1536), (1536, 2048), (2048, 2056)]):
            for c in range(8):
                fw.op('pe', lambda e, c=c, bnk=bnk, c0=c0, c1=c1: e.matmul(
                    ps[bnk][:, 0:c1 - c0], lhsT=xg[s][:, c, :], rhs=wkv_sb[:, c, c0:c1],
                    start=(c == 0), stop=(c == 7)),
                    reads=[('xg', s), 'wkv'], writes=[P[bnk]])
        g8 = (i // 8) % 2
        kb2 = i % 2
        fw.op('act', lambda e: e.activation(out=kf[:].rearrange("p h d -> p (h d)"), in_=ps[0][:],
                                            func=AF.Copy, scale=rs),
              reads=[P[0], ('rstd', i)], writes=['kf'])
        rope(kf[:], i, rtmp, 'kf')
        fw.op('pool', lambda e: e.tensor_copy(out=kab[kb2][:], in_=kf[:]), reads=['kf'], writes=[('kab', kb2)])
        fw.op('dve', lambda e: e.tensor_scalar(
            out=stVA[g8][:, :, i % 8, 0:64], in0=ps[1][:].rearrange("p (h d) -> p h d", h=8),
            scalar1=rs, scalar2=None, op0=ALU.mult),
            reads=[P[1], ('rstd', i)], writes=[('stVA', g8)])
        fw.op('act', lambda e: e.activation(out=kbb[kb2][:].rearrange("p h d -> p (h d)"), in_=ps[2][:],
                                            func=AF.Copy, scale=rs),
              reads=[P[2], ('rstd', i)], writes=[('kbb', kb2)])
        fw.op('act', lambda e: e.activation(
            out=stVB[g8][:, :, i % 8, 0:64], in_=ps[3][:].rearrange("p (h d) -> p h d", h=8),
            func=AF.Copy, scale=rs),
            reads=[P[3], ('rstd', i)], writes=[('stVB', g8)])
        fw.op('dve', lambda e: e.tensor_scalar(out=fl_all[:, i, :], in0=ps[4][:, 0:8], scalar1=rs,
                                               scalar2=None, op0=ALU.mult),
              reads=[P[4], ('rstd', i)], writes=['fl_all'])
        if i % 8 == 7:
            g = i // 8
            fw.dma('sp', lambda e, g=g: e.dma_start(
                out=VA[:, :, g * 8:(g + 1) * 8, :].rearrange("h t i e -> t h i e"), in_=stVA[g8][:]),
                reads=[('stVA', g8)], writes=['VA'])
            fw.dma('sp', lambda e, g=g: e.dma_start(
                out=VB[:, :, g * 8:(g + 1) * 8, :].rearrange("h t i e -> t h i e"), in_=stVB[g8][:]),
                reads=[('stVB', g8)], writes=['VB'])

    kv_back(NT - 1)

    kv = ksum[:].rearrange("d h (n two) -> d h n two", two=2)
    ksum2 = sb(p1a, "ksum2", [64, 8, 32], F32)
    fw.op('dve', lambda e: e.tensor_tensor(out=ksum2[:], in0=kv[:, :, :, 0], in1=kv[:, :, :, 1], op=ALU.add),
          reads=['ksum'], writes=['ksum2'])
    fw.op('dve', lambda e: e.tensor_scalar(out=kmT[:], in0=ksum2[:], scalar1=1.0 / 256, scalar2=None, op0=ALU.mult),
          reads=['ksum2'], writes=['kmT'])

    bf_sb = sb(p1a, "bf_sb", [128, 8], F32)
    tri_sb = sb(p1a, "tri_sb", [128, 128], F32)
    M_sb = sb(p1a, "M_sb", [64, 64], F32)
    visA_sb = sb(p1a, "visA_sb", [128, NT], F32)
    visB_sb = sb(p1a, "visB_sb", [128, NT], F32)
    Lt = sb(p1a, "Lt", [128, NT, 8], F32)
    T2 = sb(p1a, "T2", [64, 8], F32)
    R = sb(p1a, "R", [64, NT, 8], F32)
    fw.dma('sp', lambda e: e.dma_start(out=bf_sb[:], in_=bfg), writes=['bf_sb'])
    fw.dma('sp', lambda e: e.dma_start(out=tri_sb[:], in_=tric), writes=['tri_sb'])
    fw.dma('sp', lambda e: e.dma_start(out=M_sb[:], in_=Mord), writes=['M_sb'])
    fw.dma('sp', lambda e: e.dma_start(out=visA_sb[:], in_=visA), writes=['visA_sb'])
    fw.dma('sp', lambda e: e.dma_start(out=visB_sb[:], in_=visB), writes=['visB_sb'])
    fw.op('dve', lambda e: e.tensor_tensor(out=Lt[:], in0=fl_all[:],
                                           in1=bf_sb[:].unsqueeze(1).to_broadcast([128, NT, 8]), op=ALU.add),
          reads=['fl_all', 'bf_sb'], writes=['Lt'])
    Lt2 = Lt[:].rearrange("p j h -> p (j h)")
    fw.op('act', lambda e: e.activation(out=Lt2, in_=Lt2, func=AF.Exp, scale=-1.0), reads=['Lt'], writes=['Lt'])
    fw.op('act', lambda e: e.activation(out=Lt2, in_=Lt2, func=AF.Ln, bias=ones_f[:, 0:1], scale=1.0),
          reads=['Lt', 'ones_f'], writes=['Lt'])
    for h in range(8):
        fw.op('pe', lambda e, h=h: e.matmul(ps[1][0:64, h:h + 1], lhsT=Lt[:, :, h], rhs=ones_f[:, 0:1],
                                             start=True, stop=True),
              reads=['Lt', 'ones_f'], writes=[P[1]])
    fw.op('dve', lambda e: e.tensor_copy(out=T2[:], in_=ps[1][0:64, 0:8]), reads=[P[1]], writes=['T2'])
    fw.op('dve', lambda e: e.tensor_tensor(out=R[:], in0=M_sb[:].unsqueeze(2).to_broadcast([64, NT, 8]),
                                           in1=T2[:].unsqueeze(1).to_broadcast([64, NT, 8]), op=ALU.mult),
          reads=['M_sb', 'T2'], writes=['R'])
    fw.op('pe', lambda e: e.matmul(ps[0][:], lhsT=tri_sb[:], rhs=Lt2, start=True, stop=False),
          reads=['tri_sb', 'Lt'], writes=[P[0]])
    fw.op('pe', lambda e: e.matmul(ps[0][:], lhsT=ones_f[0:64, :], rhs=R[:].rearrange("p j h -> p (j h)"),
                                   start=False, stop=True),
          reads=['ones_f', 'R'], writes=[P[0]])
    fw.op('dve', lambda e: e.tensor_copy(out=negc[:].rearrange("p j h -> p (j h)"), in_=ps[0][:]),
          reads=[P[0]], writes=['negc'])
    fw.op('dve', lambda e: e.tensor_tensor(out=negcA[:], in0=negc[:],
                                           in1=visA_sb[:].unsqueeze(2).to_broadcast([128, NT, 8]), op=ALU.add),
          reads=['negc', 'visA_sb'], writes=['negcA'])
    fw.op('dve', lambda e: e.tensor_tensor(out=negcB[:], in0=negc[:],
                                           in1=visB_sb[:].unsqueeze(2).to_broadcast([128, NT, 8]), op=ALU.add),
          reads=['negc', 'visB_sb'], writes=['negcB'])

    fw.barrier()
    p1a.close()
    wq_sb = sb(p1, "wq_sb", [128, 8, 1024], BF16)
    for k in range(8):
        fw.dma('pool', lambda e, k=k: e.dma_start(out=wq_sb[:, k, :], in_=w_q[:, k, :]), writes=['wq'])
    pastb_sb = sb(p1, "pastb_sb", [128, NO, 32], F32)
    ownm_sb = sb(p1, "ownm_sb", [128, NO, 32], F32)
    fw.dma('sp', lambda e: e.dma_start(out=pastb_sb[:], in_=pastb), writes=['pastb_sb'])
    fw.dma('sp', lambda e: e.dma_start(out=ownm_sb[:], in_=ownm), writes=['ownm_sb'])
    qf = sb(p1, "qf", [128, 8, 64], F32)
    qaA = [sb(p1, "qaA%d" % k, [128, 8, 96], BF16) for k in range(2)]
    qaB = [sb(p1, "qaB%d" % k, [128, 8, 65], BF16) for k in range(2)]
    qT = sb(p1, "qT", [64, 8, 128], BF16)
    gm = sb(p1, "gm", [128, 8, 32], F32)
    m8 = sb(p1, "m8", [128, 8, 8], F32)
    thr = sb(p1, "thr", [128, 8], F32)
    sel = sb(p1, "sel", [128, 8, 32], F32)
    stQA = [sb(p1, "stQA%d" % k, [96, 8, 512], BF16) for k in range(2)]
    stQB = [sb(p1, "stQB%d" % k, [65, 8, 512], BF16) for k in range(2)]

    def q_front(i):
        s = i % 2
        load_xg(i, xtf, xg, s)
        rs = rstd_all[:, i:i + 1]
        for bnk, (c0, c1) in enumerate([(0, 512), (512, 1024)]):
            for c in range(8):
                fw.op('pe', lambda e, c=c, bnk=bnk, c0=c0, c1=c1: e.matmul(
                    ps[bnk][:], lhsT=xg[s][:, c, :], rhs=wq_sb[:, c, c0:c1],
                    start=(c == 0), stop=(c == 7)),
                    reads=[('xg', s), 'wq'], writes=[P[bnk]])
        fw.op('act', lambda e: e.activation(out=qf[:].rearrange("p h d -> p (h d)"), in_=ps[0][:],
                                            func=AF.Copy, scale=rs),
              reads=[P[0], ('rstd', i)], writes=['qf'])
        rope(qf[:], i, rtmp, 'qf')
        fw.op('pool', lambda e: e.tensor_copy(out=qaA[s][:, :, 0:64], in_=qf[:]), reads=['qf'], writes=[('qaA', s)])
        fw.op('act', lambda e: e.activation(out=qaB[s][:, :, 0:64], in_=ps[1][:].rearrange("p (h d) -> p h d", h=8),
                                            func=AF.Copy, scale=rs),
              reads=[P[1], ('rstd', i)], writes=[('qaB', s)])
        fw.op('pool', lambda e: e.tensor_scalar(out=qaB[s][:, :, 64], in0=negc[:, i, :], scalar1=-8.0, scalar2=None,
                                                op0=ALU.mult),
              reads=['negc', ('qaB', s)], writes=[('qaB', s)])

    def q_back(i):
        s = i % 2
        g4 = (i // 4) % 2
        pt64 = ps[6][0:64, :].bitcast(BF16).rearrange("p (h t) -> p h t", h=8)
        for h in range(8):
            fw.op('pe', lambda e, h=h: e.transpose(out=pt64[:, h, :], in_=qaA[s][:, h, 0:64], identity=identb[:]),
                  reads=[('qaA', s), 'identb'], writes=[P[6]])
        fw.op('dve', lambda e: e.tensor_copy(out=qT[:], in_=pt64), reads=[P[6]], writes=['qT'])
        psg = ps[2][:, 0:256].rearrange("p (h n) -> p h n", h=8)
        for h in range(8):
            fw.op('pe', lambda e, h=h: e.matmul(psg[:, h, :], lhsT=qT[:, h, :], rhs=kmT[:, h, :],
                                                 start=True, stop=True),
                  reads=['qT', 'kmT'], writes=[P[2]])
        pt65 = ps[3][0:65, :].bitcast(BF16).rearrange("p (h t) -> p h t", h=8)
        for h in range(8):
            fw.op('pe', lambda e, h=h: e.transpose(out=pt65[:, h, :], in_=qaB[s][:, h, :], identity=identb[:]),
                  reads=[('qaB', s), 'identb'], writes=[P[3]])
        fw.op('dve', lambda e: e.tensor_tensor(out=gm[:], in0=psg,
                                               in1=pastb_sb[:, i, :].unsqueeze(1).to_broadcast([128, 8, 32]),
                                               op=ALU.add),
              reads=[P[2], 'pastb_sb'], writes=['gm'])
        for h in range(8):
            fw.op('dve', lambda e, h=h: e.max(out=m8[:, h, :], in_=gm[:, h, :]), reads=['gm'], writes=[('m8', h)])
        fw.op('dve', lambda e: e.tensor_scalar(out=thr[:], in0=m8[:, :, 2], scalar1=-1e29, scalar2=None,
                                               op0=ALU.max),
              reads=[('m8', h_) for h_ in range(8)], writes=['thr'])
        fw.op('dve', lambda e: e.tensor_tensor(out=sel[:], in0=gm[:],
                                               in1=thr[:].unsqueeze(2).to_broadcast([128, 8, 32]), op=ALU.is_ge),
              reads=['gm', 'thr'], writes=['sel'])
        fw.op('dve', lambda e: e.tensor_tensor(out=sel[:], in0=sel[:],
                                               in1=ownm_sb[:, i, :].unsqueeze(1).to_broadcast([128, 8, 32]),
                                               op=ALU.add),
              reads=['sel', 'ownm_sb'], writes=['sel'])
        fw.op('dve', lambda e: e.tensor_scalar(out=qaA[s][:, :, 64:96], in0=sel[:], scalar1=-1.0, scalar2=-NEG,
                                               op0=ALU.add, op1=ALU.mult),
              reads=['sel', ('qaA', s)], writes=[('qaA', s)])
        fw.op('act', lambda e: e.activation(out=stQB[g4][:, :, (i % 4) * 128:(i % 4 + 1) * 128], in_=pt65, func=AF.Copy),
              reads=[P[3]], writes=[('stQB', g4)])
        pt96 = ps[7][0:96, :].bitcast(BF16).rearrange("p (h t) -> p h t", h=8)
        for h in range(8):
            fw.op('pe', lambda e, h=h: e.transpose(out=pt96[:, h, :], in_=qaA[s][:, h, :], identity=identb[:]),
                  reads=[('qaA', s), 'identb'], writes=[P[7]])
        fw.op('act', lambda e: e.activation(out=stQA[g4][:, :, (i % 4) * 128:(i % 4 + 1) * 128], in_=pt96, func=AF.Copy),
              reads=[P[7]], writes=[('stQA', g4)])
        if i % 4 == 3:
            g = i // 4
            fw.dma('sp', lambda e, g=g: e.dma_start(
                out=QTA[:, :, g * 512:(g + 1) * 512].rearrange("h d s -> d h s"), in_=stQA[g4][:]),
                reads=[('stQA', g4)], writes=['QTA'])
            fw.dma('sp', lambda e, g=g: e.dma_start(
                out=QTB[:, :, g * 512:(g + 1) * 512].rearrange("h d s -> d h s"), in_=stQB[g4][:]),
                reads=[('stQB', g4)], writes=['QTB'])

    q_front(0)
    for i in range(NO):
        if i + 1 < NO:
            q_front(i + 1)
        q_back(i)
    fw.barrier()
    p1.close()
    if STOP_AFTER <= 1:
        return finish(nc, fw, y, glob)

    p2 = ExitStack()
    KT = [sb(p2, "KT%d" % k, [128, 8192], BF16) for k in range(2)]
    VV = [sb(p2, "VV%d" % k, [128, NT, 128], BF16) for k in range(2)]
    QT = [sb(p2, "QT%d" % k, [128, 2048], BF16) for k in range(2)]
    for k in range(2):
        fw.op('pool', lambda e, k=k: e.memset(VV[k][:], 0.0), writes=[('VV', k)])
    NPT = 8
    SBK = [0, 1, 2, 3, 7]
    PT = [sb(p2, "PT%d" % k, [128, 512], BF16) for k in range(NPT)]
    trim_sb = sb(p2, "trim_sb", [128, 4, 512], BF16)
    acc = [[sb(p2, "acc%d_%d" % (k, t), [65, 1024], F32) for t in range(2)] for k in range(2)]
    QU = [sb(p2, "QU%d" % k, [128, 1024], BF16) for k in range(2)]
    uw_sb = sb(p2, "uw_sb", [128, 6, 2], F32)
    fw.dma('sp', lambda e: e.dma_start(out=uw_sb[:], in_=uwd), writes=['uw'])
    rrow = [sb(p2, "rrow%d" % k, [65, 512], BF16) for k in range(4)]
    njob = 0
    ones_bb = sb(p2, "ones_bb", [65, 64], BF16)
    fw.op('pool', lambda e: e.memset(ones_bb[:], 1.0), writes=['ones_bb'])
    pending = []
    attst = [sb(p2, "attst%d" % k, [64, 512], BF16) for k in range(4)]
    fw.dma('pool', lambda e: e.dma_start(out=trim_sb[:], in_=trim), writes=['trim_sb'])
    trimb_sb = sb(p2, "trimb_sb", [128, 4, 512], F32)
    stmp = [sb(p2, "stmp%d" % k, [128, 512], F32) for k in range(2)]
    fw.dma('sp', lambda e: e.dma_start(out=trimb_sb[:], in_=trim), writes=['trimb_sb'])
    fw.op('dve', lambda e: e.tensor_scalar(out=trimb_sb[:], in0=trimb_sb[:], scalar1=-1.0, scalar2=1e6,
                                           op0=ALU.add, op1=ALU.mult), reads=['trimb_sb'], writes=['trimb_sb'])
    ndiag = 0

    scale = 0.125
    npair = 0
    nq = 0
    for br in range(2):
        for h in range(8):
            hs = (br * 8 + h) % 2
            Kd, Vd, Qd = (KTA, VA, QTA) if br == 0 else (KTB, VB, QTB)
            Rq = 96 if br == 0 else 65
            Rr = 128
            fw.op('pool', lambda e: e.memset(KT[hs][64:128, :], 0.0), writes=[('KT', hs)])
            fw.op('pool', lambda e: e.memset(QT[hs][64:128, :], 0.0), writes=[('QT', hs)])
            fw.dma('sp', lambda e, h=h, Kd=Kd: e.dma_start(out=KT[hs][0:64, :], in_=Kd[h]),
                   reads=['KTA', 'KTB'], writes=[('KT', hs)])
            if br == 0:
                fw.dma('pool', lambda e: e.dma_start(out=KT[hs][64:96, :], in_=onehot),
                       writes=[('KT', hs)])
            else:
                fw.op('pool', lambda e: e.memset(KT[hs][64:65, :], 1.0), writes=[('KT', hs)])
            fw.dma('sp', lambda e, h=h, Vd=Vd: e.dma_start(out=VV[hs][:, :, 0:65], in_=Vd[h]),
                   reads=['VA', 'VB'], writes=[('VV', hs)])
            fw.dma('sp', lambda e, h=h, Qd=Qd: e.dma_start(out=QT[hs][0:Rq, :], in_=Qd[h]),
                   reads=['QTA', 'QTB'], writes=[('QT', hs)])
            hp = (br * 8 + h) % 2
            jobs = []
            for qt in range(4):
                u = qt % 2
                if qt < 2:
                    tiles = [(t, None) for t in range(0, 4 * u)] + [(4 * u + o, o) for o in range(4)]
                else:
                    tiles = [(t, None) for t in range(0, 8)] + [(8 + t, None) for t in range(0, 4 * u)] \
                        + [(8 + 4 * u + o, o) for o in range(4)]
                jobs.append(('own', QT[hs][:, qt * 512:(qt + 1) * 512], ('QT', hs), tiles, qt // 2, qt % 2, None))
            for un in range(6):
                for half in range(2):
                    tiles = [(16 + 8 * un + t, None) for t in range(8)]
                    jobs.append(('unit', QU[un % 2][:, half * 512:(half + 1) * 512], ('QU', un % 2), tiles, None, half, un))
            items = []
            for jn, job in enumerate(jobs):
                for n, (kt, o) in enumerate(job[3]):
                    items.append((jn, kt, o, n == 0, n == len(job[3]) - 1))

            def emit_S(it, g):
                jn, kt, o, first, last = it
                kind, qap, qkey, _, tgt, half, un = jobs[jn]
                sbk = SBK[g % 5]
                pk = g % NPT
                nxt = None
                if first and kind == 'own' and jn == 3:
                    nxt = 0
                elif first and kind == 'unit' and half == 0 and un + 1 < 6:
                    nxt = un + 1
                if nxt is not None:
                    ub = nxt % 2
                    fw.op('dve', lambda e: e.tensor_scalar(out=QU[ub][:], in0=QT[hs][:, 0:1024], scalar1=uw_sb[:, nxt, 0:1],
                                                           scalar2=None, op0=ALU.mult),
                          reads=[('QT', hs), 'uw'], writes=[('QU', ub)])
                    fw.op('dve', lambda e: e.scalar_tensor_tensor(out=QU[ub][:], in0=QT[hs][:, 1024:2048],
                                                                  scalar=uw_sb[:, nxt, 1:2], in1=QU[ub][:],
                                                                  op0=ALU.mult, op1=ALU.add),
                          reads=[('QT', hs), 'uw', ('QU', ub)], writes=[('QU', ub)])
                fw.op('pe', lambda e: e.matmul(
                    ps[sbk][:], lhsT=KT[hs][:, kt * 128:(kt + 1) * 128], rhs=qap, start=True, stop=True),
                    reads=[('KT', hs), qkey], writes=[P[sbk]])
                if br == 0:
                    fw.op('act', lambda e: e.activation(
                        out=PT[pk][:], in_=ps[sbk][:], func=AF.Exp, scale=scale),
                        reads=[P[sbk]], writes=[('PT', pk)])
                    if o is not None:
                        fw.op('dve', lambda e: e.tensor_tensor(
                            out=PT[pk][:], in0=PT[pk][:], in1=trim_sb[:, o, :], op=ALU.mult),
                            reads=[('PT', pk), 'trim_sb'], writes=[('PT', pk)])
                elif o is not None:
                    dk = g % 2
                    fw.op('dve', lambda e: e.tensor_tensor(
                        out=stmp[dk][:], in0=ps[sbk][:], in1=trimb_sb[:, o, :], op=ALU.add),
                        reads=[P[sbk], 'trimb_sb'], writes=[('stmp', dk)])
                    fw.op('act', lambda e: e.activation(
                        out=PT[pk][:], in_=stmp[dk][:], func=AF.Exp, bias=negcA[:, kt, h:h + 1], scale=scale),
                        reads=[('stmp', dk), 'negcA'], writes=[('PT', pk)])
                else:
                    fw.op('act', lambda e: e.activation(
                        out=PT[pk][:], in_=ps[sbk][:], func=AF.Exp, bias=negcA[:, kt, h:h + 1], scale=scale),
                        reads=[P[sbk], 'negcA'], writes=[('PT', pk)])

            def emit_PV(it, g):
                jn, kt, o, first, last = it
                kind, qap, qkey, _, tgt, half, un = jobs[jn]
                pk = g % NPT
                ob = 4 + ((njob + jn) % 2)
                hsl = slice(half * 512, (half + 1) * 512)
                fw.op('pe', lambda e: e.matmul(
                    ps[ob][:], lhsT=VV[hs][:, kt, :], rhs=PT[pk][:], start=first, stop=last),
                    reads=[('VV', hs), ('PT', pk)], writes=[P[ob]])
                if last and kind == 'own':
                    fw.op('dve', lambda e: e.tensor_copy(out=acc[hp][tgt][:, hsl], in_=ps[ob][0:65, :]),
                          reads=[P[ob]], writes=[('acc', hp, tgt, half)])
                elif last:
                    for tg2 in range(2):
                        fw.op('dve', lambda e, tg2=tg2: e.scalar_tensor_tensor(
                            out=acc[hp][tg2][:, hsl], in0=ps[ob][0:65, :], scalar=uw_sb[0:65, un, tg2:tg2 + 1],
                            in1=acc[hp][tg2][:, hsl], op0=ALU.mult, op1=ALU.add),
                            reads=[P[ob], 'uw', ('acc', hp, tg2, half)], writes=[('acc', hp, tg2, half)])

            LA = 4
            GP = 2
            nit = len(items)
            for idx in range(0, nit + LA, GP):
                pv = [k - LA for k in range(idx, idx + GP) if 0 <= k - LA < nit]
                if pv:
                    fw.prewait('pe', reads=[('PT', (npair + pv[-1]) % NPT)])
                for k in range(idx, idx + GP):
                    if k < nit:
                        emit_S(items[k], npair + k)
                for k in pv:
                    emit_PV(items[k], npair + k)
                for pd in list(pending):
                    pd[0] -= GP
                    if pd[0] <= 0:
                        pending.remove(pd)
                        pd[1]()
            npair += len(items)
            njob += len(jobs)
            for qt in range(4):
                tgt, half = qt // 2, qt % 2
                hsl = slice(half * 512, (half + 1) * 512)
                with nc.allow_low_precision("softmax normaliser 1/l is broadcast through a bf16 K=1 matmul"):
                    fw.op('dve', lambda e, qt=qt, tgt=tgt, hsl=hsl: e.reciprocal(out=rrow[qt][64:65, :], in_=acc[hp][tgt][64:65, hsl]),
                          reads=[('acc', hp, tgt, half)], writes=[('rrow', qt)])

                def fin(qt=qt, tgt=tgt, half=half, hsl=hsl, hp=hp, br=br, h=h):
                    fw.op('pe', lambda e: e.matmul(ps[6][0:64, :], lhsT=ones_bb[64:65, 0:64], rhs=rrow[qt][64:65, :],
                                                   start=True, stop=True),
                          reads=[('rrow', qt), 'ones_bb'], writes=[P[6]])
                    fw.op('dve', lambda e: e.tensor_tensor(out=attst[qt][:], in0=acc[hp][tgt][0:64, hsl], in1=ps[6][0:64, :],
                                                           op=ALU.mult),
                          reads=[('acc', hp, tgt, half), P[6]], writes=[('attst', qt)])
                    fw.dma('sp', lambda e: e.dma_start(
                        out=ATT[br, h, :, qt * 512:(qt + 1) * 512], in_=attst[qt][:]),
                        reads=[('attst', qt)], writes=['ATT'])
                pending.append([16 + 6 * qt, fin])
    for pd in pending:
        pd[1]()
    fw.barrier()
    p2.close()
    if STOP_AFTER <= 2:
        return finish(nc, fw, y, glob)

    p3 = ExitStack()
    wg_sb = sb(p3, "wg_sb", [128, 8, 2048], BF16)
    wba_sb = sb(p3, "wba_sb", [64, 8, D], BF16)
    wbb_sb = sb(p3, "wbb_sb", [64, 8, D], BF16)
    wout_sb = sb(p3, "wout_sb", [128, 8, D], BF16)
    for k in range(8):
        fw.dma('pool', lambda e, k=k: e.dma_start(out=wg_sb[:, k, :], in_=w_g[:, k, :]), writes=['wg'])
        fw.dma('pool', lambda e, k=k: e.dma_start(out=wout_sb[:, k, :], in_=wout[:, k, :]), writes=['wout'])
        fw.dma('pool', lambda e, k=k: e.dma_start(out=wba_sb[:, k, :], in_=wba[:, k, :]), writes=['wba'])
        fw.dma('pool', lambda e, k=k: e.dma_start(out=wbb_sb[:, k, :], in_=wbb[:, k, :]), writes=['wbb'])
    xtf3 = [sb(p3, "xtf3_%d" % k, [128, 8, 128], F32) for k in range(2)]
    xg3 = [sb(p3, "xg3_%d" % k, [128, 8, 128], BF16) for k in range(2)]
    att = [sb(p3, "att%d" % k, [64, 16, 128], BF16) for k in range(2)]
    xo = [sb(p3, "xo%d" % k, [128, D], F32) for k in range(2)]
    sgA = sb(p3, "sgA", [128, D], F32)
    sgB = sb(p3, "sgB", [128, D], F32)
    mg = sb(p3, "mg", [128, D], F32)
    mgb = sb(p3, "mgb", [128, D], BF16)
    mT = sb(p3, "mT", [128, 8, 128], BF16)
    hh3 = [sb(p3, "hh3_%d" % k, [128, D], F32) for k in range(2)]
    for i in range(NO):
        s = i % 2
        fw.dma('sp', lambda e: e.dma_start(out=xtf3[s][:], in_=xT[i]), writes=[('xtf3', s)])
        fw.dma('sp', lambda e: e.dma_start(
            out=att[s][:], in_=ATT[:, :, :, i * 128:(i + 1) * 128].rearrange("b h d t -> d (b h) t")),
            reads=['ATT'], writes=[('att', s)])
        fw.dma('sp', lambda e: e.dma_start(out=xo[s][:], in_=xown[i]), writes=[('xo', s)])
        fw.op('dve', lambda e: e.tensor_tensor(
            out=xg3[s][:], in0=xtf3[s][:],
            in1=gmix_sb[:].unsqueeze(2).to_broadcast([128, 8, 128]), op=ALU.mult),
            reads=[('xtf3', s), 'gmix'], writes=[('xg3', s)])
        rs = rstd_all[:, i:i + 1]
        for bnk in range(4):
            for c in range(8):
                fw.op('pe', lambda e, c=c, bnk=bnk: e.matmul(
                    ps[bnk][:], lhsT=xg3[s][:, c, :], rhs=wg_sb[:, c, bnk * 512:(bnk + 1) * 512],
                    start=(c == 0), stop=(c == 7)),
                    reads=[('xg3', s), 'wg'], writes=[P[bnk]])
        for bnk in range(4):
            dst = (sgA if bnk < 2 else sgB)[:, (bnk % 2) * 512:(bnk % 2 + 1) * 512]
            fw.op('act', lambda e, bnk=bnk, dst=dst: e.activation(out=dst, in_=ps[bnk][:], func=AF.Sigmoid, scale=rs),
                  reads=[P[bnk]], writes=[('sg', bnk)])
        for br in range(2):
            wsb = wba_sb if br == 0 else wbb_sb
            for half in range(2):
                bnk = 4 + br * 2 + half
                for h in range(8):
                    fw.op('pe', lambda e, h=h, bnk=bnk, br=br, half=half, wsb=wsb: e.matmul(
                        ps[bnk][:], lhsT=att[s][:, br * 8 + h, :], rhs=wsb[:, h, half * 512:(half + 1) * 512],
                        start=(h == 0), stop=(h == 7)),
                        reads=[('att', s), 'wba', 'wbb'], writes=[P[bnk]])
        for half in range(2):
            sl = slice(half * 512, (half + 1) * 512)
            fw.op('dve', lambda e, half=half, sl=sl: e.tensor_tensor(out=mg[:, sl], in0=sgA[:, sl], in1=ps[4 + half][:], op=ALU.mult),
                  reads=[('sg', half), P[4 + half]], writes=[('mg', half)])
            fw.op('dve', lambda e, half=half, sl=sl: e.tensor_tensor(out=sgB[:, sl], in0=sgB[:, sl], in1=ps[6 + half][:], op=ALU.mult),
                  reads=[('sg', 2 + half), P[6 + half]], writes=[('sg', 2 + half)])
            fw.op('dve', lambda e, half=half, sl=sl: e.tensor_tensor(out=mgb[:, sl], in0=mg[:, sl], in1=sgB[:, sl], op=ALU.add),
                  reads=[('mg', half), ('sg', 2 + half)], writes=[('mgb', half)])
        ptm = ps[0][:].bitcast(BF16).rearrange("p (c t) -> p c t", c=8)
        for c in range(8):
            fw.op('pe', lambda e, c=c: e.transpose(out=ptm[:, c, :], in_=mgb[:, c * 128:(c + 1) * 128], identity=identb[:]),
                  reads=[('mgb', c // 4), 'identb'], writes=[P[0]])
        fw.op('act', lambda e: e.activation(out=mT[:], in_=ptm, func=AF.Copy), reads=[P[0]], writes=['mT'])
        for half in range(2):
            for c in range(8):
                fw.op('pe', lambda e, c=c, half=half: e.matmul(
                    ps[1 + half][:], lhsT=mT[:, c, :], rhs=wout_sb[:, c, half * 512:(half + 1) * 512],
                    start=(c == 0), stop=(c == 7)),
                    reads=['mT', 'wout'], writes=[P[1 + half]])
        for half in range(2):
            sl = slice(half * 512, (half + 1) * 512)
            fw.op('dve', lambda e, half=half, sl=sl: e.tensor_tensor(out=hh3[s][:, sl], in0=xo[s][:, sl], in1=ps[1 + half][:], op=ALU.add),
                  reads=[('xo', s), P[1 + half]], writes=[('hh3', s)])
        fw.dma('sp', lambda e, i=i: e.dma_start(out=HS[i], in_=hh3[s][:]), reads=[('hh3', s)], writes=['HS'])
    fw.barrier()
    p3.close()
    if STOP_AFTER <= 3:
        return finish(nc, fw, y, glob)

    p4 = ExitStack()
    xn2T_all = sb(p4, "xn2T_all", [128, 8, 2048], BF16)
    eps2 = sb(p4, "eps2", [128, 1], F32)
    p4b = ExitStack()
    selT_all = sb(p4b, "selT_all", [128, NO, 3, 128], BF16)
    p41 = ExitStack()
    wpq_sb = sb(p41, "wpq_sb", [128, 8, 2048], BF16)
    skT_sb = sb(p41, "skT_sb", [128, 16, 128], BF16)
    gffn_sb = sb(p41, "gffn_sb", [128, D], F32)
    iota_sb = sb(p41, "iota_sb", [128, 16], F32)
    lo_sb = sb(p41, "lo_sb", [128, 16], F32)
    hi_sb = sb(p41, "hi_sb", [128, 16], F32)
    for k in range(8):
        fw.dma('pool', lambda e, k=k: e.dma_start(out=wpq_sb[:, k, :], in_=wpq[:, k, :]), writes=['wpq'])
    fw.dma('pool', lambda e: e.dma_start(out=skT_sb[:], in_=skT), writes=['skT'])
    fw.dma('sp', lambda e: e.dma_start(out=gffn_sb[:], in_=g_ffn), writes=['gffn'])
    fw.dma('sp', lambda e: e.dma_start(out=iota_sb[:], in_=iota16), writes=['iota'])
    fw.op('dve', lambda e: e.tensor_scalar(out=lo_sb[:], in0=iota_sb[:], scalar1=16.0, scalar2=None, op0=ALU.mult),
          reads=['iota'], writes=['lo'])
    fw.op('dve', lambda e: e.tensor_scalar(out=hi_sb[:], in0=iota_sb[:], scalar1=16.0, scalar2=16.0, op0=ALU.mult, op1=ALU.add),
          reads=['iota'], writes=['hi'])
    hh = [sb(p41, "hh%d" % k, [128, D], F32) for k in range(2)]
    sel3 = sb(p41, "sel3", [128, 3, 128], F32)
    junk = sb(p41, "junk", [128, D], F32)
    ss2 = sb(p41, "ss2", [128, 1], F32)
    rs2 = sb(p41, "rs2", [128, 1], F32)
    xn2b = sb(p41, "xn2b", [128, D], BF16)
    qpb = sb(p41, "qpb", [128, 2048], BF16)
    qpT = sb(p41, "qpT", [128, 16, 128], BF16)
    scw = sb(p41, "scw", [128, 16, 128], F32)
    sv = sb(p41, "sv", [128, 16, 16], F32)
    si = sb(p41, "si", [128, 16, 16], U32)
    sif = sb(p41, "sif", [128, 16, 16], F32)
    cand = sb(p41, "cand", [128, 8, 256], F32)
    candw = sb(p41, "candw", [128, 8, 256], F32)
    best = sb(p41, "best", [128, 8, 16], F32)
    pick = sb(p41, "pick", [128, 8, 16], U32)
    pf = sb(p41, "pf", [128, 8, 16], F32)
    af = sb(p41, "af", [128, 8, 16], F32)
    bfl = sb(p41, "bfl", [128, 8, 16], F32)
    eq = sb(p41, "eq", [128, 8, 16, 16], F32)
    eq2 = sb(p41, "eq2", [128, 8, 16, 16], F32)
    gex = sb(p41, "gex", [128, 8, 16], F32)
    gsum = sb(p41, "gsum", [128, 8], F32)
    B4 = [128, 8, 16, 16]

    def stage_F(i):
        s = i % 2
        B0 = 4 * (i % 2)
        fw.dma('sp', lambda e, i=i: e.dma_start(out=hh[s][:], in_=HS[i]), reads=['HS'], writes=[('hh', s)])
        fw.op('act', lambda e: e.activation(out=junk[:], in_=hh[s][:], func=AF.Square, accum_out=ss2[:]),
              reads=[('hh', s)], writes=['junk', 'ss2'])
        fw.op('act', lambda e: e.activation(out=rs2[:], in_=ss2[:], func=AF.Sqrt, bias=eps_sb[:], scale=1.0 / D),
              reads=['ss2', 'eps_sb'], writes=['rs2'])
        fw.op('dve', lambda e: e.reciprocal(out=rs2[:], in_=rs2[:]), reads=['rs2'], writes=['rs2'])
        fw.op('dve', lambda e: e.scalar_tensor_tensor(out=xn2b[:], in0=hh[s][:], scalar=rs2[:, 0:1], in1=gffn_sb[:],
                                                      op0=ALU.mult, op1=ALU.mult),
              reads=[('hh', s), 'rs2', 'gffn'], writes=['xn2b'])
        ptx = ps[B0][:].bitcast(BF16).rearrange("p (c t) -> p c t", c=8)
        for c in range(8):
            fw.op('pe', lambda e, c=c: e.transpose(out=ptx[:, c, :], in_=xn2b[:, c * 128:(c + 1) * 128], identity=identb[:]),
                  reads=['xn2b', 'identb'], writes=[P[B0]])
        xT_i = xn2T_all[:, :, i * 128:(i + 1) * 128]
        fw.op('act', lambda e: e.activation(out=xT_i, in_=ptx, func=AF.Copy), reads=[P[B0]], writes=[('xn2T', i)])
        QB = [B0 + 1, B0 + 2, B0 + 3, B0]
        for bnk in range(4):
            for c in range(8):
                fw.op('pe', lambda e, c=c, bnk=bnk: e.matmul(
                    ps[QB[bnk]][:], lhsT=xn2T_all[:, c, i * 128:(i + 1) * 128], rhs=wpq_sb[:, c, bnk * 512:(bnk + 1) * 512],
                    start=(c == 0), stop=(c == 7)),
                    reads=[('xn2T', i), 'wpq'], writes=[P[QB[bnk]]])
            fw.op('act', lambda e, bnk=bnk: e.activation(out=qpb[:, bnk * 512:(bnk + 1) * 512], in_=ps[QB[bnk]][:], func=AF.Copy),
                  reads=[P[QB[bnk]]], writes=[('qpb', bnk)])
        for half in range(2):
            ptq = ps[B0 + 1 + half][:].bitcast(BF16).rearrange("p (c t) -> p c t", c=8)
            for c in range(8):
                gidx = half * 8 + c
                fw.op('pe', lambda e, c=c, gidx=gidx, ptq=ptq: e.transpose(
                    out=ptq[:, c, :], in_=qpb[:, gidx * 128:(gidx + 1) * 128], identity=identb[:]),
                    reads=[('qpb', gidx // 4), 'identb'], writes=[P[B0 + 1 + half]])
            fw.op('act', lambda e, ptq=ptq, half=half: e.activation(out=qpT[:, half * 8:(half + 1) * 8, :], in_=ptq, func=AF.Copy),
                  reads=[P[B0 + 1 + half]], writes=[('qpT', half)])
        for gidx in range(16):
            bnk = B0 + gidx // 4
            fw.op('pe', lambda e, gidx=gidx, bnk=bnk: e.matmul(
                ps[bnk][:, (gidx % 4) * 128:(gidx % 4 + 1) * 128], lhsT=qpT[:, gidx, :], rhs=skT_sb[:, gidx, :],
                start=True, stop=True),
                reads=[('qpT', gidx // 8), 'skT'], writes=[P[bnk]])
    def stage_T(i):
        s = i % 2
        B0 = 4 * (i % 2)
        def srcg(gidx):
            return ps[B0 + gidx // 4][:, (gidx % 4) * 128:(gidx % 4 + 1) * 128]
        for gidx in range(16):
            fw.op('dve', lambda e, gidx=gidx: e.max(out=sv[:, gidx, 0:8], in_=srcg(gidx)),
                  reads=[P[B0 + gidx // 4]], writes=[('sv', gidx, 0)])
        for gidx in range(16):
            fw.op('dve', lambda e, gidx=gidx: e.max_index(out=si[:, gidx, 0:8], in_max=sv[:, gidx, 0:8], in_values=srcg(gidx)),
                  reads=[P[B0 + gidx // 4], ('sv', gidx, 0)], writes=[('si', gidx, 0)])
        for gidx in range(16):
            fw.op('dve', lambda e, gidx=gidx: e.match_replace(out=scw[:, gidx, :], in_to_replace=sv[:, gidx, 0:8],
                                                              in_values=srcg(gidx), imm_value=-1e30),
                  reads=[P[B0 + gidx // 4], ('sv', gidx, 0)], writes=[('scw', gidx)])
        for gidx in range(16):
            fw.op('dve', lambda e, gidx=gidx: e.max(out=sv[:, gidx, 8:16], in_=scw[:, gidx, :]),
                  reads=[('scw', gidx)], writes=[('sv', gidx, 1)])
        for gidx in range(16):
            fw.op('dve', lambda e, gidx=gidx: e.max_index(out=si[:, gidx, 8:16], in_max=sv[:, gidx, 8:16], in_values=scw[:, gidx, :]),
                  reads=[('scw', gidx), ('sv', gidx, 1)], writes=[('si', gidx, 1)])
        fw.op('dve', lambda e: e.tensor_copy(out=sif[:], in_=si[:]),
              reads=[('si', g_, k_) for g_ in range(16) for k_ in range(2)], writes=['sif'])
        sv4 = sv[:].rearrange("p (h two) k -> p h two k", two=2)
        sif4 = sif[:].rearrange("p (h two) k -> p h two k", two=2)
        cand4 = cand[:].rearrange("p h (a b) -> p h a b", a=16)
        fw.op('dve', lambda e: e.tensor_tensor(out=cand4, in0=sv4[:, :, 0, :].unsqueeze(3).to_broadcast(B4),
                                               in1=sv4[:, :, 1, :].unsqueeze(2).to_broadcast(B4), op=ALU.add),
              reads=[('sv', g_, k_) for g_ in range(16) for k_ in range(2)], writes=['cand'])
        for h in []:
            fw.op('dve', lambda e, h=h: e.max(out=best[:, h, 0:8], in_=cand[:, h, :]), reads=['cand'], writes=['best'])
            fw.op('dve', lambda e, h=h: e.max_index(out=pick[:, h, 0:8], in_max=best[:, h, 0:8], in_values=cand[:, h, :]),
                  reads=['cand', 'best'], writes=['pick'])
            fw.op('dve', lambda e, h=h: e.match_replace(out=candw[:, h, :], in_to_replace=best[:, h, 0:8],
                                                        in_values=cand[:, h, :], imm_value=-1e30),
                  reads=['cand', 'best'], writes=['candw'])
            fw.op('dve', lambda e, h=h: e.max(out=best[:, h, 8:16], in_=candw[:, h, :]), reads=['candw'], writes=['best'])
            fw.op('dve', lambda e, h=h: e.max_index(out=pick[:, h, 8:16], in_max=best[:, h, 8:16], in_values=candw[:, h, :]),
                  reads=['candw', 'best'], writes=['pick'])
        for h in range(8):
            fw.op('dve', lambda e, h=h: e.max(out=best[:, h, 0:8], in_=cand[:, h, :]), reads=['cand'], writes=[('best', h, 0)])
        for h in range(8):
            fw.op('dve', lambda e, h=h: e.max_index(out=pick[:, h, 0:8], in_max=best[:, h, 0:8], in_values=cand[:, h, :]),
                  reads=['cand', ('best', h, 0)], writes=[('pick', h, 0)])
        for h in range(8):
            fw.op('dve', lambda e, h=h: e.match_replace(out=candw[:, h, :], in_to_replace=best[:, h, 0:8],
                                                        in_values=cand[:, h, :], imm_value=-1e30),
                  reads=['cand', ('best', h, 0)], writes=[('candw', h)])
        for h in range(8):
            fw.op('dve', lambda e, h=h: e.max(out=best[:, h, 8:16], in_=candw[:, h, :]), reads=[('candw', h)], writes=[('best', h, 1)])
        for h in range(8):
            fw.op('dve', lambda e, h=h: e.max_index(out=pick[:, h, 8:16], in_max=best[:, h, 8:16], in_values=candw[:, h, :]),
                  reads=[('candw', h), ('best', h, 1)], writes=[('pick', h, 1)])
        BESTK = [('best', h_, k_) for h_ in range(8) for k_ in range(2)]
        PICKK = [('pick', h_, k_) for h_ in range(8) for k_ in range(2)]
        fw.op('dve', lambda e: e.tensor_copy(out=pf[:], in_=pick[:]), reads=PICKK, writes=['pf'])
        pf4 = pf[:].unsqueeze(3).to_broadcast(B4)
        lo4 = lo_sb[:].unsqueeze(1).unsqueeze(1).to_broadcast(B4)
        hi4 = hi_sb[:].unsqueeze(1).unsqueeze(1).to_broadcast(B4)
        io4 = iota_sb[:].unsqueeze(1).unsqueeze(1).to_broadcast(B4)
        e1v = sel3[:, 0, :].rearrange("p (h k) -> p h k", h=8)
        e2v = sel3[:, 1, :].rearrange("p (h k) -> p h k", h=8)
        gtv = sel3[:, 2, :].rearrange("p (h k) -> p h k", h=8)
        fw.op('dve', lambda e: e.tensor_tensor(out=eq[:], in0=pf4, in1=lo4, op=ALU.is_ge), reads=['pf', 'lo'], writes=['eq'])
        fw.op('dve', lambda e: e.tensor_tensor(out=eq2[:], in0=pf4, in1=hi4, op=ALU.is_lt), reads=['pf', 'hi'], writes=['eq2'])
        fw.op('dve', lambda e: e.tensor_tensor(out=eq[:], in0=eq[:], in1=eq2[:], op=ALU.mult), reads=['eq', 'eq2'], writes=['eq'])
        fw.op('dve', lambda e: e.tensor_tensor(out=eq2[:], in0=eq[:], in1=io4, op=ALU.mult), reads=['eq', 'iota'], writes=['eq2'])
        fw.op('dve', lambda e: e.tensor_reduce(out=af[:], in_=eq2[:], axis=AX.X, op=ALU.add), reads=['eq2'], writes=['af'])
        fw.op('dve', lambda e: e.tensor_tensor(out=eq2[:], in0=eq[:], in1=sif4[:, :, 0, :].unsqueeze(2).to_broadcast(B4), op=ALU.mult),
              reads=['eq', 'sif'], writes=['eq2'])
        fw.op('dve', lambda e: e.tensor_reduce(out=e1v, in_=eq2[:], axis=AX.X, op=ALU.add), reads=['eq2'], writes=['sel3'])
        fw.op('dve', lambda e: e.scalar_tensor_tensor(out=bfl[:], in0=af[:], scalar=-16.0, in1=pf[:], op0=ALU.mult, op1=ALU.add),
              reads=['af', 'pf'], writes=['bfl'])
        fw.op('dve', lambda e: e.tensor_tensor(out=eq[:], in0=bfl[:].unsqueeze(3).to_broadcast(B4), in1=io4, op=ALU.is_equal),
              reads=['bfl', 'iota'], writes=['eq'])
        fw.op('dve', lambda e: e.tensor_tensor(out=eq2[:], in0=eq[:], in1=sif4[:, :, 1, :].unsqueeze(2).to_broadcast(B4), op=ALU.mult),
              reads=['eq', 'sif'], writes=['eq2'])
        fw.op('dve', lambda e: e.tensor_reduce(out=e2v, in_=eq2[:], axis=AX.X, op=ALU.add), reads=['eq2'], writes=['sel3'])
        fw.op('dve', lambda e: e.tensor_tensor(out=gex[:], in0=best[:], in1=best[:, :, 0:1].to_broadcast([128, 8, 16]),
                                               op=ALU.subtract),
              reads=BESTK, writes=['gex'])
        fw.op('act', lambda e: e.activation(out=gex[:], in_=gex[:], func=AF.Exp), reads=['gex'], writes=['gex'])
        fw.op('dve', lambda e: e.tensor_reduce(out=gsum[:], in_=gex[:], axis=AX.X, op=ALU.add), reads=['gex'], writes=['gsum'])
        fw.op('dve', lambda e: e.reciprocal(out=gsum[:], in_=gsum[:]), reads=['gsum'], writes=['gsum'])
        fw.op('dve', lambda e: e.tensor_tensor(out=gtv, in0=gex[:],
                                               in1=gsum[:].unsqueeze(2).to_broadcast([128, 8, 16]), op=ALU.mult),
              reads=['gex', 'gsum'], writes=['sel3'])
        pst = ps[B0][:, 0:384].rearrange("p (a t) -> p a t", a=3)
        for a in range(3):
            fw.op('pe', lambda e, a=a: e.transpose(out=pst[:, a, :], in_=sel3[:, a, :], identity=identf[:]),
                  reads=['sel3', 'identf'], writes=[P[B0]])
        fw.op('act', lambda e, i=i: e.activation(out=selT_all[:, i, :, :], in_=pst, func=AF.Copy),
              reads=[P[B0]], writes=[('selT', i)])
    stage_F(0)
    for i in range(NO):
        if i + 1 < NO:
            stage_F(i + 1)
        stage_T(i)
    fw.barrier()
    p41.close()
    if STOP_AFTER <= 4:
        p4b.close()
        p4.close()
        return finish(nc, fw, y, glob)

    p42 = ExitStack()
    iota128 = sb(p42, "iota128_sb", [128, 128], BF16)
    TB = 32
    Aoh = [sb(p42, "Aoh%d" % k, [128, TB, 128], BF16) for k in range(2)]
    Boh = [sb(p42, "Boh%d" % k, [128, TB, 128], BF16) for k in range(2)]
    Gs = sb(p42, "Gs", [128, 128, 256], BF16)
    fw.dma('pool', lambda e: e.dma_start(out=iota128[:], in_=iota128d), writes=['iota128'])
    nblk = 0
    nev = 0
    for i in range(NO):
        ch = i // 2
        for blk in range(128 // TB):
            ab = nblk % 2
            nblk += 1
            t0 = blk * TB
            io3 = iota128[:].unsqueeze(1).to_broadcast([128, TB, 128])
            fw.op('dve', lambda e: e.tensor_tensor(
                out=Aoh[ab][:], in0=io3, in1=selT_all[:, i, 0, t0:t0 + TB].unsqueeze(2).to_broadcast([128, TB, 128]),
                op=ALU.is_equal), reads=['iota128'], writes=[('Aoh', ab)])
            fw.op('dve', lambda e: e.tensor_tensor(
                out=Aoh[ab][:], in0=Aoh[ab][:], in1=selT_all[:, i, 2, t0:t0 + TB].unsqueeze(2).to_broadcast([128, TB, 128]),
                op=ALU.mult), reads=[('Aoh', ab)], writes=[('Aoh', ab)])
            fw.op('dve', lambda e: e.tensor_tensor(
                out=Boh[ab][:], in0=io3, in1=selT_all[:, i, 1, t0:t0 + TB].unsqueeze(2).to_broadcast([128, TB, 128]),
                op=ALU.is_equal), reads=['iota128'], writes=[('Boh', ab)])
            for q16 in range(TB // 16):
                grp = nev % 2
                nev += 1
                for tt in range(16):
                    t = q16 * 16 + tt
                    fw.op('pe', lambda e, t=t, tt=tt, grp=grp: e.matmul(
                        psall[:, grp * 2048 + tt * 128:grp * 2048 + (tt + 1) * 128],
                        lhsT=Aoh[ab][:, t, :], rhs=Boh[ab][:, t, :], start=True, stop=True),
                        reads=[('Aoh', ab), ('Boh', ab)], writes=[('psg', grp)])
                tg = (i % 2) * 128 + t0 + q16 * 16
                dst = Gs[:, :, tg:tg + 16]
                srcp = psall[:, grp * 2048:(grp + 1) * 2048].rearrange("p (t j) -> p j t", t=16)
                fw.op('act', lambda e, dst=dst, srcp=srcp: e.activation(out=dst, in_=srcp, func=AF.Copy),
                      reads=[('psg', grp)], writes=['Gs'])
        if i % 2 == 1:
            for jb in range(8):
                fw.dma('sp', lambda e, jb=jb, ch=ch: e.dma_start(
                    out=Gscr[jb * 16:(jb + 1) * 16, :, ch * 256:(ch + 1) * 256].rearrange("j i t -> i j t"),
                    in_=Gs[:, jb * 16:(jb + 1) * 16, :]),
                    reads=['Gs'], writes=['Gscr'])
    fw.barrier()
    p42.close()
    p4b.close()
    if STOP_AFTER <= 5:
        p4.close()
        return finish(nc, fw, y, glob)

    p43 = ExitStack()
    JG = 4
    NJG = 128 // JG
    acc = sb(p43, "acc", [128, NO, D], F32)
    gfin_sb = sb(p43, "gfin_sb", [128, D], F32)
    Uj = [sb(p43, "Uj%d" % k, [128, 8, 128], BF16) for k in range(2 * JG)]
    Vj = [sb(p43, "Vj%d" % k, [128, D], BF16) for k in range(2 * JG)]
    Gj = [sb(p43, "Gj%d" % k, [128, 2048], BF16) for k in range(2)]
    gl = [sb(p43, "gl%d" % k, [128, 512], BF16) for k in range(2)]
    AT = [sb(p43, "AT%d" % k, [128, 2048], BF16) for k in range(2 * JG)]
    junk3 = sb(p43, "junk3", [128, D], F32)
    ss3 = sb(p43, "ss3", [128, 1], F32)
    rs3 = sb(p43, "rs3", [128, 1], F32)
    yo = [sb(p43, "yo%d" % k, [128, D], F32) for k in range(2)]
    fw.dma('sp', lambda e: e.dma_start(out=gfin_sb[:], in_=g_fin), writes=['gfin'])
    for i in range(NO):
        fw.dma('sp', lambda e, i=i: e.dma_start(out=acc[:, i, :], in_=HS[i]), reads=['HS'], writes=[('acc', i)])

    cnt = {'h': 0, 'g': 0, 'o': 0}

    def H_units(jg):
        units = []
        for jj in range(JG):
            j = jg * JG + jj
            slot = (jg % 2) * JG + jj

            def load(j=j, slot=slot):
                fw.dma('pool', lambda e: e.dma_start(out=Uj[slot][:], in_=Ur[j]), writes=[('Uj', slot)])
                fw.dma('pool', lambda e: e.dma_start(out=Vj[slot][:], in_=Vr[j]), writes=[('Vj', slot)])
            for tg in range(4):
                def unit(j=j, slot=slot, tg=tg, load=load):
                    if tg == 0:
                        load()
                        gsl = cnt['g'] % 2
                        cnt['g'] += 1
                        fw.dma('sp', lambda e: e.dma_start(out=Gj[gsl][:], in_=Gscr[j]), reads=['Gscr'], writes=[('Gj', gsl)])
                        unit_state['gsl'] = gsl
                    gsl = unit_state['gsl']
                    hb = cnt['h'] % 2
                    cnt['h'] += 1
                    for c in range(8):
                        fw.op('pe', lambda e, c=c: e.matmul(
                            ps[hb][:], lhsT=Uj[slot][:, c, :], rhs=xn2T_all[:, c, tg * 512:(tg + 1) * 512],
                            start=(c == 0), stop=(c == 7)),
                            reads=[('Uj', slot), 'xn2T_all'], writes=[P[hb]])
                    fw.op('act', lambda e: e.activation(out=gl[hb][:], in_=ps[hb][:], func=AF.Gelu),
                          reads=[P[hb]], writes=[('gl', hb)])
                    fw.op('dve', lambda e: e.tensor_tensor(
                        out=AT[slot][:, tg * 512:(tg + 1) * 512], in0=gl[hb][:], in1=Gj[gsl][:, tg * 512:(tg + 1) * 512],
                        op=ALU.mult),
                        reads=[('gl', hb), ('Gj', gsl)], writes=[('AT', slot, tg)])
                units.append(unit)
        return units

    unit_state = {}

    def V_units(jg):
        units = []
        for tile in range(NO):
            for half in range(2):
                def unit(tile=tile, half=half):
                    ob = 2 + (cnt['o'] % 6)
                    cnt['o'] += 1
                    for jj in range(JG):
                        slot = (jg % 2) * JG + jj
                        fw.op('pe', lambda e, jj=jj, slot=slot: e.matmul(
                            ps[ob][:], lhsT=AT[slot][:, tile * 128:(tile + 1) * 128],
                            rhs=Vj[slot][:, half * 512:(half + 1) * 512], start=(jj == 0), stop=(jj == JG - 1)),
                            reads=[('AT', slot, tile // 4), ('Vj', slot)], writes=[P[ob]])
                    dst = acc[:, tile, half * 512:(half + 1) * 512]
                    fw.op('dve', lambda e, dst=dst, ob=ob: e.tensor_tensor(out=dst, in0=dst, in1=ps[ob][:], op=ALU.add),
                          reads=[P[ob], ('acc', tile)], writes=[('acc', tile)])
                units.append(unit)
        return units

    for u in H_units(0):
        u()
    for jg in range(NJG):
        hu = H_units(jg + 1) if jg + 1 < NJG else []
        vu = V_units(jg)
        hi_ = 0
        for k, v in enumerate(vu):
            if k % 2 == 0 and hi_ < len(hu):
                hu[hi_]()
                hi_ += 1
            v()
        while hi_ < len(hu):
            hu[hi_]()
            hi_ += 1
    for i in range(NO):
        s = i % 2
        fw.op('act', lambda e, i=i: e.activation(out=junk3[:], in_=acc[:, i, :], func=AF.Square, accum_out=ss3[:]),
              reads=[('acc', i)], writes=['junk3', 'ss3'])
        fw.op('act', lambda e: e.activation(out=rs3[:], in_=ss3[:], func=AF.Sqrt, bias=eps_sb[:], scale=1.0 / D),
              reads=['ss3', 'eps_sb'], writes=['rs3'])
        fw.op('dve', lambda e: e.reciprocal(out=rs3[:], in_=rs3[:]), reads=['rs3'], writes=['rs3'])
        fw.op('dve', lambda e, i=i: e.scalar_tensor_tensor(out=yo[s][:], in0=acc[:, i, :], scalar=rs3[:, 0:1], in1=gfin_sb[:],
                                                           op0=ALU.mult, op1=ALU.mult),
              reads=[('acc', i), 'rs3', 'gfin'], writes=[('yo', s)])
        fw.dma('sp', lambda e, i=i: e.dma_start(out=y[i], in_=yo[s][:]), reads=[('yo', s)], writes=['y'])
    fw.barrier()
    p43.close()
    p4.close()
    return finish(nc, fw, y, glob)


def finish(nc, fw, y, glob):
    fw.barrier()
    glob.close()
    return nc


def prep(inputs):
    x = np.asarray(inputs["x"], np.float32)
    w_in = np.asarray(inputs["w_in"], np.float32)[0]

    def pc(w):
        return np.ascontiguousarray(w.reshape(8, 128, -1).transpose(1, 0, 2))

    cols_kv = np.r_[512:1024, 1024:1536, 2048:2560, 2560:3072, 3072:3080]
    cols_q = np.r_[0:512, 1536:2048]
    cols_g = np.r_[3080:5128]
    shared = {
        "w_kv": pc(w_in[:, cols_kv]),
        "w_q": pc(w_in[:, cols_q]),
        "w_g": pc(w_in[:, cols_g]),
        "g_mix": np.ascontiguousarray(np.asarray(inputs["norm_mix_g"], np.float32)[0].reshape(8, 128).T),
        "bfg": np.ascontiguousarray(np.broadcast_to(np.asarray(inputs["b_forget"], np.float32)[0][None, :], (128, 8))),
        "wba": np.ascontiguousarray(np.asarray(inputs["w_branch_a"], np.float32)[0].reshape(8, 64, D).transpose(1, 0, 2)),
        "wbb": np.ascontiguousarray(np.asarray(inputs["w_branch_b"], np.float32)[0].reshape(8, 64, D).transpose(1, 0, 2)),
        "wout": pc(np.asarray(inputs["w_out"], np.float32)[0]),
        "g_ffn": np.ascontiguousarray(np.broadcast_to(np.asarray(inputs["norm_ffn_g"], np.float32)[0][None, :], (128, D))),
        "wpq": pc(np.asarray(inputs["w_peer_q"], np.float32)[0]),
        "skT": np.ascontiguousarray(np.asarray(inputs["peer_sub_keys"], np.float32)[0].reshape(16, 128, 128).transpose(2, 0, 1)),
        "Ur": np.ascontiguousarray(np.asarray(inputs["peer_expert_u"], np.float32)[0].reshape(128, 128, 8, 128).transpose(1, 3, 2, 0)),
        "Vr": np.ascontiguousarray(np.asarray(inputs["peer_expert_v"], np.float32)[0].reshape(128, 128, D).transpose(1, 0, 2)),
        "iota128": np.ascontiguousarray(np.broadcast_to(np.arange(128, dtype=np.float32)[None, :], (128, 128))),
        "g_fin": np.ascontiguousarray(np.broadcast_to(np.asarray(inputs["norm_final_g"], np.float32)[None, :], (128, D))),
    }
    s_ = np.arange(128)[:, None]
    t_ = np.arange(512)[None, :]
    trim = np.stack([(o * 128 + s_ <= t_) for o in range(4)], axis=1).astype(np.float32)
    shared["trim"] = np.ascontiguousarray(trim)
    shared["tric"] = (np.arange(128)[:, None] <= np.arange(128)[None, :]).astype(np.float32)
    shared["onehot"] = (np.arange(32)[:, None] == (np.arange(8192)[None, :] // 256)).astype(np.float32)
    shared["iota16"] = np.ascontiguousarray(np.broadcast_to(np.arange(16, dtype=np.float32)[None, :], (128, 16)))
    half = 8
    inv_freq = np.power(np.float32(500000.0), -np.arange(half, dtype=np.float32) / np.float32(half)).astype(np.float32)
    maps = []
    for c in range(8):
        b = c // 4
        order = chunk_order(c)
        tok = np.concatenate([np.arange(k * 1024, (k + 1) * 1024) for k in order])
        xb = x[b][tok]
        m = dict(shared)
        m["xT"] = np.ascontiguousarray(xb.reshape(NT, 128, 8, 128).transpose(0, 3, 2, 1))
        m["xown"] = np.ascontiguousarray(xb[:2048].reshape(NO, 128, D))
        ang = tok.astype(np.float32)[:, None] * inv_freq[None, :]
        m["ropec"] = np.ascontiguousarray(np.cos(ang).astype(np.float32).reshape(NT, 128, 8).transpose(1, 0, 2))
        m["ropes"] = np.ascontiguousarray(np.sin(ang).astype(np.float32).reshape(NT, 128, 8).transpose(1, 0, 2))
        true_blk = np.array([order[n // 4] * 4 + n % 4 for n in range(32)])
        true_tile = np.array([order[n // 8] * 8 + n % 8 for n in range(NT)])
        pastb = np.zeros((NO, 32), np.float32)
        ownm = np.zeros((NO, 32), np.float32)
        units = core_units(c)
        slot_sel = np.array([-1, -1] + [q for (q, _) in units])
        for i in range(NO):
            qb = true_blk[i // 2]
            qsel = i // 8
            slot_of_blk = np.arange(32) // 4
            allowed = (slot_of_blk == 0) | ((slot_of_blk == 1) & (qsel == 1)) | (slot_sel[slot_of_blk] == qsel)
            pastb[i] = np.where((true_blk < qb) & allowed, 0.0, -1e30)
            ownm[i, i // 2] = 1.0
        uw = np.zeros((6, 2), np.float32)
        for u, (q, _) in enumerate(units):
            uw[u, q] = 1.0
        m["uw"] = np.ascontiguousarray(np.broadcast_to(uw[None], (128, 6, 2)))
        m["pastb"] = np.ascontiguousarray(np.broadcast_to(pastb[None], (128, NO, 32)))
        m["ownm"] = np.ascontiguousarray(np.broadcast_to(ownm[None], (128, NO, 32)))
        chunk_of_tile = np.array([order[n // 8] for n in range(NT)])
        visA = np.zeros(NT, np.float32)
        visB = np.zeros(NT, np.float32)
        m["visA"] = np.ascontiguousarray(np.broadcast_to(visA[None], (128, NT)))
        m["visB"] = np.ascontiguousarray(np.broadcast_to(visB[None], (128, NT)))
        first_occ = np.array([n == int(np.argmax(true_tile == true_tile[n])) for n in range(NT)])
        m["Mord"] = ((true_tile[:, None] < true_tile[None, :]) & first_occ[:, None]).astype(np.float32)
        maps.append(m)
    return maps


_NC = None


def kernel(**inputs):
    global _NC
    maps = prep(inputs)
    if _NC is None:
        _NC = build()
    res = run_bass_kernel_spmd(_NC, maps, core_ids=list(range(8)))
    out = np.zeros((2, 8192, D), np.float32)
    for c in range(8):
        b = c // 4
        order = chunk_order(c)
        yc = np.asarray(res.results[c]["y"]).reshape(2048, D)
        out[b, order[0] * 1024:(order[0] + 1) * 1024] = yc[:1024]
        out[b, order[1] * 1024:(order[1] + 1) * 1024] = yc[1024:]
    return out
```

```python
import numpy as np
from contextlib import ExitStack
import concourse.bass as bass
import concourse.mybir as mybir
from concourse.bass_utils import run_bass_kernel_spmd

F32 = mybir.dt.float32
BF16 = mybir.dt.bfloat16
U32 = mybir.dt.uint32
I32 = mybir.dt.int32
AF = mybir.ActivationFunctionType
ALU = mybir.AluOpType
AX = mybir.AxisListType

NDS = 48
SAME_ENG_WAIT = True
NEG = -30000.0
FILLER = False
DEBUG = False
STOP_AFTER = 99


class FW:
    def __init__(self, nc):
        self.nc = nc
        self.E = {'pe': nc.tensor, 'act': nc.scalar, 'dve': nc.vector,
                  'pool': nc.gpsimd, 'sp': nc.sync}
        self.sems = {}
        for e in self.E:
            self.sems['c' + e] = nc.alloc_semaphore(name='c_' + e)
        for i in range(NDS):
            self.sems['d%d' % i] = nc.alloc_semaphore(name='d_%d' % i)
        self.val = {k: 0 for k in self.sems}
        self.seen = {e: {} for e in self.E}
        self.res = {}
        self.dnext = {}
        self.ninst = {e: 0 for e in self.E}

    def _wait(self, eng, toks):
        need = {}
        for t in toks:
            if t is None:
                continue
            s, v = t
            if s == 'c' + eng and (eng == 'pe' or not SAME_ENG_WAIT):
                continue
            if v > need.get(s, 0):
                need[s] = v
        for s, v in need.items():
            if v > self.seen[eng].get(s, 0):
                self.E[eng].wait_ge(self.sems[s], v)
                self.seen[eng][s] = v

    def _deps(self, reads, writes):
        toks = []
        for r in reads:
            st = self.res.get(r)
            if st:
                toks.append(st['w'])
        for w in writes:
            st = self.res.get(w)
            if st:
                toks.append(st['w'])
                toks.extend(st['r'].values())
        return toks

    def _commit(self, tok, reads, writes):
        for r in reads:
            st = self.res.setdefault(r, {'w': None, 'r': {}})
            st['r'][tok[0]] = tok
        for w in writes:
            self.res[w] = {'w': tok, 'r': {}}

    def prewait(self, eng, reads=(), writes=()):
        self._wait(eng, self._deps(reads, writes))

    def op(self, eng, fn, reads=(), writes=()):
        self._wait(eng, self._deps(reads, writes))
        inst = fn(self.E[eng])
        s = 'c' + eng
        self.val[s] += 1
        inst.then_inc(self.sems[s], 1)
        self.ninst[eng] += 1
        self._commit((s, self.val[s]), reads, writes)
        return inst

    def dma(self, eng, fn, reads=(), writes=()):
        half = NDS // 2
        k = self.dnext.get(eng, 0)
        self.dnext[eng] = (k + 1) % half
        i = k + (half if eng == 'pool' else 0)
        s = 'd%d' % i
        toks = self._deps(reads, writes)
        toks.append((s, self.val[s]) if self.val[s] else None)
        self._wait(eng, toks)
        inst = fn(self.E[eng])
        self.val[s] += 16
        inst.then_inc(self.sems[s], 16)
        self.ninst[eng] += 1
        self._commit((s, self.val[s]), reads, writes)
        return inst

    def barrier(self):
        toks = [(s, v) for s, v in self.val.items() if v]
        for e in self.E:
            for s, v in toks:
                if s == 'c' + e:
                    continue
                if v > self.seen[e].get(s, 0):
                    self.E[e].wait_ge(self.sems[s], v)
                    self.seen[e][s] = v
        self.res = {}


NT = 64
NO = 16
D = 1024


def core_units(c):
    j = c % 4
    return [(0, k) for k in range(8) if k < j] + [(1, k) for k in range(8) if k < 7 - j and k != j]


def chunk_order(c):
    j = c % 4
    return [j, 7 - j] + [k for (_, k) in core_units(c)]


def build():
    nc = bass.Bass("TRN2", target_bir_lowering=False)
    fw = FW(nc)

    def din(name, shape, dt=F32):
        return nc.dram_tensor(name, list(shape), dt, kind="ExternalInput").ap()

    def dscr(name, shape, dt):
        return nc.dram_tensor(name, list(shape), dt,
                              kind="ExternalOutput" if DEBUG else "Internal").ap()

    xT = din("xT", [NT, 128, 8, 128])
    xown = din("xown", [NO, 128, D])
    w_kv = din("w_kv", [128, 8, 2056])
    w_q = din("w_q", [128, 8, 1024])
    w_g = din("w_g", [128, 8, 2048])
    g_mix = din("g_mix", [128, 8])
    bfg = din("bfg", [128, 8])
    ropec = din("ropec", [128, NT, 8])
    ropes = din("ropes", [128, NT, 8])
    wba = din("wba", [64, 8, D])
    wbb = din("wbb", [64, 8, D])
    wout = din("wout", [128, 8, D])
    g_ffn = din("g_ffn", [128, D])
    wpq = din("wpq", [128, 8, 2048])
    skT = din("skT", [128, 16, 128])
    Ur = din("Ur", [128, 128, 8, 128])
    Vr = din("Vr", [128, 128, D])
    iota128d = din("iota128", [128, 128])
    g_fin = din("g_fin", [128, D])
    trim = din("trim", [128, 4, 512])
    tric = din("tric", [128, 128])
    onehot = din("onehot", [32, 8192])
    pastb = din("pastb", [128, NO, 32])
    ownm = din("ownm", [128, NO, 32])
    visA = din("visA", [128, NT])
    visB = din("visB", [128, NT])
    Mord = din("Mord", [64, 64])
    iota16 = din("iota16", [128, 16])
    uwd = din("uw", [128, 6, 2])
    y = nc.dram_tensor("y", [NO, 128, D], F32, kind="ExternalOutput").ap()

    KTA = dscr("KTA", [8, 64, 8192], BF16)
    KTB = dscr("KTB", [8, 64, 8192], BF16)
    VA = dscr("VA", [8, 128, NT, 65], BF16)
    VB = dscr("VB", [8, 128, NT, 65], BF16)
    QTA = dscr("QTA", [8, 96, 2048], BF16)
    QTB = dscr("QTB", [8, 65, 2048], BF16)
    ATT = dscr("ATT", [2, 8, 64, 2048], BF16)
    HS = dscr("HS", [NO, 128, D], F32)
    Gscr = nc.dram_tensor("Gscr", [128, 128, 2048], BF16, kind="Internal").ap()

    psall = nc.alloc_psum_tensor("psall", [128, 4096], F32)
    ps = [psall[:, i * 512:(i + 1) * 512] for i in range(8)]
    P = ['ps%d' % i for i in range(8)]

    glob = ExitStack()

    def sb(stack, name, shape, dt):
        return stack.enter_context(nc.sbuf_tensor(name, list(shape), dt))

    identf = sb(glob, "identf", [128, 128], F32)
    identb = sb(glob, "identb", [128, 128], BF16)
    ones_f = sb(glob, "ones_f", [128, 128], F32)
    rstd_all = sb(glob, "rstd_all", [128, NT], F32)
    gmix_sb = sb(glob, "gmix_sb", [128, 8], F32)
    negcA = sb(glob, "negcA", [128, NT, 8], F32)
    negcB = sb(glob, "negcB", [128, NT, 8], F32)
    eps_sb = sb(glob, "eps_sb", [128, 1], F32)

    fw.op('pool', lambda e: e.memset(identf[:], 0.0), writes=['identf'])
    fw.op('pool', lambda e: e.affine_select(out=identf[:], in_=identf[:], pattern=[[-1, 128]],
                                            compare_op=ALU.not_equal, fill=1.0, base=0,
                                            channel_multiplier=1), reads=['identf'], writes=['identf'])
    fw.op('dve', lambda e: e.tensor_copy(out=identb[:], in_=identf[:]), reads=['identf'], writes=['identb'])
    fw.op('dve', lambda e: e.memset(ones_f[:], 1.0), writes=['ones_f'])
    fw.op('dve', lambda e: e.memset(eps_sb[:], 1e-6), writes=['eps_sb'])
    fw.dma('sp', lambda e: e.dma_start(out=gmix_sb[:], in_=g_mix), writes=['gmix'])

    def load_xg(i, xtf, xg, slot):
        fw.dma('sp', lambda e: e.dma_start(out=xtf[slot][:], in_=xT[i]), writes=[('xtf', slot)])
        fw.op('dve', lambda e: e.tensor_tensor(
            out=xg[slot][:], in0=xtf[slot][:],
            in1=gmix_sb[:].unsqueeze(2).to_broadcast([128, 8, 128]), op=ALU.mult),
            reads=[('xtf', slot), 'gmix'], writes=[('xg', slot)])

    def rope(src3, i, tmp, key):
        x1 = src3[:, :, 0:8]
        x2 = src3[:, :, 8:16]
        cs = ropec_sb[:, i, :].unsqueeze(1).to_broadcast([128, 8, 8])
        sn = ropes_sb[:, i, :].unsqueeze(1).to_broadcast([128, 8, 8])
        rk = [key, 'rope_tab']
        fw.op('pool', lambda e: e.tensor_tensor(out=tmp[:, 0], in0=x1, in1=cs, op=ALU.mult), reads=rk, writes=['rt0'])
        fw.op('pool', lambda e: e.tensor_tensor(out=tmp[:, 1], in0=x2, in1=sn, op=ALU.mult), reads=rk, writes=['rt1'])
        fw.op('pool', lambda e: e.tensor_tensor(out=tmp[:, 2], in0=x1, in1=sn, op=ALU.mult), reads=rk, writes=['rt2'])
        fw.op('pool', lambda e: e.tensor_tensor(out=tmp[:, 3], in0=x2, in1=cs, op=ALU.mult), reads=rk, writes=['rt3'])
        fw.op('pool', lambda e: e.tensor_tensor(out=x1, in0=tmp[:, 0], in1=tmp[:, 1], op=ALU.subtract),
              reads=['rt0', 'rt1'], writes=[key])
        fw.op('pool', lambda e: e.tensor_tensor(out=x2, in0=tmp[:, 2], in1=tmp[:, 3], op=ALU.add),
              reads=['rt2', 'rt3'], writes=[key])

    p1 = ExitStack()
    p1a = ExitStack()
    ropec_sb = sb(p1, "ropec_sb", [128, NT, 8], F32)
    ropes_sb = sb(p1, "ropes_sb", [128, NT, 8], F32)
    xtf = [sb(p1, "xtf%d" % k, [128, 8, 128], F32) for k in range(2)]
    xg = [sb(p1, "xg%d" % k, [128, 8, 128], BF16) for k in range(2)]
    rtmp = sb(p1, "rtmp", [128, 4, 8, 8], F32)
    kmT = sb(p1, "kmT", [64, 8, 32], BF16)
    negc = sb(p1, "negc", [128, NT, 8], F32)
    wkv_sb = sb(p1a, "wkv_sb", [128, 8, 2056], BF16)
    sq = sb(p1a, "sq", [128, 8, 128], BF16)
    ones_b = sb(p1a, "ones_b", [128, 1], BF16)
    ssb = sb(p1a, "ssb", [128, 1], F32)
    kf = sb(p1a, "kf", [128, 8, 64], F32)
    kab = [sb(p1a, "kab%d" % k, [128, 8, 64], BF16) for k in range(2)]
    kbb = [sb(p1a, "kbb%d" % k, [128, 8, 64], BF16) for k in range(2)]
    KV_BACK = [None]
    stKA = [sb(p1a, "stKA%d" % k, [64, 8, 512], BF16) for k in range(2)]
    stKB = [sb(p1a, "stKB%d" % k, [64, 8, 512], BF16) for k in range(2)]
    stVA = [sb(p1a, "stVA%d" % k, [128, 8, 8, 65], BF16) for k in range(2)]
    stVB = [sb(p1a, "stVB%d" % k, [128, 8, 8, 65], BF16) for k in range(2)]
    fl_all = sb(p1a, "fl_all", [128, NT, 8], F32)
    ksum = sb(p1a, "ksum", [64, 8, NT], F32)

    for k in range(8):
        fw.dma('pool', lambda e, k=k: e.dma_start(out=wkv_sb[:, k, :], in_=w_kv[:, k, :]), writes=['wkv'])
    fw.dma('sp', lambda e: e.dma_start(out=ropec_sb[:], in_=ropec), writes=['rope_tab'])
    fw.dma('sp', lambda e: e.dma_start(out=ropes_sb[:], in_=ropes), writes=['rope_tab'])
    fw.op('pool', lambda e: e.memset(ones_b[:], 1.0), writes=['ones_b'])
    for k in range(2):
        fw.op('pool', lambda e, k=k: e.memset(stVA[k][:], 1.0), writes=[('stVA', k)])
        fw.op('pool', lambda e, k=k: e.memset(stVB[k][:], 1.0), writes=[('stVB', k)])

    def kv_back(i):
        g4 = (i // 4) % 2
        kb2 = i % 2
        ptA = ps[6][0:64, :].bitcast(BF16).rearrange("p (h t) -> p h t", h=8)
        ptB = ps[7][0:64, :].bitcast(BF16).rearrange("p (h t) -> p h t", h=8)
        for h in range(8):
            fw.op('pe', lambda e, h=h: e.transpose(out=ptA[:, h, :], in_=kab[kb2][:, h, :], identity=identb[:]),
                  reads=[('kab', kb2), 'identb'], writes=[P[6]])
        fw.op('dve', lambda e: e.tensor_copy(out=stKA[g4][:, :, (i % 4) * 128:(i % 4 + 1) * 128], in_=ptA),
              reads=[P[6]], writes=[('stKA', g4)])
        fw.op('dve', lambda e: e.tensor_reduce(out=ksum[:, :, i], in_=ptA, axis=AX.X, op=ALU.add),
              reads=[P[6]], writes=['ksum'])
        for h in range(8):
            fw.op('pe', lambda e, h=h: e.transpose(out=ptB[:, h, :], in_=kbb[kb2][:, h, :], identity=identb[:]),
                  reads=[('kbb', kb2), 'identb'], writes=[P[7]])
        fw.op('dve', lambda e: e.tensor_copy(out=stKB[g4][:, :, (i % 4) * 128:(i % 4 + 1) * 128], in_=ptB),
              reads=[P[7]], writes=[('stKB', g4)])
        if i % 4 == 3:
            g = i // 4
            fw.dma('sp', lambda e, g=g: e.dma_start(
                out=KTA[:, :, g * 512:(g + 1) * 512].rearrange("h d s -> d h s"), in_=stKA[g4][:]),
                reads=[('stKA', g4)], writes=['KTA'])
            fw.dma('sp', lambda e, g=g: e.dma_start(
                out=KTB[:, :, g * 512:(g + 1) * 512].rearrange("h d s -> d h s"), in_=stKB[g4][:]),
                reads=[('stKB', g4)], writes=['KTB'])

    KV_BACK[0] = kv_back
    for i in range(NT):
        s = i % 2
        load_xg(i, xtf, xg, s)
        if i >= 1:
            KV_BACK[0](i - 1)
        fw.op('act', lambda e: e.activation(out=sq[:], in_=xtf[s][:], func=AF.Square),
              reads=[('xtf', s)], writes=['sq'])
        for c in range(8):
            fw.op('pe', lambda e, c=c: e.matmul(ps[5][:, 0:1], lhsT=sq[:, c, :], rhs=ones_b[:, 0:1],
                                                 start=(c == 0), stop=(c == 7)),
                  reads=['sq', 'ones_b'], writes=[P[5]])
        fw.op('act', lambda e: e.activation(out=ssb[:], in_=ps[5][:, 0:1], func=AF.Sqrt,
                                            bias=eps_sb[:], scale=1.0 / D),
              reads=[P[5], 'eps_sb'], writes=['ssb'])
        fw.op('dve', lambda e: e.reciprocal(out=rstd_all[:, i:i + 1], in_=ssb[:]),
              reads=['ssb'], writes=[('rstd', i)])
        rs = rstd_all[:, i:i + 1]
        for bnk, (c0, c1) in enumerate([(0, 512), (512, 1024), (1024, 1536), (1536, 2048), (2048, 2056)]):
            for c in range(8):
                fw.op('pe', lambda e, c=c, bnk=bnk, c0=c0, c1=c1: e.matmul(
                    ps[bnk][:, 0:c1 - c0], lhsT=xg[s][:, c, :], rhs=wkv_sb[:, c, c0:c1],
                    start=(c == 0), stop=(c == 7)),
                    reads=[('xg', s), 'wkv'], writes=[P[bnk]])
        g8 = (i // 8) % 2
        kb2 = i % 2
        fw.op('act', lambda e: e.activation(out=kf[:].rearrange("p h d -> p (h d)"), in_=ps[0][:],
                                            func=AF.Copy, scale=rs),
              reads=[P[0], ('rstd', i)], writes=['kf'])
        rope(kf[:], i, rtmp, 'kf')
        fw.op('pool', lambda e: e.tensor_copy(out=kab[kb2][:], in_=kf[:]), reads=['kf'], writes=[('kab', kb2)])
        fw.op('dve', lambda e: e.tensor_scalar(
            out=stVA[g8][:, :, i % 8, 0:64], in0=ps[1][:].rearrange("p (h d) -> p h d", h=8),
            scalar1=rs, scalar2=None, op0=ALU.mult),
            reads=[P[1], ('rstd', i)], writes=[('stVA', g8)])
        fw.op('act', lambda e: e.activation(out=kbb[kb2][:].rearrange("p h d -> p (h d)"), in_=ps[2][:],
                                            func=AF.Copy, scale=rs),
              reads=[P[2], ('rstd', i)], writes=[('kbb', kb2)])
        fw.op('act', lambda e: e.activation(
            out=stVB[g8][:, :, i % 8, 0:64], in_=ps[3][:].rearrange("p (h d) -> p h d", h=8),
            func=AF.Copy, scale=rs),
            reads=[P[3], ('rstd', i)], writes=[('stVB', g8)])
        fw.op('dve', lambda e: e.tensor_scalar(out=fl_all[:, i, :], in0=ps[4][:, 0:8], scalar1=rs,
                                               scalar2=None, op0=ALU.mult),
              reads=[P[4], ('rstd', i)], writes=['fl_all'])
        if i % 8 == 7:
            g = i // 8
            fw.dma('sp', lambda e, g=g: e.dma_start(
                out=VA[:, :, g * 8:(g + 1) * 8, :].rearrange("h t i e -> t h i e"), in_=stVA[g8][:]),
                reads=[('stVA', g8)], writes=['VA'])
            fw.dma('sp', lambda e, g=g: e.dma_start(
                out=VB[:, :, g * 8:(g + 1) * 8, :].rearrange("h t i e -> t h i e"), in_=stVB[g8][:]),
                reads=[('stVB', g8)], writes=['VB'])

    kv_back(NT - 1)

    kv = ksum[:].rearrange("d h (n two) -> d h n two", two=2)
    ksum2 = sb(p1a, "ksum2", [64, 8, 32], F32)
    fw.op('dve', lambda e: e.tensor_tensor(out=ksum2[:], in0=kv[:, :, :, 0], in1=kv[:, :, :, 1], op=ALU.add),
          reads=['ksum'], writes=['ksum2'])
    fw.op('dve', lambda e: e.tensor_scalar(out=kmT[:], in0=ksum2[:], scalar1=1.0 / 256, scalar2=None, op0=ALU.mult),
          reads=['ksum2'], writes=['kmT'])

    bf_sb = sb(p1a, "bf_sb", [128, 8], F32)
    tri_sb = sb(p1a, "tri_sb", [128, 128], F32)
    M_sb = sb(p1a, "M_sb", [64, 64], F32)
    visA_sb = sb(p1a, "visA_sb", [128, NT], F32)
    visB_sb = sb(p1a, "visB_sb", [128, NT], F32)
    Lt = sb(p1a, "Lt", [128, NT, 8], F32)
    T2 = sb(p1a, "T2", [64, 8], F32)
    R = sb(p1a, "R", [64, NT, 8], F32)
    fw.dma('sp', lambda e: e.dma_start(out=bf_sb[:], in_=bfg), writes=['bf_sb'])
    fw.dma('sp', lambda e: e.dma_start(out=tri_sb[:], in_=tric), writes=['tri_sb'])
    fw.dma('sp', lambda e: e.dma_start(out=M_sb[:], in_=Mord), writes=['M_sb'])
    fw.dma('sp', lambda e: e.dma_start(out=visA_sb[:], in_=visA), writes=['visA_sb'])
    fw.dma('sp', lambda e: e.dma_start(out=visB_sb[:], in_=visB), writes=['visB_sb'])
    fw.op('dve', lambda e: e.tensor_tensor(out=Lt[:], in0=fl_all[:],
                                           in1=bf_sb[:].unsqueeze(1).to_broadcast([128, NT, 8]), op=ALU.add),
          reads=['fl_all', 'bf_sb'], writes=['Lt'])
    Lt2 = Lt[:].rearrange("p j h -> p (j h)")
    fw.op('act', lambda e: e.activation(out=Lt2, in_=Lt2, func=AF.Exp, scale=-1.0), reads=['Lt'], writes=['Lt'])
    fw.op('act', lambda e: e.activation(out=Lt2, in_=Lt2, func=AF.Ln, bias=ones_f[:, 0:1], scale=1.0),
          reads=['Lt', 'ones_f'], writes=['Lt'])
    for h in range(8):
        fw.op('pe', lambda e, h=h: e.matmul(ps[1][0:64, h:h + 1], lhsT=Lt[:, :, h], rhs=ones_f[:, 0:1],
                                             start=True, stop=True),
              reads=['Lt', 'ones_f'], writes=[P[1]])
    fw.op('dve', lambda e: e.tensor_copy(out=T2[:], in_=ps[1][0:64, 0:8]), reads=[P[1]], writes=['T2'])
    fw.op('dve', lambda e: e.tensor_tensor(out=R[:], in0=M_sb[:].unsqueeze(2).to_broadcast([64, NT, 8]),
                                           in1=T2[:].unsqueeze(1).to_broadcast([64, NT, 8]), op=ALU.mult),
          reads=['M_sb', 'T2'], writes=['R'])
    fw.op('pe', lambda e: e.matmul(ps[0][:], lhsT=tri_sb[:], rhs=Lt2, start=True, stop=False),
          reads=['tri_sb', 'Lt'], writes=[P[0]])
    fw.op('pe', lambda e: e.matmul(ps[0][:], lhsT=ones_f[0:64, :], rhs=R[:].rearrange("p j h -> p (j h)"),
                                   start=False, stop=True),
          reads=['ones_f', 'R'], writes=[P[0]])
    fw.op('dve', lambda e: e.tensor_copy(out=negc[:].rearrange("p j h -> p (j h)"), in_=ps[0][:]),
          reads=[P[0]], writes=['negc'])
    fw.op('dve', lambda e: e.tensor_tensor(out=negcA[:], in0=negc[:],
                                           in1=visA_sb[:].unsqueeze(2).to_broadcast([128, NT, 8]), op=ALU.add),
          reads=['negc', 'visA_sb'], writes=['negcA'])
    fw.op('dve', lambda e: e.tensor_tensor(out=negcB[:], in0=negc[:],
                                           in1=visB_sb[:].unsqueeze(2).to_broadcast([128, NT, 8]), op=ALU.add),
          reads=['negc', 'visB_sb'], writes=['negcB'])

    fw.barrier()
    p1a.close()
    wq_sb = sb(p1, "wq_sb", [128, 8, 1024], BF16)
    for k in range(8):
        fw.dma('pool', lambda e, k=k: e.dma_start(out=wq_sb[:, k, :], in_=w_q[:, k, :]), writes=['wq'])
    pastb_sb = sb(p1, "pastb_sb", [128, NO, 32], F32)
    ownm_sb = sb(p1, "ownm_sb", [128, NO, 32], F32)
    fw.dma('sp', lambda e: e.dma_start(out=pastb_sb[:], in_=pastb), writes=['pastb_sb'])
    fw.dma('sp', lambda e: e.dma_start(out=ownm_sb[:], in_=ownm), writes=['ownm_sb'])
    qf = sb(p1, "qf", [128, 8, 64], F32)
    qaA = [sb(p1, "qaA%d" % k, [128, 8, 96], BF16) for k in range(2)]
    qaB = [sb(p1, "qaB%d" % k, [128, 8, 65], BF16) for k in range(2)]
    qT = sb(p1, "qT", [64, 8, 128], BF16)
    gm = sb(p1, "gm", [128, 8, 32], F32)
    m8 = sb(p1, "m8", [128, 8, 8], F32)
    thr = sb(p1, "thr", [128, 8], F32)
    sel = sb(p1, "sel", [128, 8, 32], F32)
    stQA = [sb(p1, "stQA%d" % k, [96, 8, 512], BF16) for k in range(2)]
    stQB = [sb(p1, "stQB%d" % k, [65, 8, 512], BF16) for k in range(2)]

    def q_front(i):
        s = i % 2
        load_xg(i, xtf, xg, s)
        rs = rstd_all[:, i:i + 1]
        for bnk, (c0, c1) in enumerate([(0, 512), (512, 1024)]):
            for c in range(8):
                fw.op('pe', lambda e, c=c, bnk=bnk, c0=c0, c1=c1: e.matmul(
                    ps[bnk][:], lhsT=xg[s][:, c, :], rhs=wq_sb[:, c, c0:c1],
                    start=(c == 0), stop=(c == 7)),
                    reads=[('xg', s), 'wq'], writes=[P[bnk]])
        fw.op('act', lambda e: e.activation(out=qf[:].rearrange("p h d -> p (h d)"), in_=ps[0][:],
                                            func=AF.Copy, scale=rs),
              reads=[P[0], ('rstd', i)], writes=['qf'])
        rope(qf[:], i, rtmp, 'qf')
        fw.op('pool', lambda e: e.tensor_copy(out=qaA[s][:, :, 0:64], in_=qf[:]), reads=['qf'], writes=[('qaA', s)])
        fw.op('act', lambda e: e.activation(out=qaB[s][:, :, 0:64], in_=ps[1][:].rearrange("p (h d) -> p h d", h=8),
                                            func=AF.Copy, scale=rs),
              reads=[P[1], ('rstd', i)], writes=[('qaB', s)])
        fw.op('pool', lambda e: e.tensor_scalar(out=qaB[s][:, :, 64], in0=negc[:, i, :], scalar1=-8.0, scalar2=None,
                                                op0=ALU.mult),
              reads=['negc', ('qaB', s)], writes=[('qaB', s)])

    def q_back(i):
        s = i % 2
        g4 = (i // 4) % 2
        pt64 = ps[6][0:64, :].bitcast(BF16).rearrange("p (h t) -> p h t", h=8)
        for h in range(8):
            fw.op('pe', lambda e, h=h: e.transpose(out=pt64[:, h, :], in_=qaA[s][:, h, 0:64], identity=identb[:]),
                  reads=[('qaA', s), 'identb'], writes=[P[6]])
        fw.op('dve', lambda e: e.tensor_copy(out=qT[:], in_=pt64), reads=[P[6]], writes=['qT'])
        psg = ps[2][:, 0:256].rearrange("p (h n) -> p h n", h=8)
        for h in range(8):
            fw.op('pe', lambda e, h=h: e.matmul(psg[:, h, :], lhsT=qT[:, h, :], rhs=kmT[:, h, :],
                                                 start=True, stop=True),
                  reads=['qT', 'kmT'], writes=[P[2]])
        pt65 = ps[3][0:65, :].bitcast(BF16).rearrange("p (h t) -> p h t", h=8)
        for h in range(8):
            fw.op('pe', lambda e, h=h: e.transpose(out=pt65[:, h, :], in_=qaB[s][:, h, :], identity=identb[:]),
                  reads=[('qaB', s), 'identb'], writes=[P[3]])
        fw.op('dve', lambda e: e.tensor_tensor(out=gm[:], in0=psg,
                                               in1=pastb_sb[:, i, :].unsqueeze(1).to_broadcast([128, 8, 32]),
                                               op=ALU.add),
              reads=[P[2], 'pastb_sb'], writes=['gm'])
        for h in range(8):
            fw.op('dve', lambda e, h=h: e.max(out=m8[:, h, :], in_=gm[:, h, :]), reads=['gm'], writes=[('m8', h)])
        fw.op('dve', lambda e: e.tensor_scalar(out=thr[:], in0=m8[:, :, 2], scalar1=-1e29, scalar2=None,
                                               op0=ALU.max),
              reads=[('m8', h_) for h_ in range(8)], writes=['thr'])
        fw.op('dve', lambda e: e.tensor_tensor(out=sel[:], in0=gm[:],
                                               in1=thr[:].unsqueeze(2).to_broadcast([128, 8, 32]), op=ALU.is_ge),
              reads=['gm', 'thr'], writes=['sel'])
        fw.op('dve', lambda e: e.tensor_tensor(out=sel[:], in0=sel[:],
                                               in1=ownm_sb[:, i, :].unsqueeze(1).to_broadcast([128, 8, 32]),
                                               op=ALU.add),
              reads=['sel', 'ownm_sb'], writes=['sel'])
        fw.op('dve', lambda e: e.tensor_scalar(out=qaA[s][:, :, 64:96], in0=sel[:], scalar1=-1.0, scalar2=-NEG,
                                               op0=ALU.add, op1=ALU.mult),
              reads=['sel', ('qaA', s)], writes=[('qaA', s)])
        fw.op('act', lambda e: e.activation(out=stQB[g4][:, :, (i % 4) * 128:(i % 4 + 1) * 128], in_=pt65, func=AF.Copy),
              reads=[P[3]], writes=[('stQB', g4)])
        pt96 = ps[7][0:96, :].bitcast(BF16).rearrange("p (h t) -> p h t", h=8)
        for h in range(8):
            fw.op('pe', lambda e, h=h: e.transpose(out=pt96[:, h, :], in_=qaA[s][:, h, :], identity=identb[:]),
                  reads=[('qaA', s), 'identb'], writes=[P[7]])
        fw.op('act', lambda e: e.activation(out=stQA[g4][:, :, (i % 4) * 128:(i % 4 + 1) * 128], in_=pt96, func=AF.Copy),
              reads=[P[7]], writes=[('stQA', g4)])
        if i % 4 == 3:
            g = i // 4
            fw.dma('sp', lambda e, g=g: e.dma_start(
                out=QTA[:, :, g * 512:(g + 1) * 512].rearrange("h d s -> d h s"), in_=stQA[g4][:]),
                reads=[('stQA', g4)], writes=['QTA'])
            fw.dma('sp', lambda e, g=g: e.dma_start(
                out=QTB[:, :, g * 512:(g + 1) * 512].rearrange("h d s -> d h s"), in_=stQB[g4][:]),
                reads=[('stQB', g4)], writes=['QTB'])

    q_front(0)
    for i in range(NO):
        if i + 1 < NO:
            q_front(i + 1)
        q_back(i)
    fw.barrier()
    p1.close()
    if STOP_AFTER <= 1:
        return finish(nc, fw, y, glob)

    p2 = ExitStack()
    KT = [sb(p2, "KT%d" % k, [128, 8192], BF16) for k in range(2)]
    VV = [sb(p2, "VV%d" % k, [128, NT, 128], BF16) for k in range(2)]
    QT = [sb(p2, "QT%d" % k, [128, 2048], BF16) for k in range(2)]
    for k in range(2):
        fw.op('pool', lambda e, k=k: e.memset(VV[k][:], 0.0), writes=[('VV', k)])
    NPT = 8
    SBK = [0, 1, 2, 3, 7]
    PT = [sb(p2, "PT%d" % k, [128, 512], BF16) for k in range(NPT)]
    trim_sb = sb(p2, "trim_sb", [128, 4, 512], BF16)
    acc = [[sb(p2, "acc%d_%d" % (k, t), [65, 1024], F32) for t in range(2)] for k in range(2)]
    QU = [sb(p2, "QU%d" % k, [128, 1024], BF16) for k in range(2)]
    uw_sb = sb(p2, "uw_sb", [128, 6, 2], F32)
    fw.dma('sp', lambda e: e.dma_start(out=uw_sb[:], in_=uwd), writes=['uw'])
    rrow = [sb(p2, "rrow%d" % k, [65, 512], BF16) for k in range(4)]
    njob = 0
    ones_bb = sb(p2, "ones_bb", [65, 64], BF16)
    fw.op('pool', lambda e: e.memset(ones_bb[:], 1.0), writes=['ones_bb'])
    pending = []
    attst = [sb(p2, "attst%d" % k, [64, 512], BF16) for k in range(4)]
    fw.dma('pool', lambda e: e.dma_start(out=trim_sb[:], in_=trim), writes=['trim_sb'])
    trimb_sb = sb(p2, "trimb_sb", [128, 4, 512], F32)
    stmp = [sb(p2, "stmp%d" % k, [128, 512], F32) for k in range(2)]
    fw.dma('sp', lambda e: e.dma_start(out=trimb_sb[:], in_=trim), writes=['trimb_sb'])
    fw.op('dve', lambda e: e.tensor_scalar(out=trimb_sb[:], in0=trimb_sb[:], scalar1=-1.0, scalar2=1e6,
                                           op0=ALU.add, op1=ALU.mult), reads=['trimb_sb'], writes=['trimb_sb'])
    ndiag = 0

    scale = 0.125
    npair = 0
    nq = 0
    for br in range(2):
        for h in range(8):
            hs = (br * 8 + h) % 2
            Kd, Vd, Qd = (KTA, VA, QTA) if br == 0 else (KTB, VB, QTB)
            Rq = 96 if br == 0 else 65
            Rr = 128
            fw.op('pool', lambda e: e.memset(KT[hs][64:128, :], 0.0), writes=[('KT', hs)])
            fw.op('pool', lambda e: e.memset(QT[hs][64:128, :], 0.0), writes=[('QT', hs)])
            fw.dma('sp', lambda e, h=h, Kd=Kd: e.dma_start(out=KT[hs][0:64, :], in_=Kd[h]),
                   reads=['KTA', 'KTB'], writes=[('KT', hs)])
            if br == 0:
                fw.dma('pool', lambda e: e.dma_start(out=KT[hs][64:96, :], in_=onehot),
                       writes=[('KT', hs)])
            else:
                fw.op('pool', lambda e: e.memset(KT[hs][64:65, :], 1.0), writes=[('KT', hs)])
            fw.dma('sp', lambda e, h=h, Vd=Vd: e.dma_start(out=VV[hs][:, :, 0:65], in_=Vd[h]),
                   reads=['VA', 'VB'], writes=[('VV', hs)])
            fw.dma('sp', lambda e, h=h, Qd=Qd: e.dma_start(out=QT[hs][0:Rq, :], in_=Qd[h]),
                   reads=['QTA', 'QTB'], writes=[('QT', hs)])
            hp = (br * 8 + h) % 2
            jobs = []
            for qt in range(4):
                u = qt % 2
                if qt < 2:
                    tiles = [(t, None) for t in range(0, 4 * u)] + [(4 * u + o, o) for o in range(4)]
                else:
                    tiles = [(t, None) for t in range(0, 8)] + [(8 + t, None) for t in range(0, 4 * u)] \
                        + [(8 + 4 * u + o, o) for o in range(4)]
                jobs.append(('own', QT[hs][:, qt * 512:(qt + 1) * 512], ('QT', hs), tiles, qt // 2, qt % 2, None))
            for un in range(6):
                for half in range(2):
                    tiles = [(16 + 8 * un + t, None) for t in range(8)]
                    jobs.append(('unit', QU[un % 2][:, half * 512:(half + 1) * 512], ('QU', un % 2), tiles, None, half, un))
            items = []
            for jn, job in enumerate(jobs):
                for n, (kt, o) in enumerate(job[3]):
                    items.append((jn, kt, o, n == 0, n == len(job[3]) - 1))

            def emit_S(it, g):
                jn, kt, o, first, last = it
                kind, qap, qkey, _, tgt, half, un = jobs[jn]
                sbk = SBK[g % 5]
                pk = g % NPT
                nxt = None
                if first and kind == 'own' and jn == 3:
                    nxt = 0
                elif first and kind == 'unit' and half == 0 and un + 1 < 6:
                    nxt = un + 1
                if nxt is not None:
                    ub = nxt % 2
                    fw.op('dve', lambda e: e.tensor_scalar(out=QU[ub][:], in0=QT[hs][:, 0:1024], scalar1=uw_sb[:, nxt, 0:1],
                                                           scalar2=None, op0=ALU.mult),
                          reads=[('QT', hs), 'uw'], writes=[('QU', ub)])
                    fw.op('dve', lambda e: e.scalar_tensor_tensor(out=QU[ub][:], in0=QT[hs][:, 1024:2048],
                                                                  scalar=uw_sb[:, nxt, 1:2], in1=QU[ub][:],
                                                                  op0=ALU.mult, op1=ALU.add),
                          reads=[('QT', hs), 'uw', ('QU', ub)], writes=[('QU', ub)])
                fw.op('pe', lambda e: e.matmul(
                    ps[sbk][:], lhsT=KT[hs][:, kt * 128:(kt + 1) * 128], rhs=qap, start=True, stop=True),
                    reads=[('KT', hs), qkey], writes=[P[sbk]])
                if br == 0:
                    fw.op('act', lambda e: e.activation(
                        out=PT[pk][:], in_=ps[sbk][:], func=AF.Exp, scale=scale),
                        reads=[P[sbk]], writes=[('PT', pk)])
                    if o is not None:
                        fw.op('dve', lambda e: e.tensor_tensor(
                            out=PT[pk][:], in0=PT[pk][:], in1=trim_sb[:, o, :], op=ALU.mult),
                            reads=[('PT', pk), 'trim_sb'], writes=[('PT', pk)])
                elif o is not None:
                    dk = g % 2
                    fw.op('dve', lambda e: e.tensor_tensor(
                        out=stmp[dk][:], in0=ps[sbk][:], in1=trimb_sb[:, o, :], op=ALU.add),
                        reads=[P[sbk], 'trimb_sb'], writes=[('stmp', dk)])
                    fw.op('act', lambda e: e.activation(
                        out=PT[pk][:], in_=stmp[dk][:], func=AF.Exp, bias=negcA[:, kt, h:h + 1], scale=scale),
                        reads=[('stmp', dk), 'negcA'], writes=[('PT', pk)])
                else:
                    fw.op('act', lambda e: e.activation(
                        out=PT[pk][:], in_=ps[sbk][:], func=AF.Exp, bias=negcA[:, kt, h:h + 1], scale=scale),
                        reads=[P[sbk], 'negcA'], writes=[('PT', pk)])

            def emit_PV(it, g):
                jn, kt, o, first, last = it
                kind, qap, qkey, _, tgt, half, un = jobs[jn]
                pk = g % NPT
                ob = 4 + ((njob + jn) % 2)
                hsl = slice(half * 512, (half + 1) * 512)
                fw.op('pe', lambda e: e.matmul(
                    ps[ob][:], lhsT=VV[hs][:, kt, :], rhs=PT[pk][:], start=first, stop=last),
                    reads=[('VV', hs), ('PT', pk)], writes=[P[ob]])
                if last and kind == 'own':
                    fw.op('dve', lambda e: e.tensor_copy(out=acc[hp][tgt][:, hsl], in_=ps[ob][0:65, :]),
                          reads=[P[ob]], writes=[('acc', hp, tgt, half)])
                elif last:
                    for tg2 in range(2):
                        fw.op('dve', lambda e, tg2=tg2: e.scalar_tensor_tensor(
                            out=acc[hp][tg2][:, hsl], in0=ps[ob][0:65, :], scalar=uw_sb[0:65, un, tg2:tg2 + 1],
                            in1=acc[hp][tg2][:, hsl], op0=ALU.mult, op1=ALU.add),
                            reads=[P[ob], 'uw', ('acc', hp, tg2, half)], writes=[('acc', hp, tg2, half)])

            LA = 4
            GP = 2
            nit = len(items)
            for idx in range(0, nit + LA, GP):
                pv = [k - LA for k in range(idx, idx + GP) if 0 <= k - LA < nit]
                if pv:
                    fw.prewait('pe', reads=[('PT', (npair + pv[-1]) % NPT)])
                for k in range(idx, idx + GP):
                    if k < nit:
                        emit_S(items[k], npair + k)
                for k in pv:
                    emit_PV(items[k], npair + k)
                for pd in list(pending):
                    pd[0] -= GP
                    if pd[0] <= 0:
                        pending.remove(pd)
                        pd[1]()
            npair += len(items)
            njob += len(jobs)
            for qt in range(4):
                tgt, half = qt // 2, qt % 2
                hsl = slice(half * 512, (half + 1) * 512)
                with nc.allow_low_precision("softmax normaliser 1/l is broadcast through a bf16 K=1 matmul"):
                    fw.op('dve', lambda e, qt=qt, tgt=tgt, hsl=hsl: e.reciprocal(out=rrow[qt][64:65, :], in_=acc[hp][tgt][64:65, hsl]),
                          reads=[('acc', hp, tgt, half)], writes=[('rrow', qt)])

                def fin(qt=qt, tgt=tgt, half=half, hsl=hsl, hp=hp, br=br, h=h):
                    fw.op('pe', lambda e: e.matmul(ps[6][0:64, :], lhsT=ones_bb[64:65, 0:64], rhs=rrow[qt][64:65, :],
                                                   start=True, stop=True),
                          reads=[('rrow', qt), 'ones_bb'], writes=[P[6]])
                    fw.op('dve', lambda e: e.tensor_tensor(out=attst[qt][:], in0=acc[hp][tgt][0:64, hsl], in1=ps[6][0:64, :],
                                                           op=ALU.mult),
                          reads=[('acc', hp, tgt, half), P[6]], writes=[('attst', qt)])
                    fw.dma('sp', lambda e: e.dma_start(
                        out=ATT[br, h, :, qt * 512:(qt + 1) * 512], in_=attst[qt][:]),
                        reads=[('attst', qt)], writes=['ATT'])
                pending.append([16 + 6 * qt, fin])
    for pd in pending:
        pd[1]()
    fw.barrier()
    p2.close()
    if STOP_AFTER <= 2:
        return finish(nc, fw, y, glob)

    p3 = ExitStack()
    wg_sb = sb(p3, "wg_sb", [128, 8, 2048], BF16)
    wba_sb = sb(p3, "wba_sb", [64, 8, D], BF16)
    wbb_sb = sb(p3, "wbb_sb", [64, 8, D], BF16)
    wout_sb = sb(p3, "wout_sb", [128, 8, D], BF16)
    for k in range(8):
        fw.dma('pool', lambda e, k=k: e.dma_start(out=wg_sb[:, k, :], in_=w_g[:, k, :]), writes=['wg'])
        fw.dma('pool', lambda e, k=k: e.dma_start(out=wout_sb[:, k, :], in_=wout[:, k, :]), writes=['wout'])
        fw.dma('pool', lambda e, k=k: e.dma_start(out=wba_sb[:, k, :], in_=wba[:, k, :]), writes=['wba'])
        fw.dma('pool', lambda e, k=k: e.dma_start(out=wbb_sb[:, k, :], in_=wbb[:, k, :]), writes=['wbb'])
    xtf3 = [sb(p3, "xtf3_%d" % k, [128, 8, 128], F32) for k in range(2)]
    xg3 = [sb(p3, "xg3_%d" % k, [128, 8, 128], BF16) for k in range(2)]
    att = [sb(p3, "att%d" % k, [64, 16, 128], BF16) for k in range(2)]
    xo = [sb(p3, "xo%d" % k, [128, D], F32) for k in range(2)]
    sgA = sb(p3, "sgA", [128, D], F32)
    sgB = sb(p3, "sgB", [128, D], F32)
    mg = sb(p3, "mg", [128, D], F32)
    mgb = sb(p3, "mgb", [128, D], BF16)
    mT = sb(p3, "mT", [128, 8, 128], BF16)
    hh3 = [sb(p3, "hh3_%d" % k, [128, D], F32) for k in range(2)]
    for i in range(NO):
        s = i % 2
        fw.dma('sp', lambda e: e.dma_start(out=xtf3[s][:], in_=xT[i]), writes=[('xtf3', s)])
        fw.dma('sp', lambda e: e.dma_start(
            out=att[s][:], in_=ATT[:, :, :, i * 128:(i + 1) * 128].rearrange("b h d t -> d (b h) t")),
            reads=['ATT'], writes=[('att', s)])
        fw.dma('sp', lambda e: e.dma_start(out=xo[s][:], in_=xown[i]), writes=[('xo', s)])
        fw.op('dve', lambda e: e.tensor_tensor(
            out=xg3[s][:], in0=xtf3[s][:],
            in1=gmix_sb[:].unsqueeze(2).to_broadcast([128, 8, 128]), op=ALU.mult),
            reads=[('xtf3', s), 'gmix'], writes=[('xg3', s)])
        rs = rstd_all[:, i:i + 1]
        for bnk in range(4):
            for c in range(8):
                fw.op('pe', lambda e, c=c, bnk=bnk: e.matmul(
                    ps[bnk][:], lhsT=xg3[s][:, c, :], rhs=wg_sb[:, c, bnk * 512:(bnk + 1) * 512],
                    start=(c == 0), stop=(c == 7)),
                    reads=[('xg3', s), 'wg'], writes=[P[bnk]])
        for bnk in range(4):
            dst = (sgA if bnk < 2 else sgB)[:, (bnk % 2) * 512:(bnk % 2 + 1) * 512]
            fw.op('act', lambda e, bnk=bnk, dst=dst: e.activation(out=dst, in_=ps[bnk][:], func=AF.Sigmoid, scale=rs),
                  reads=[P[bnk]], writes=[('sg', bnk)])
        for br in range(2):
            wsb = wba_sb if br == 0 else wbb_sb
            for half in range(2):
                bnk = 4 + br * 2 + half
                for h in range(8):
                    fw.op('pe', lambda e, h=h, bnk=bnk, br=br, half=half, wsb=wsb: e.matmul(
                        ps[bnk][:], lhsT=att[s][:, br * 8 + h, :], rhs=wsb[:, h, half * 512:(half + 1) * 512],
                        start=(h == 0), stop=(h == 7)),
                        reads=[('att', s), 'wba', 'wbb'], writes=[P[bnk]])
        for half in range(2):
            sl = slice(half * 512, (half + 1) * 512)
            fw.op('dve', lambda e, half=half, sl=sl: e.tensor_tensor(out=mg[:, sl], in0=sgA[:, sl], in1=ps[4 + half][:], op=ALU.mult),
                  reads=[('sg', half), P[4 + half]], writes=[('mg', half)])
            fw.op('dve', lambda e, half=half, sl=sl: e.tensor_tensor(out=sgB[:, sl], in0=sgB[:, sl], in1=ps[6 + half][:], op=ALU.mult),
                  reads=[('sg', 2 + half), P[6 + half]], writes=[('sg', 2 + half)])
            fw.op('dve', lambda e, half=half, sl=sl: e.tensor_tensor(out=mgb[:, sl], in0=mg[:, sl], in1=sgB[:, sl], op=ALU.add),
                  reads=[('mg', half), ('sg', 2 + half)], writes=[('mgb', half)])
        ptm = ps[0][:].bitcast(BF16).rearrange("p (c t) -> p c t", c=8)
        for c in range(8):
            fw.op('pe', lambda e, c=c: e.transpose(out=ptm[:, c, :], in_=mgb[:, c * 128:(c + 1) * 128], identity=identb[:]),
                  reads=[('mgb', c // 4), 'identb'], writes=[P[0]])
        fw.op('act', lambda e: e.activation(out=mT[:], in_=ptm, func=AF.Copy), reads=[P[0]], writes=['mT'])
        for half in range(2):
            for c in range(8):
                fw.op('pe', lambda e, c=c, half=half: e.matmul(
                    ps[1 + half][:], lhsT=mT[:, c, :], rhs=wout_sb[:, c, half * 512:(half + 1) * 512],
                    start=(c == 0), stop=(c == 7)),
                    reads=['mT', 'wout'], writes=[P[1 + half]])
        for half in range(2):
            sl = slice(half * 512, (half + 1) * 512)
            fw.op('dve', lambda e, half=half, sl=sl: e.tensor_tensor(out=hh3[s][:, sl], in0=xo[s][:, sl], in1=ps[1 + half][:], op=ALU.add),
                  reads=[('xo', s), P[1 + half]], writes=[('hh3', s)])
        fw.dma('sp', lambda e, i=i: e.dma_start(out=HS[i], in_=hh3[s][:]), reads=[('hh3', s)], writes=['HS'])
    fw.barrier()
    p3.close()
    if STOP_AFTER <= 3:
        return finish(nc, fw, y, glob)

    p4 = ExitStack()
    xn2T_all = sb(p4, "xn2T_all", [128, 8, 2048], BF16)
    eps2 = sb(p4, "eps2", [128, 1], F32)
    p4b = ExitStack()
    selT_all = sb(p4b, "selT_all", [128, NO, 3, 128], BF16)
    p41 = ExitStack()
    wpq_sb = sb(p41, "wpq_sb", [128, 8, 2048], BF16)
    skT_sb = sb(p41, "skT_sb", [128, 16, 128], BF16)
    gffn_sb = sb(p41, "gffn_sb", [128, D], F32)
    iota_sb = sb(p41, "iota_sb", [128, 16], F32)
    lo_sb = sb(p41, "lo_sb", [128, 16], F32)
    hi_sb = sb(p41, "hi_sb", [128, 16], F32)
    for k in range(8):
        fw.dma('pool', lambda e, k=k: e.dma_start(out=wpq_sb[:, k, :], in_=wpq[:, k, :]), writes=['wpq'])
    fw.dma('pool', lambda e: e.dma_start(out=skT_sb[:], in_=skT), writes=['skT'])
    fw.dma('sp', lambda e: e.dma_start(out=gffn_sb[:], in_=g_ffn), writes=['gffn'])
    fw.dma('sp', lambda e: e.dma_start(out=iota_sb[:], in_=iota16), writes=['iota'])
    fw.op('dve', lambda e: e.tensor_scalar(out=lo_sb[:], in0=iota_sb[:], scalar1=16.0, scalar2=None, op0=ALU.mult),
          reads=['iota'], writes=['lo'])
    fw.op('dve', lambda e: e.tensor_scalar(out=hi_sb[:], in0=iota_sb[:], scalar1=16.0, scalar2=16.0, op0=ALU.mult, op1=ALU.add),
          reads=['iota'], writes=['hi'])
    hh = [sb(p41, "hh%d" % k, [128, D], F32) for k in range(2)]
    sel3 = sb(p41, "sel3", [128, 3, 128], F32)
    junk = sb(p41, "junk", [128, D], F32)
    ss2 = sb(p41, "ss2", [128, 1], F32)
    rs2 = sb(p41, "rs2", [128, 1], F32)
    xn2b = sb(p41, "xn2b", [128, D], BF16)
    qpb = sb(p41, "qpb", [128, 2048], BF16)
    qpT = sb(p41, "qpT", [128, 16, 128], BF16)
    scw = sb(p41, "scw", [128, 16, 128], F32)
    sv = sb(p41, "sv", [128, 16, 16], F32)
    si = sb(p41, "si", [128, 16, 16], U32)
    sif = sb(p41, "sif", [128, 16, 16], F32)
    cand = sb(p41, "cand", [128, 8, 256], F32)
    candw = sb(p41, "candw", [128, 8, 256], F32)
    best = sb(p41, "best", [128, 8, 16], F32)
    pick = sb(p41, "pick", [128, 8, 16], U32)
    pf = sb(p41, "pf", [128, 8, 16], F32)
    af = sb(p41, "af", [128, 8, 16], F32)
    bfl = sb(p41, "bfl", [128, 8, 16], F32)
    eq = sb(p41, "eq", [128, 8, 16, 16], F32)
    eq2 = sb(p41, "eq2", [128, 8, 16, 16], F32)
    gex = sb(p41, "gex", [128, 8, 16], F32)
    gsum = sb(p41, "gsum", [128, 8], F32)
    B4 = [128, 8, 16, 16]

    def stage_F(i):
        s = i % 2
        B0 = 4 * (i % 2)
        fw.dma('sp', lambda e, i=i: e.dma_start(out=hh[s][:], in_=HS[i]), reads=['HS'], writes=[('hh', s)])
        fw.op('act', lambda e: e.activation(out=junk[:], in_=hh[s][:], func=AF.Square, accum_out=ss2[:]),
              reads=[('hh', s)], writes=['junk', 'ss2'])
        fw.op('act', lambda e: e.activation(out=rs2[:], in_=ss2[:], func=AF.Sqrt, bias=eps_sb[:], scale=1.0 / D),
              reads=['ss2', 'eps_sb'], writes=['rs2'])
        fw.op('dve', lambda e: e.reciprocal(out=rs2[:], in_=rs2[:]), reads=['rs2'], writes=['rs2'])
        fw.op('dve', lambda e: e.scalar_tensor_tensor(out=xn2b[:], in0=hh[s][:], scalar=rs2[:, 0:1], in1=gffn_sb[:],
                                                      op0=ALU.mult, op1=ALU.mult),
              reads=[('hh', s), 'rs2', 'gffn'], writes=['xn2b'])
        ptx = ps[B0][:].bitcast(BF16).rearrange("p (c t) -> p c t", c=8)
        for c in range(8):
            fw.op('pe', lambda e, c=c: e.transpose(out=ptx[:, c, :], in_=xn2b[:, c * 128:(c + 1) * 128], identity=identb[:]),
                  reads=['xn2b', 'identb'], writes=[P[B0]])
        xT_i = xn2T_all[:, :, i * 128:(i + 1) * 128]
        fw.op('act', lambda e: e.activation(out=xT_i, in_=ptx, func=AF.Copy), reads=[P[B0]], writes=[('xn2T', i)])
        QB = [B0 + 1, B0 + 2, B0 + 3, B0]
        for bnk in range(4):
            for c in range(8):
                fw.op('pe', lambda e, c=c, bnk=bnk: e.matmul(
                    ps[QB[bnk]][:], lhsT=xn2T_all[:, c, i * 128:(i + 1) * 128], rhs=wpq_sb[:, c, bnk * 512:(bnk + 1) * 512],
                    start=(c == 0), stop=(c == 7)),
                    reads=[('xn2T', i), 'wpq'], writes=[P[QB[bnk]]])
            fw.op('act', lambda e, bnk=bnk: e.activation(out=qpb[:, bnk * 512:(bnk + 1) * 512], in_=ps[QB[bnk]][:], func=AF.Copy),
                  reads=[P[QB[bnk]]], writes=[('qpb', bnk)])
        for half in range(2):
            ptq = ps[B0 + 1 + half][:].bitcast(BF16).rearrange("p (c t) -> p c t", c=8)
            for c in range(8):
                gidx = half * 8 + c
                fw.op('pe', lambda e, c=c, gidx=gidx, ptq=ptq: e.transpose(
                    out=ptq[:, c, :], in_=qpb[:, gidx * 128:(gidx + 1) * 128], identity=identb[:]),
                    reads=[('qpb', gidx // 4), 'identb'], writes=[P[B0 + 1 + half]])
            fw.op('act', lambda e, ptq=ptq, half=half: e.activation(out=qpT[:, half * 8:(half + 1) * 8, :], in_=ptq, func=AF.Copy),
                  reads=[P[B0 + 1 + half]], writes=[('qpT', half)])
        for gidx in range(16):
            bnk = B0 + gidx // 4
            fw.op('pe', lambda e, gidx=gidx, bnk=bnk: e.matmul(
                ps[bnk][:, (gidx % 4) * 128:(gidx % 4 + 1) * 128], lhsT=qpT[:, gidx, :], rhs=skT_sb[:, gidx, :],
                start=True, stop=True),
                reads=[('qpT', gidx // 8), 'skT'], writes=[P[bnk]])
    def stage_T(i):
        s = i % 2
        B0 = 4 * (i % 2)
        def srcg(gidx):
            return ps[B0 + gidx // 4][:, (gidx % 4) * 128:(gidx % 4 + 1) * 128]
        for gidx in range(16):
            fw.op('dve', lambda e, gidx=gidx: e.max(out=sv[:, gidx, 0:8], in_=srcg(gidx)),
                  reads=[P[B0 + gidx // 4]], writes=[('sv', gidx, 0)])
        for gidx in range(16):
            fw.op('dve', lambda e, gidx=gidx: e.max_index(out=si[:, gidx, 0:8], in_max=sv[:, gidx, 0:8], in_values=srcg(gidx)),
                  reads=[P[B0 + gidx // 4], ('sv', gidx, 0)], writes=[('si', gidx, 0)])
        for gidx in range(16):
            fw.op('dve', lambda e, gidx=gidx: e.match_replace(out=scw[:, gidx, :], in_to_replace=sv[:, gidx, 0:8],
                                                              in_values=srcg(gidx), imm_value=-1e30),
                  reads=[P[B0 + gidx // 4], ('sv', gidx, 0)], writes=[('scw', gidx)])
        for gidx in range(16):
            fw.op('dve', lambda e, gidx=gidx: e.max(out=sv[:, gidx, 8:16], in_=scw[:, gidx, :]),
                  reads=[('scw', gidx)], writes=[('sv', gidx, 1)])
        for gidx in range(16):
            fw.op('dve', lambda e, gidx=gidx: e.max_index(out=si[:, gidx, 8:16], in_max=sv[:, gidx, 8:16], in_values=scw[:, gidx, :]),
                  reads=[('scw', gidx), ('sv', gidx, 1)], writes=[('si', gidx, 1)])
        fw.op('dve', lambda e: e.tensor_copy(out=sif[:], in_=si[:]),
              reads=[('si', g_, k_) for g_ in range(16) for k_ in range(2)], writes=['sif'])
        sv4 = sv[:].rearrange("p (h two) k -> p h two k", two=2)
        sif4 = sif[:].rearrange("p (h two) k -> p h two k", two=2)
        cand4 = cand[:].rearrange("p h (a b) -> p h a b", a=16)
        fw.op('dve', lambda e: e.tensor_tensor(out=cand4, in0=sv4[:, :, 0, :].unsqueeze(3).to_broadcast(B4),
                                               in1=sv4[:, :, 1, :].unsqueeze(2).to_broadcast(B4), op=ALU.add),
              reads=[('sv', g_, k_) for g_ in range(16) for k_ in range(2)], writes=['cand'])
        for h in []:
            fw.op('dve', lambda e, h=h: e.max(out=best[:, h, 0:8], in_=cand[:, h, :]), reads=['cand'], writes=['best'])
            fw.op('dve', lambda e, h=h: e.max_index(out=pick[:, h, 0:8], in_max=best[:, h, 0:8], in_values=cand[:, h, :]),
                  reads=['cand', 'best'], writes=['pick'])
            fw.op('dve', lambda e, h=h: e.match_replace(out=candw[:, h, :], in_to_replace=best[:, h, 0:8],
                                                        in_values=cand[:, h, :], imm_value=-1e30),
                  reads=['cand', 'best'], writes=['candw'])
            fw.op('dve', lambda e, h=h: e.max(out=best[:, h, 8:16], in_=candw[:, h, :]), reads=['candw'], writes=['best'])
            fw.op('dve', lambda e, h=h: e.max_index(out=pick[:, h, 8:16], in_max=best[:, h, 8:16], in_values=candw[:, h, :]),
                  reads=['candw', 'best'], writes=['pick'])
        for h in range(8):
            fw.op('dve', lambda e, h=h: e.max(out=best[:, h, 0:8], in_=cand[:, h, :]), reads=['cand'], writes=[('best', h, 0)])
        for h in range(8):
            fw.op('dve', lambda e, h=h: e.max_index(out=pick[:, h, 0:8], in_max=best[:, h, 0:8], in_values=cand[:, h, :]),
                  reads=['cand', ('best', h, 0)], writes=[('pick', h, 0)])
        for h in range(8):
            fw.op('dve', lambda e, h=h: e.match_replace(out=candw[:, h, :], in_to_replace=best[:, h, 0:8],
                                                        in_values=cand[:, h, :], imm_value=-1e30),
                  reads=['cand', ('best', h, 0)], writes=[('candw', h)])
        for h in range(8):
            fw.op('dve', lambda e, h=h: e.max(out=best[:, h, 8:16], in_=candw[:, h, :]), reads=[('candw', h)], writes=[('best', h, 1)])
        for h in range(8):
            fw.op('dve', lambda e, h=h: e.max_index(out=pick[:, h, 8:16], in_max=best[:, h, 8:16], in_values=candw[:, h, :]),
                  reads=[('candw', h), ('best', h, 1)], writes=[('pick', h, 1)])
        BESTK = [('best', h_, k_) for h_ in range(8) for k_ in range(2)]
        PICKK = [('pick', h_, k_) for h_ in range(8) for k_ in range(2)]
        fw.op('dve', lambda e: e.tensor_copy(out=pf[:], in_=pick[:]), reads=PICKK, writes=['pf'])
        pf4 = pf[:].unsqueeze(3).to_broadcast(B4)
        lo4 = lo_sb[:].unsqueeze(1).unsqueeze(1).to_broadcast(B4)
        hi4 = hi_sb[:].unsqueeze(1).unsqueeze(1).to_broadcast(B4)
        io4 = iota_sb[:].unsqueeze(1).unsqueeze(1).to_broadcast(B4)
        e1v = sel3[:, 0, :].rearrange("p (h k) -> p h k", h=8)
        e2v = sel3[:, 1, :].rearrange("p (h k) -> p h k", h=8)
        gtv = sel3[:, 2, :].rearrange("p (h k) -> p h k", h=8)
        fw.op('dve', lambda e: e.tensor_tensor(out=eq2[:], in0=pf4, in1=hi4, op=ALU.is_ge), reads=['pf', 'hi'], writes=['eq2'])
        fw.op('dve', lambda e: e.tensor_reduce(out=af[:], in_=eq2[:], axis=AX.X, op=ALU.add), reads=['eq2'], writes=['af'])
        fw.op('dve', lambda e: e.tensor_tensor(out=eq[:], in0=af[:].unsqueeze(3).to_broadcast(B4), in1=io4, op=ALU.is_equal),
              reads=['af', 'iota'], writes=['eq'])
        fw.op('dve', lambda e: e.tensor_tensor(out=eq2[:], in0=eq[:], in1=sif4[:, :, 0, :].unsqueeze(2).to_broadcast(B4), op=ALU.mult),
              reads=['eq', 'sif'], writes=['eq2'])
        fw.op('dve', lambda e: e.tensor_reduce(out=e1v, in_=eq2[:], axis=AX.X, op=ALU.add), reads=['eq2'], writes=['sel3'])
        fw.op('dve', lambda e: e.scalar_tensor_tensor(out=bfl[:], in0=af[:], scalar=-16.0, in1=pf[:], op0=ALU.mult, op1=ALU.add),
              reads=['af', 'pf'], writes=['bfl'])
        fw.op('dve', lambda e: e.tensor_tensor(out=eq[:], in0=bfl[:].unsqueeze(3).to_broadcast(B4), in1=io4, op=ALU.is_equal),
              reads=['bfl', 'iota'], writes=['eq'])
        fw.op('dve', lambda e: e.tensor_tensor(out=eq2[:], in0=eq[:], in1=sif4[:, :, 1, :].unsqueeze(2).to_broadcast(B4), op=ALU.mult),
              reads=['eq', 'sif'], writes=['eq2'])
        fw.op('dve', lambda e: e.tensor_reduce(out=e2v, in_=eq2[:], axis=AX.X, op=ALU.add), reads=['eq2'], writes=['sel3'])
        fw.op('dve', lambda e: e.tensor_tensor(out=gex[:], in0=best[:], in1=best[:, :, 0:1].to_broadcast([128, 8, 16]),
                                               op=ALU.subtract),
              reads=BESTK, writes=['gex'])
        fw.op('act', lambda e: e.activation(out=gex[:], in_=gex[:], func=AF.Exp), reads=['gex'], writes=['gex'])
        fw.op('dve', lambda e: e.tensor_reduce(out=gsum[:], in_=gex[:], axis=AX.X, op=ALU.add), reads=['gex'], writes=['gsum'])
        fw.op('dve', lambda e: e.reciprocal(out=gsum[:], in_=gsum[:]), reads=['gsum'], writes=['gsum'])
        fw.op('dve', lambda e: e.tensor_tensor(out=gtv, in0=gex[:],
                                               in1=gsum[:].unsqueeze(2).to_broadcast([128, 8, 16]), op=ALU.mult),
              reads=['gex', 'gsum'], writes=['sel3'])
        pst = ps[B0][:, 0:384].rearrange("p (a t) -> p a t", a=3)
        for a in range(3):
            fw.op('pe', lambda e, a=a: e.transpose(out=pst[:, a, :], in_=sel3[:, a, :], identity=identf[:]),
                  reads=['sel3', 'identf'], writes=[P[B0]])
        fw.op('act', lambda e, i=i: e.activation(out=selT_all[:, i, :, :], in_=pst, func=AF.Copy),
              reads=[P[B0]], writes=[('selT', i)])
    stage_F(0)
    for i in range(NO):
        if i + 1 < NO:
            stage_F(i + 1)
        stage_T(i)
    fw.barrier()
    p41.close()
    if STOP_AFTER <= 4:
        p4b.close()
        p4.close()
        return finish(nc, fw, y, glob)

    p42 = ExitStack()
    iota128 = sb(p42, "iota128_sb", [128, 128], BF16)
    TB = 32
    Aoh = [sb(p42, "Aoh%d" % k, [128, TB, 128], BF16) for k in range(2)]
    Boh = [sb(p42, "Boh%d" % k, [128, TB, 128], BF16) for k in range(2)]
    Gs = sb(p42, "Gs", [128, 128, 256], BF16)
    fw.dma('pool', lambda e: e.dma_start(out=iota128[:], in_=iota128d), writes=['iota128'])
    nblk = 0
    nev = 0
    for i in range(NO):
        ch = i // 2
        for blk in range(128 // TB):
            ab = nblk % 2
            nblk += 1
            t0 = blk * TB
            io3 = iota128[:].unsqueeze(1).to_broadcast([128, TB, 128])
            for t in range(TB):
                fw.op('dve', lambda e, t=t: e.tensor_scalar(
                    out=Aoh[ab][:, t, :], in0=iota128[:], scalar1=selT_all[:, i, 0, t0 + t:t0 + t + 1],
                    scalar2=selT_all[:, i, 2, t0 + t:t0 + t + 1], op0=ALU.is_equal, op1=ALU.mult),
                    reads=['iota128'], writes=[('Aoh', ab, t)])
            fw.op('dve', lambda e: e.tensor_tensor(
                out=Boh[ab][:], in0=io3, in1=selT_all[:, i, 1, t0:t0 + TB].unsqueeze(2).to_broadcast([128, TB, 128]),
                op=ALU.is_equal), reads=['iota128'], writes=[('Boh', ab)])
            for q16 in range(TB // 16):
                grp = nev % 2
                nev += 1
                for tt in range(16):
                    t = q16 * 16 + tt
                    fw.op('pe', lambda e, t=t, tt=tt, grp=grp: e.matmul(
                        psall[:, grp * 2048 + tt * 128:grp * 2048 + (tt + 1) * 128],
                        lhsT=Aoh[ab][:, t, :], rhs=Boh[ab][:, t, :], start=True, stop=True),
                        reads=[('Aoh', ab, t), ('Boh', ab)], writes=[('psg', grp)])
                tg = (i % 2) * 128 + t0 + q16 * 16
                dst = Gs[:, :, tg:tg + 16]
                srcp = psall[:, grp * 2048:(grp + 1) * 2048].rearrange("p (t j) -> p j t", t=16)
                fw.op('act', lambda e, dst=dst, srcp=srcp: e.activation(out=dst, in_=srcp, func=AF.Copy),
                      reads=[('psg', grp)], writes=['Gs'])
        if i % 2 == 1:
            for jb in range(8):
                fw.dma('sp', lambda e, jb=jb, ch=ch: e.dma_start(
                    out=Gscr[jb * 16:(jb + 1) * 16, :, ch * 256:(ch + 1) * 256].rearrange("j i t -> i j t"),
                    in_=Gs[:, jb * 16:(jb + 1) * 16, :]),
                    reads=['Gs'], writes=['Gscr'])
    fw.barrier()
    p42.close()
    p4b.close()
    if STOP_AFTER <= 5:
        p4.close()
        return finish(nc, fw, y, glob)

    p43 = ExitStack()
    JG = 4
    NJG = 128 // JG
    acc = sb(p43, "acc", [128, NO, D], F32)
    gfin_sb = sb(p43, "gfin_sb", [128, D], F32)
    Uj = [sb(p43, "Uj%d" % k, [128, 8, 128], BF16) for k in range(2 * JG)]
    Vj = [sb(p43, "Vj%d" % k, [128, D], BF16) for k in range(2 * JG)]
    Gj = [sb(p43, "Gj%d" % k, [128, 2048], BF16) for k in range(2)]
    gl = [sb(p43, "gl%d" % k, [128, 512], BF16) for k in range(2)]
    AT = [sb(p43, "AT%d" % k, [128, 2048], BF16) for k in range(2 * JG)]
    junk3 = sb(p43, "junk3", [128, D], F32)
    ss3 = sb(p43, "ss3", [128, 1], F32)
    rs3 = sb(p43, "rs3", [128, 1], F32)
    yo = [sb(p43, "yo%d" % k, [128, D], F32) for k in range(2)]
    fw.dma('sp', lambda e: e.dma_start(out=gfin_sb[:], in_=g_fin), writes=['gfin'])
    for i in range(NO):
        fw.dma('sp', lambda e, i=i: e.dma_start(out=acc[:, i, :], in_=HS[i]), reads=['HS'], writes=[('acc', i)])

    cnt = {'h': 0, 'g': 0, 'o': 0}

    def H_units(jg):
        units = []
        for jj in range(JG):
            j = jg * JG + jj
            slot = (jg % 2) * JG + jj

            def load(j=j, slot=slot):
                fw.dma('pool', lambda e: e.dma_start(out=Uj[slot][:], in_=Ur[j]), writes=[('Uj', slot)])
                fw.dma('pool', lambda e: e.dma_start(out=Vj[slot][:], in_=Vr[j]), writes=[('Vj', slot)])
            for tg in range(4):
                def unit(j=j, slot=slot, tg=tg, load=load):
                    if tg == 0:
                        load()
                        gsl = cnt['g'] % 2
                        cnt['g'] += 1
                        fw.dma('sp', lambda e: e.dma_start(out=Gj[gsl][:], in_=Gscr[j]), reads=['Gscr'], writes=[('Gj', gsl)])
                        unit_state['gsl'] = gsl
                    gsl = unit_state['gsl']
                    hb = cnt['h'] % 2
                    cnt['h'] += 1
                    for c in range(8):
                        fw.op('pe', lambda e, c=c: e.matmul(
                            ps[hb][:], lhsT=Uj[slot][:, c, :], rhs=xn2T_all[:, c, tg * 512:(tg + 1) * 512],
                            start=(c == 0), stop=(c == 7)),
                            reads=[('Uj', slot), 'xn2T_all'], writes=[P[hb]])
                    fw.op('act', lambda e: e.activation(out=gl[hb][:], in_=ps[hb][:], func=AF.Gelu),
                          reads=[P[hb]], writes=[('gl', hb)])
                    fw.op('dve', lambda e: e.tensor_tensor(
                        out=AT[slot][:, tg * 512:(tg + 1) * 512], in0=gl[hb][:], in1=Gj[gsl][:, tg * 512:(tg + 1) * 512],
                        op=ALU.mult),
                        reads=[('gl', hb), ('Gj', gsl)], writes=[('AT', slot, tg)])
                units.append(unit)
        return units

    unit_state = {}

    def V_units(jg):
        units = []
        for tile in range(NO):
            for half in range(2):
                def unit(tile=tile, half=half):
                    ob = 2 + (cnt['o'] % 6)
                    cnt['o'] += 1
                    for jj in range(JG):
                        slot = (jg % 2) * JG + jj
                        fw.op('pe', lambda e, jj=jj, slot=slot: e.matmul(
                            ps[ob][:], lhsT=AT[slot][:, tile * 128:(tile + 1) * 128],
                            rhs=Vj[slot][:, half * 512:(half + 1) * 512], start=(jj == 0), stop=(jj == JG - 1)),
                            reads=[('AT', slot, tile // 4), ('Vj', slot)], writes=[P[ob]])
                    dst = acc[:, tile, half * 512:(half + 1) * 512]
                    fw.op('dve', lambda e, dst=dst, ob=ob: e.tensor_tensor(out=dst, in0=dst, in1=ps[ob][:], op=ALU.add),
                          reads=[P[ob], ('acc', tile)], writes=[('acc', tile)])
                units.append(unit)
        return units

    for u in H_units(0):
        u()
    for jg in range(NJG):
        hu = H_units(jg + 1) if jg + 1 < NJG else []
        vu = V_units(jg)
        hi_ = 0
        for k, v in enumerate(vu):
            if k % 2 == 0 and hi_ < len(hu):
                hu[hi_]()
                hi_ += 1
            v()
        while hi_ < len(hu):
            hu[hi_]()
            hi_ += 1
    for i in range(NO):
        s = i % 2
        fw.op('act', lambda e, i=i: e.activation(out=junk3[:], in_=acc[:, i, :], func=AF.Square, accum_out=ss3[:]),
              reads=[('acc', i)], writes=['junk3', 'ss3'])
        fw.op('act', lambda e: e.activation(out=rs3[:], in_=ss3[:], func=AF.Sqrt, bias=eps_sb[:], scale=1.0 / D),
              reads=['ss3', 'eps_sb'], writes=['rs3'])
        fw.op('dve', lambda e: e.reciprocal(out=rs3[:], in_=rs3[:]), reads=['rs3'], writes=['rs3'])
        fw.op('dve', lambda e, i=i: e.scalar_tensor_tensor(out=yo[s][:], in0=acc[:, i, :], scalar=rs3[:, 0:1], in1=gfin_sb[:],
                                                           op0=ALU.mult, op1=ALU.mult),
              reads=[('acc', i), 'rs3', 'gfin'], writes=[('yo', s)])
        fw.dma('sp', lambda e, i=i: e.dma_start(out=y[i], in_=yo[s][:]), reads=[('yo', s)], writes=['y'])
    fw.barrier()
    p43.close()
    p4.close()
    return finish(nc, fw, y, glob)


def finish(nc, fw, y, glob):
    fw.barrier()
    glob.close()
    return nc


def prep(inputs):
    x = np.asarray(inputs["x"], np.float32)
    w_in = np.asarray(inputs["w_in"], np.float32)[0]

    def pc(w):
        return np.ascontiguousarray(w.reshape(8, 128, -1).transpose(1, 0, 2))

    cols_kv = np.r_[512:1024, 1024:1536, 2048:2560, 2560:3072, 3072:3080]
    cols_q = np.r_[0:512, 1536:2048]
    cols_g = np.r_[3080:5128]
    shared = {
        "w_kv": pc(w_in[:, cols_kv]),
        "w_q": pc(w_in[:, cols_q]),
        "w_g": pc(w_in[:, cols_g]),
        "g_mix": np.ascontiguousarray(np.asarray(inputs["norm_mix_g"], np.float32)[0].reshape(8, 128).T),
        "bfg": np.ascontiguousarray(np.broadcast_to(np.asarray(inputs["b_forget"], np.float32)[0][None, :], (128, 8))),
        "wba": np.ascontiguousarray(np.asarray(inputs["w_branch_a"], np.float32)[0].reshape(8, 64, D).transpose(1, 0, 2)),
        "wbb": np.ascontiguousarray(np.asarray(inputs["w_branch_b"], np.float32)[0].reshape(8, 64, D).transpose(1, 0, 2)),
        "wout": pc(np.asarray(inputs["w_out"], np.float32)[0]),
        "g_ffn": np.ascontiguousarray(np.broadcast_to(np.asarray(inputs["norm_ffn_g"], np.float32)[0][None, :], (128, D))),
        "wpq": pc(np.asarray(inputs["w_peer_q"], np.float32)[0]),
        "skT": np.ascontiguousarray(np.asarray(inputs["peer_sub_keys"], np.float32)[0].reshape(16, 128, 128).transpose(2, 0, 1)),
        "Ur": np.ascontiguousarray(np.asarray(inputs["peer_expert_u"], np.float32)[0].reshape(128, 128, 8, 128).transpose(1, 3, 2, 0)),
        "Vr": np.ascontiguousarray(np.asarray(inputs["peer_expert_v"], np.float32)[0].reshape(128, 128, D).transpose(1, 0, 2)),
        "iota128": np.ascontiguousarray(np.broadcast_to(np.arange(128, dtype=np.float32)[None, :], (128, 128))),
        "g_fin": np.ascontiguousarray(np.broadcast_to(np.asarray(inputs["norm_final_g"], np.float32)[None, :], (128, D))),
    }
    s_ = np.arange(128)[:, None]
    t_ = np.arange(512)[None, :]
    trim = np.stack([(o * 128 + s_ <= t_) for o in range(4)], axis=1).astype(np.float32)
    shared["trim"] = np.ascontiguousarray(trim)
    shared["tric"] = (np.arange(128)[:, None] <= np.arange(128)[None, :]).astype(np.float32)
    shared["onehot"] = (np.arange(32)[:, None] == (np.arange(8192)[None, :] // 256)).astype(np.float32)
    shared["iota16"] = np.ascontiguousarray(np.broadcast_to(np.arange(16, dtype=np.float32)[None, :], (128, 16)))
    half = 8
    inv_freq = np.power(np.float32(500000.0), -np.arange(half, dtype=np.float32) / np.float32(half)).astype(np.float32)
    maps = []
    for c in range(8):
        b = c // 4
        order = chunk_order(c)
        tok = np.concatenate([np.arange(k * 1024, (k + 1) * 1024) for k in order])
        xb = x[b][tok]
        m = dict(shared)
        m["xT"] = np.ascontiguousarray(xb.reshape(NT, 128, 8, 128).transpose(0, 3, 2, 1))
        m["xown"] = np.ascontiguousarray(xb[:2048].reshape(NO, 128, D))
        ang = tok.astype(np.float32)[:, None] * inv_freq[None, :]
        m["ropec"] = np.ascontiguousarray(np.cos(ang).astype(np.float32).reshape(NT, 128, 8).transpose(1, 0, 2))
        m["ropes"] = np.ascontiguousarray(np.sin(ang).astype(np.float32).reshape(NT, 128, 8).transpose(1, 0, 2))
        true_blk = np.array([order[n // 4] * 4 + n % 4 for n in range(32)])
        true_tile = np.array([order[n // 8] * 8 + n % 8 for n in range(NT)])
        pastb = np.zeros((NO, 32), np.float32)
        ownm = np.zeros((NO, 32), np.float32)
        units = core_units(c)
        slot_sel = np.array([-1, -1] + [q for (q, _) in units])
        for i in range(NO):
            qb = true_blk[i // 2]
            qsel = i // 8
            slot_of_blk = np.arange(32) // 4
            allowed = (slot_of_blk == 0) | ((slot_of_blk == 1) & (qsel == 1)) | (slot_sel[slot_of_blk] == qsel)
            pastb[i] = np.where((true_blk < qb) & allowed, 0.0, -1e30)
            ownm[i, i // 2] = 1.0
        uw = np.zeros((6, 2), np.float32)
        for u, (q, _) in enumerate(units):
            uw[u, q] = 1.0
        m["uw"] = np.ascontiguousarray(np.broadcast_to(uw[None], (128, 6, 2)))
        m["pastb"] = np.ascontiguousarray(np.broadcast_to(pastb[None], (128, NO, 32)))
        m["ownm"] = np.ascontiguousarray(np.broadcast_to(ownm[None], (128, NO, 32)))
        chunk_of_tile = np.array([order[n // 8] for n in range(NT)])
        visA = np.zeros(NT, np.float32)
        visB = np.zeros(NT, np.float32)
        m["visA"] = np.ascontiguousarray(np.broadcast_to(visA[None], (128, NT)))
        m["visB"] = np.ascontiguousarray(np.broadcast_to(visB[None], (128, NT)))
        first_occ = np.array([n == int(np.argmax(true_tile == true_tile[n])) for n in range(NT)])
        m["Mord"] = ((true_tile[:, None] < true_tile[None, :]) & first_occ[:, None]).astype(np.float32)
        maps.append(m)
    return maps


_NC = None


def kernel(**inputs):
    global _NC
    maps = prep(inputs)
    if _NC is None:
        _NC = build()
    res = run_bass_kernel_spmd(_NC, maps, core_ids=list(range(8)))
    out = np.zeros((2, 8192, D), np.float32)
    for c in range(8):
        b = c // 4
        order = chunk_order(c)
        yc = np.asarray(res.results[c]["y"]).reshape(2048, D)
        out[b, order[0] * 1024:(order[0] + 1) * 1024] = yc[:1024]
        out[b, order[1] * 1024:(order[1] + 1) * 1024] = yc[1024:]
    return out
```
